# Optimizing a Trainium2 kernel written in Bass

```python
import jax, jax.numpy as jnp
from jax import lax
import numpy as np

D_MODEL = 1024
BATCH = 2
SEQ = 8192
DEPTH = 4

GRID_W = 64
CTX_LEN = 256
EPS = 1e-6

MLA_HEADS = 8
MLA_NOPE = 64
MLA_ROPE = 32
MLA_QK = MLA_NOPE + MLA_ROPE
MLA_V = 64
Q_LORA = 256
KV_LORA = 128
ROPE_THETA = 10000.0
Q_BLOCK = 128

ML_HEADS = 4
ML_DH = 64
ML_W = ML_HEADS * ML_DH
ML_CONV = 5
ML_CHUNK = 64

GLA_HEADS = 4
GLA_DK = 32
GLA_DV = 64
GLA_LR = 16
GLA_TAU = 16.0
GLA_CHUNK = 64

N_GROUPS = 4
EXP_PER_GROUP = 8
N_EXPERTS = N_GROUPS * EXP_PER_GROUP
TOP_K = 2
D_EXPERT = 256
MOE_BLOCK = 128

IN_WIDTHS = (Q_LORA, KV_LORA, MLA_ROPE, ML_W, ML_W, ML_W, 4 * ML_HEADS,
             GLA_HEADS * GLA_DK, GLA_HEADS * GLA_DK, GLA_HEADS * GLA_DV, GLA_HEADS * GLA_DV, 2 * GLA_LR)
D_IN = sum(IN_WIDTHS)
D_MIX = MLA_HEADS * MLA_V + ML_W + GLA_HEADS * GLA_DV

kernel_name = 'hybrid_mla_mlstm_gla_hmoe_dit'


def _split_cols(p):
    offs = np.cumsum(IN_WIDTHS)[:-1].tolist()
    return jnp.split(p, offs, axis=-1)


def _rms_norm(x, w):
    xf = x.astype(jnp.float32)
    y = xf * lax.rsqrt(jnp.mean(xf * xf, axis=-1, keepdims=True) + EPS)
    return (y * w.astype(jnp.float32)).astype(x.dtype)


def _modulate(x, w, shift, scale):
    return _rms_norm(x, w) * (1 + scale) + shift


def _axial_rope(n_tokens, dtype):
    rows = n_tokens // GRID_W
    row = jnp.broadcast_to(jnp.arange(rows, dtype=jnp.float32)[:, None], (rows, GRID_W)).reshape(-1)
    col = jnp.broadcast_to(jnp.arange(GRID_W, dtype=jnp.float32)[None, :], (rows, GRID_W)).reshape(-1)
    n_freq = MLA_ROPE // 4
    inv = ROPE_THETA ** (-jnp.arange(n_freq, dtype=jnp.float32) / n_freq)
    ang = jnp.concatenate([row[:, None] * inv, col[:, None] * inv], axis=-1)
    return jnp.cos(ang).astype(dtype), jnp.sin(ang).astype(dtype)


def _apply_rope(x, cos, sin):
    half = MLA_ROPE // 2
    x1, x2 = x[..., :half], x[..., half:]
    return jnp.concatenate([x1 * cos - x2 * sin, x1 * sin + x2 * cos], axis=-1)


def _mla_qkv(cq, ckv, kr, q_a_norm, w_uq, kv_a_norm, w_ukv, q_norm_w, k_norm_w, rope):
    b, t, _ = cq.shape
    q = (_rms_norm(cq, q_a_norm) @ w_uq).reshape(b, t, MLA_HEADS, MLA_QK)
    kv = (_rms_norm(ckv, kv_a_norm) @ w_ukv).reshape(b, t, MLA_HEADS, MLA_NOPE + MLA_V)
    k = jnp.concatenate([kv[..., :MLA_NOPE],
                         jnp.broadcast_to(kr[:, :, None, :], (b, t, MLA_HEADS, MLA_ROPE))], axis=-1)
    v = kv[..., MLA_NOPE:]
    q = _rms_norm(q, q_norm_w)
    k = _rms_norm(k, k_norm_w)
    if rope is not None:
        cos, sin = rope[0][:, None, :], rope[1][:, None, :]
        q = jnp.concatenate([q[..., :MLA_NOPE], _apply_rope(q[..., MLA_NOPE:], cos, sin)], axis=-1)
        k = jnp.concatenate([k[..., :MLA_NOPE], _apply_rope(k[..., MLA_NOPE:], cos, sin)], axis=-1)
    return q, k, v


def _attend(q, k, v):
    s = jnp.einsum('bqhd,bkhd->bhqk', q, k, preferred_element_type=jnp.float32) * (MLA_QK ** -0.5)
    p = jax.nn.softmax(s, axis=-1).astype(v.dtype)
    return jnp.einsum('bhqk,bkhd->bqhd', p, v)


def _latent_attention(q, k_lat, v_lat, k_ctx, v_ctx):
    b, s, h, _ = q.shape
    k_all = jnp.concatenate([k_lat, k_ctx], axis=1)
    v_all = jnp.concatenate([v_lat, v_ctx], axis=1)
    qb = q.reshape(b, s // Q_BLOCK, Q_BLOCK, h, MLA_QK).transpose(1, 0, 2, 3, 4)
    o = lax.map(lambda qi: _attend(qi, k_all, v_all), qb)
    return o.transpose(1, 0, 2, 3, 4).reshape(b, s, h * MLA_V)


def _dwconv(x, w, b):
    k, ch = w.shape
    y = lax.conv_general_dilated(x, w[:, None, :].astype(x.dtype), window_strides=(1,),
                                 padding=[(k // 2, k // 2)],
                                 dimension_numbers=('NWC', 'WIO', 'NWC'),
                                 feature_group_count=ch)
    return y + b


def _mlstm_prep(xm, v, gates, conv_w, conv_b, wq, wk, gate_b):
    b, t, _ = xm.shape
    xconv = jax.nn.silu(_dwconv(xm, conv_w, conv_b))
    xh = xconv.reshape(b, t, ML_HEADS, ML_DH)
    q = jnp.einsum('bthd,hde->bhte', xh, wq).astype(jnp.float32) * (ML_DH ** -0.5)
    k = jnp.einsum('bthd,hde->bhte', xh, wk).astype(jnp.float32)
    v = v.reshape(b, t, ML_HEADS, ML_DH).transpose(0, 2, 1, 3).astype(jnp.float32)
    g = (gates.astype(jnp.float32) + gate_b.astype(jnp.float32)).reshape(b, t, 2, 2, ML_HEADS)
    g = g.transpose(2, 3, 0, 4, 1)
    logi = g[:, 0]
    logf = jax.nn.log_sigmoid(g[:, 1])
    return xconv, (q, k, v, logi[0], logf[0]), (q, k, v, logi[1], logf[1])


def _mlstm_scan(q, k, v, logi, logf, state):
    n_chunk = q.shape[2] // ML_CHUNK
    tri = jnp.tril(jnp.ones((ML_CHUNK, ML_CHUNK), dtype=bool))

    def chunks(a):
        return jnp.moveaxis(a.reshape(a.shape[:2] + (n_chunk, ML_CHUNK) + a.shape[3:]), 2, 0)

    def step(carry, xs):
        c_st, n_st, m_st = carry
        qc, kc, vc, ic, fc = xs
        bcum = jnp.cumsum(fc, axis=-1)
        d = jnp.where(tri, bcum[..., :, None] - bcum[..., None, :] + ic[..., None, :], -jnp.inf)
        inter = bcum + m_st[..., None]
        m_t = jnp.maximum(inter, jnp.max(d, axis=-1))
        s = jnp.einsum('bhtd,bhjd->bhtj', qc, kc) * jnp.exp(d - m_t[..., None])
        w_inter = jnp.exp(inter - m_t)
        num = (jnp.einsum('bhtj,bhjv->bhtv', s, vc)
               + w_inter[..., None] * jnp.einsum('bhtd,bhdv->bhtv', qc, c_st))
        den = jnp.sum(s, axis=-1) + w_inter * jnp.einsum('bhtd,bhd->bht', qc, n_st)
        h = num / jnp.maximum(jnp.abs(den), jnp.exp(-m_t))[..., None]
        b_end = bcum[..., -1]
        g = b_end[..., None] - bcum + ic
        m_new = jnp.maximum(b_end + m_st, jnp.max(g, axis=-1))
        w_j = jnp.exp(g - m_new[..., None])
        decay = jnp.exp(b_end + m_st - m_new)
        c_new = decay[..., None, None] * c_st + jnp.einsum('bhj,bhjd,bhjv->bhdv', w_j, kc, vc)
        n_new = decay[..., None] * n_st + jnp.einsum('bhj,bhjd->bhd', w_j, kc)
        return (c_new, n_new, m_new), h

    state, hs = lax.scan(step, state, tuple(chunks(a) for a in (q, k, v, logi, logf)))
    h = jnp.moveaxis(hs, 0, 2)
    return h.reshape(h.shape[:2] + (-1, h.shape[-1])), state


def _mlstm_out(h, xconv, o, norm_w, skip):
    b, nh, t, dv = h.shape
    hn = _rms_norm(h.transpose(0, 2, 1, 3), norm_w.reshape(nh, dv)).reshape(b, t, nh * dv)
    return jax.nn.sigmoid(o) * (hn.astype(xconv.dtype) + skip * xconv)


def _gla_prep(q, k, v, a_lr, wa, ba):
    b, t, _ = q.shape

    def heads(a, d):
        return a.reshape(b, t, GLA_HEADS, d).transpose(0, 2, 1, 3).astype(jnp.float32)

    q = heads(q, GLA_DK) * (GLA_DK ** -0.5)
    k = heads(k, GLA_DK)
    v = heads(v, GLA_DV)
    a = a_lr.reshape(b, t, 2, GLA_LR)
    pre = jnp.einsum('btur,urk->ubtk', a, wa).astype(jnp.float32) + ba[:, None, None, :].astype(jnp.float32)
    loga = jax.nn.log_sigmoid(pre) / GLA_TAU
    loga = loga.reshape(2, b, t, GLA_HEADS, GLA_DK).transpose(0, 1, 3, 2, 4)
    return (q, k, v, loga[0]), (q, k, v, loga[1])


def _gla_scan(q, k, v, loga, state):
    n_chunk = q.shape[2] // GLA_CHUNK
    tri = jnp.tril(jnp.ones((GLA_CHUNK, GLA_CHUNK), dtype=bool))[:, :, None]

    def chunks(a):
        return jnp.moveaxis(a.reshape(a.shape[:2] + (n_chunk, GLA_CHUNK) + a.shape[3:]), 2, 0)

    def step(s_st, xs):
        qc, kc, vc, ac = xs
        bcum = jnp.cumsum(ac, axis=2)
        diff = jnp.where(tri, bcum[:, :, :, None, :] - bcum[:, :, None, :, :], -jnp.inf)
        att = jnp.einsum('bhtd,bhjd,bhtjd->bhtj', qc, kc, jnp.exp(diff))
        o = jnp.einsum('bhtj,bhjv->bhtv', att, vc) + jnp.einsum('bhtd,bhdv->bhtv', qc * jnp.exp(bcum), s_st)
        b_end = bcum[:, :, -1]
        s_new = (jnp.exp(b_end)[..., None] * s_st
                 + jnp.einsum('bhjd,bhjv->bhdv', kc * jnp.exp(b_end[:, :, None, :] - bcum), vc))
        return s_new, o

    state, os_ = lax.scan(step, state, tuple(chunks(a) for a in (q, k, v, loga)))
    o = jnp.moveaxis(os_, 0, 2)
    return o.reshape(o.shape[:2] + (-1, o.shape[-1])), state


def _gla_out(o, r, norm_w):
    b, nh, t, dv = o.shape
    on = _rms_norm(o.transpose(0, 2, 1, 3), norm_w.reshape(nh, dv)).reshape(b, t, nh * dv)
    return on.astype(r.dtype) * jax.nn.silu(r)


def _bidirectional(scan_fn, init, ctx_fwd, ctx_bwd, lat_fwd, lat_bwd):
    def flip(args):
        return tuple(jnp.flip(a, axis=2) for a in args)

    h_cf, st_f = scan_fn(*ctx_fwd, init)
    h_lf, _ = scan_fn(*lat_fwd, st_f)
    h_cb, st_b = scan_fn(*flip(ctx_bwd), init)
    h_lb, _ = scan_fn(*flip(lat_bwd), st_b)
    return h_cf + jnp.flip(h_cb, axis=2), h_lf + jnp.flip(h_lb, axis=2)


def _moe(h, w_grp, b_grp, w_erouter, b_erouter, w_gate, w_up, w_down):
    n, d = h.shape
    hb = h.reshape(n // MOE_BLOCK, MOE_BLOCK, d)

    def blk(t):
        g_prob = jax.nn.softmax((t @ w_grp + b_grp).astype(jnp.float32), axis=-1)
        g_w, g_i = lax.top_k(g_prob, 1)
        e_logit = (t @ w_erouter + b_erouter).astype(jnp.float32).reshape(-1, N_GROUPS, EXP_PER_GROUP)
        e_sel = e_logit[jnp.arange(t.shape[0]), g_i[:, 0]]
        e_w, e_i = lax.top_k(jax.nn.softmax(e_sel, axis=-1), TOP_K)
        e_w = e_w / jnp.sum(e_w, axis=-1, keepdims=True)
        wts = g_w * e_w
        ids = g_i * EXP_PER_GROUP + e_i
        gate = jnp.sum(jax.nn.one_hot(ids, N_EXPERTS, dtype=jnp.float32) * wts[..., None], axis=1)
        a = jax.nn.silu(jnp.einsum('nd,edf->nef', t, w_gate)) * jnp.einsum('nd,edf->nef', t, w_up)
        a = a * gate[..., None].astype(a.dtype)
        return jnp.einsum('nef,efd->nd', a, w_down)

    return lax.map(blk, hb).reshape(n, d)


def _layer(x, xc, c, c_ctx, w_mod, b_mod, norm1_w, w_in, q_a_norm, w_uq, kv_a_norm, w_ukv,
           q_norm_w, k_norm_w, ml_conv_w, ml_conv_b, ml_wq, ml_wk, ml_gate_b, ml_norm_w, ml_skip,
           gla_wa, gla_ba, gla_norm_w, w_out, norm2_w, w_grp, b_grp, w_erouter, b_erouter,
           w_gate, w_up, w_down, rope, need_ctx):
    b, s, d = x.shape
    mod_l = jnp.split((jax.nn.silu(c) @ w_mod + b_mod)[:, None, :], 6, axis=-1)
    mod_c = jnp.split((jax.nn.silu(c_ctx) @ w_mod + b_mod)[None, None, :], 6, axis=-1)

    (cq_l, ckv_l, kr_l, mx_l, mv_l, mo_l, mg_l, gq_l, gk_l, gv_l, gr_l, ga_l) = _split_cols(
        _modulate(x, norm1_w, mod_l[0], mod_l[1]) @ w_in)
    (cq_c, ckv_c, kr_c, mx_c, mv_c, mo_c, mg_c, gq_c, gk_c, gv_c, gr_c, ga_c) = _split_cols(
        _modulate(xc, norm1_w, mod_c[0], mod_c[1]) @ w_in)

    q_l, k_l, v_l = _mla_qkv(cq_l, ckv_l, kr_l, q_a_norm, w_uq, kv_a_norm, w_ukv, q_norm_w, k_norm_w, rope)
    q_c, k_c, v_c = _mla_qkv(cq_c, ckv_c, kr_c, q_a_norm, w_uq, kv_a_norm, w_ukv, q_norm_w, k_norm_w, None)
    a_l = _latent_attention(q_l, k_l, v_l, k_c, v_c)

    xconv_l, mf_l, mb_l = _mlstm_prep(mx_l, mv_l, mg_l, ml_conv_w, ml_conv_b, ml_wq, ml_wk, ml_gate_b)
    xconv_c, mf_c, mb_c = _mlstm_prep(mx_c, mv_c, mg_c, ml_conv_w, ml_conv_b, ml_wq, ml_wk, ml_gate_b)
    ml_init = (jnp.zeros((b, ML_HEADS, ML_DH, ML_DH), jnp.float32),
               jnp.zeros((b, ML_HEADS, ML_DH), jnp.float32),
               jnp.zeros((b, ML_HEADS), jnp.float32))
    mh_c, mh_l = _bidirectional(_mlstm_scan, ml_init, mf_c, mb_c, mf_l, mb_l)
    m_l = _mlstm_out(mh_l, xconv_l, mo_l, ml_norm_w, ml_skip)

    gf_l, gb_l = _gla_prep(gq_l, gk_l, gv_l, ga_l, gla_wa, gla_ba)
    gf_c, gb_c = _gla_prep(gq_c, gk_c, gv_c, ga_c, gla_wa, gla_ba)
    gla_init = jnp.zeros((b, GLA_HEADS, GLA_DK, GLA_DV), jnp.float32)
    go_c, go_l = _bidirectional(_gla_scan, gla_init, gf_c, gb_c, gf_l, gb_l)
    g_l = _gla_out(go_l, gr_l, gla_norm_w)

    x = x + mod_l[2] * (jnp.concatenate([a_l, m_l, g_l], axis=-1) @ w_out)
    h2 = _modulate(x, norm2_w, mod_l[3], mod_l[4])
    x = x + mod_l[5] * _moe(h2.reshape(b * s, d), w_grp, b_grp, w_erouter, b_erouter,
                           w_gate, w_up, w_down).reshape(b, s, d)

    if need_ctx:
        n_ctx = xc.shape[1]
        a_c = _attend(q_c, k_c, v_c).reshape(b, n_ctx, MLA_HEADS * MLA_V)
        m_c = _mlstm_out(mh_c, xconv_c, mo_c, ml_norm_w, ml_skip)
        g_c = _gla_out(go_c, gr_c, gla_norm_w)
        xc = xc + mod_c[2] * (jnp.concatenate([a_c, m_c, g_c], axis=-1) @ w_out)
        h2c = _modulate(xc, norm2_w, mod_c[3], mod_c[4])
        xc = xc + mod_c[5] * _moe(h2c.reshape(b * n_ctx, d), w_grp, b_grp, w_erouter, b_erouter,
                                 w_gate, w_up, w_down).reshape(b, n_ctx, d)
    return x, xc


def setup_inputs(seed: int = 0) -> dict:
    key = jax.random.key(seed)
    ks = iter(jax.random.split(key, 48))

    def nrm(shape, scale):
        return scale * jax.random.normal(next(ks), shape, dtype=jnp.float32)

    def gain(shape):
        return 1.0 + nrm(shape, 0.02)

    L, D = DEPTH, D_MODEL
    ib = nrm((L, 2, 1, ML_HEADS), 0.1)
    fb = jnp.linspace(3.0, 6.0, ML_HEADS, dtype=jnp.float32) + nrm((L, 2, 1, ML_HEADS), 0.1)
    ml_gate_b = jnp.concatenate([ib, fb], axis=2).reshape(L, 4 * ML_HEADS)
    return {
        'x': nrm((BATCH, SEQ, D), 1.0),
        'c': nrm((BATCH, D), 1.0),
        'ctx': nrm((BATCH, CTX_LEN, D), 1.0),
        'c_ctx': nrm((D,), 1.0),
        'w_mod': nrm((L, D, 6 * D), 0.5 * D ** -0.5),
        'b_mod': nrm((L, 6 * D), 0.01),
        'norm1_w': gain((L, D)),
        'w_in': nrm((L, D, D_IN), D ** -0.5),
        'q_a_norm': gain((L, Q_LORA)),
        'w_uq': nrm((L, Q_LORA, MLA_HEADS * MLA_QK), Q_LORA ** -0.5),
        'kv_a_norm': gain((L, KV_LORA)),
        'w_ukv': nrm((L, KV_LORA, MLA_HEADS * (MLA_NOPE + MLA_V)), KV_LORA ** -0.5),
        'q_norm_w': gain((L, MLA_QK)),
        'k_norm_w': gain((L, MLA_QK)),
        'ml_conv_w': nrm((L, ML_CONV, ML_W), ML_CONV ** -0.5),
        'ml_conv_b': nrm((L, ML_W), 0.01),
        'ml_wq': nrm((L, ML_HEADS, ML_DH, ML_DH), ML_DH ** -0.5),
        'ml_wk': nrm((L, ML_HEADS, ML_DH, ML_DH), ML_DH ** -0.5),
        'ml_gate_b': ml_gate_b,
        'ml_norm_w': gain((L, ML_W)),
        'ml_skip': gain((L, ML_W)),
        'gla_wa': nrm((L, 2, GLA_LR, GLA_HEADS * GLA_DK), GLA_LR ** -0.5),
        'gla_ba': nrm((L, 2, GLA_HEADS * GLA_DK), 0.1),
        'gla_norm_w': gain((L, GLA_HEADS * GLA_DV)),
        'w_out': nrm((L, D_MIX, D), D_MIX ** -0.5),
        'norm2_w': gain((L, D)),
        'w_grp': nrm((L, D, N_GROUPS), D ** -0.5),
        'b_grp': nrm((L, N_GROUPS), 0.01),
        'w_erouter': nrm((L, D, N_EXPERTS), D ** -0.5),
        'b_erouter': nrm((L, N_EXPERTS), 0.01),
        'w_gate': nrm((L, N_EXPERTS, D, D_EXPERT), D ** -0.5),
        'w_up': nrm((L, N_EXPERTS, D, D_EXPERT), D ** -0.5),
        'w_down': nrm((L, N_EXPERTS, D_EXPERT, D), D_EXPERT ** -0.5),
    }


def reference(x, c, ctx, c_ctx, w_mod, b_mod, norm1_w, w_in, q_a_norm, w_uq, kv_a_norm, w_ukv,
              q_norm_w, k_norm_w, ml_conv_w, ml_conv_b, ml_wq, ml_wk, ml_gate_b, ml_norm_w, ml_skip,
              gla_wa, gla_ba, gla_norm_w, w_out, norm2_w, w_grp, b_grp, w_erouter, b_erouter,
              w_gate, w_up, w_down):
    rope = _axial_rope(x.shape[1], x.dtype)
    xc = ctx
    for l in range(DEPTH):
        x, xc = _layer(x, xc, c, c_ctx, w_mod[l], b_mod[l], norm1_w[l], w_in[l], q_a_norm[l], w_uq[l],
                       kv_a_norm[l], w_ukv[l], q_norm_w[l], k_norm_w[l], ml_conv_w[l], ml_conv_b[l],
                       ml_wq[l], ml_wk[l], ml_gate_b[l], ml_norm_w[l], ml_skip[l], gla_wa[l], gla_ba[l],
                       gla_norm_w[l], w_out[l], norm2_w[l], w_grp[l], b_grp[l], w_erouter[l], b_erouter[l],
                       w_gate[l], w_up[l], w_down[l], rope=rope, need_ctx=(l < DEPTH - 1))
    return x
```

```python
import numpy as np
import ml_dtypes
from contextlib import ExitStack
import concourse.bass as bass
import concourse.mybir as mybir
from concourse.bass_utils import run_bass_kernel_spmd

F32 = mybir.dt.float32
BF16 = mybir.dt.bfloat16
AF = mybir.ActivationFunctionType
ALU = mybir.AluOpType
AX = mybir.AxisListType

D = 1024
NB = 2
SEQ = 8192
DEPTH = 4
NCTX = 256
EPS = 1e-6
NCORE = 8
TL = 2048
NT = 18
NTOK = NT * 128
D_IN = 2000
O_CQ, O_CKV, O_KR, O_MX, O_MV, O_MO, O_MG, O_GQ, O_GK, O_GV, O_GR, O_GA = (
    0, 256, 384, 416, 672, 928, 1184, 1200, 1328, 1456, 1712, 1968)

SAME_ENG_SYNC = True
import os
DBG = float(os.environ.get('KDBG', '99'))


class Res:
    __slots__ = ("name", "w", "r", "dsem", "dcnt", "dkey", "excl")

    def __init__(self, name):
        self.name = name
        self.excl = False
        self.w = None
        self.r = {}
        self.dsem = None
        self.dcnt = 0
        self.dkey = None


class T:
    def __init__(self, th, name):
        self.t = th
        self.res = Res(name)
        self.name = name

    def __getitem__(self, idx):
        return self.t[idx]


def _res(x):
    return x.res if isinstance(x, T) else x


class KB:
    def __init__(self, nc, stack):
        self.nc = nc
        self.st = stack
        self.eng = {"pe": nc.tensor, "dve": nc.vector, "act": nc.scalar,
                    "pool": nc.gpsimd, "sp": nc.sync}
        self.semh = {}
        self.cnt = {}
        for k in self.eng:
            self.semh[k] = stack.enter_context(nc.semaphore("s_" + k))
            self.cnt[k] = 0
        self.waited = {k: {} for k in self.eng}
        self.ndsem = 0
        self.n_inst = 0
        self.n_wait = 0
        self.uid = 0
        self.stacks = [stack]
        self.dres = []

    def sb(self, name, shape, dt=F32):
        self.uid += 1
        nm = "%s_%d" % (name, self.uid)
        return T(self.stacks[-1].enter_context(self.nc.sbuf_tensor(nm, list(shape), dt)), nm)

    def ps(self, name, shape, dt=F32):
        self.uid += 1
        nm = "%s_%d" % (name, self.uid)
        t = T(self.stacks[-1].enter_context(self.nc.psum_tensor(nm, list(shape), dt)), nm)
        t.res.excl = True
        return t

    def push(self):
        self.stacks.append(ExitStack())

    def pop(self):
        self.barrier()
        self.stacks.pop().close()

    def barrier(self):
        for e in self.eng:
            deps = {k: self.cnt[k] for k in self.eng if k != e and self.cnt[k] > 0}
            for res in self.dres:
                deps[res.dkey] = res.dcnt
            self._wait(e, deps)

    def _wait(self, e, deps):
        for key, val in deps.items():
            if self.waited[e].get(key, 0) >= val:
                continue
            if key == e and (e == "pe" or not SAME_ENG_SYNC):
                continue
            self.eng[e].wait_ge(self.semh[key], val)
            self.waited[e][key] = val
            self.n_wait += 1

    def _collect(self, reads, writes, dma_write=None):
        deps = {}

        def add(ev):
            if ev is None:
                return
            k, v = ev
            if deps.get(k, 0) < v:
                deps[k] = v

        def cur(res):
            if res.w is None:
                return None
            if res.w[0] == res.dkey:
                return (res.dkey, res.dcnt)
            return res.w

        for r in reads:
            add(cur(r))
        for w in writes:
            if dma_write is not None and w is dma_write and w.w is not None \
                    and w.w[0] == w.dkey and not w.r:
                continue
            add(cur(w))
            for k, v in w.r.items():
                add((k, v))
        return deps

    def op(self, e, fn, r=(), w=()):
        reads = [_res(x) for x in r]
        writes = [_res(x) for x in w]
        ex = [x for x in reads if x.excl and x not in writes]
        if ex:
            reads = [x for x in reads if not x.excl]
            writes = writes + ex
        self._wait(e, self._collect(reads, writes))
        ins = fn(self.eng[e])
        self.cnt[e] += 1
        ins.then_inc(self.semh[e], 1)
        self.n_inst += 1
        ev = (e, self.cnt[e])
        for rr in reads:
            if rr.r.get(e, 0) < ev[1]:
                rr.r[e] = ev[1]
        for ww in writes:
            ww.w = ev
            ww.r = {}
        return ins

    def dma(self, q, out, in_, r=(), w=None, **kw):
        reads = [_res(x) for x in r]
        wres = _res(w)
        if wres.dsem is None:
            wres.dkey = "d%d" % self.ndsem
            self.ndsem += 1
            wres.dsem = self.st.enter_context(self.nc.semaphore(wres.dkey))
            self.semh[wres.dkey] = wres.dsem
            self.dres.append(wres)
        self._wait(q, self._collect(reads, [wres], dma_write=wres))
        ins = self.eng[q].dma_start(out=out, in_=in_, **kw)
        wres.dcnt += 16
        ins.then_inc(wres.dsem, 16)
        self.n_inst += 1
        ev = (wres.dkey, wres.dcnt)
        for rr in reads:
            if rr.r.get(ev[0], 0) < ev[1]:
                rr.r[ev[0]] = ev[1]
        wres.w = ev
        wres.r = {}
        return ins

    def finish(self, outs, e="sp"):
        self._wait(e, self._collect([_res(x) for x in outs], []))

    def mm(self, out, lhsT, rhs, start, stop, r, w):
        return self.op("pe", lambda e: e.matmul(out, lhsT=lhsT, rhs=rhs, start=start, stop=stop), r=r, w=w)

    def tr(self, out, in_, ident, r, w):
        return self.op("pe", lambda e: e.transpose(out=out, in_=in_, identity=ident), r=r, w=w)

    def copy(self, eng, out, in_, r, w):
        if eng == "act":
            return self.op("act", lambda e: e.copy(out=out, in_=in_), r=r, w=w)
        return self.op(eng, lambda e: e.tensor_copy(out=out, in_=in_), r=r, w=w)

    def act(self, out, in_, func, r, w, **kw):
        return self.op("act", lambda e: e.activation(out=out, in_=in_, func=func, **kw), r=r, w=w)

    def tt(self, out, in0, in1, op, r, w, eng="dve"):
        return self.op(eng, lambda e: e.tensor_tensor(out=out, in0=in0, in1=in1, op=op), r=r, w=w)

    def ts(self, out, in0, s1, s2, op0, op1=None, r=(), w=(), eng="dve"):
        if op1 is None:
            return self.op(eng, lambda e: e.tensor_scalar(out=out, in0=in0, scalar1=s1, scalar2=None, op0=op0), r=r, w=w)
        return self.op(eng, lambda e: e.tensor_scalar(out=out, in0=in0, scalar1=s1, scalar2=s2, op0=op0, op1=op1), r=r, w=w)

    def stt(self, out, in0, scalar, in1, op0, op1, r, w, accum_out=None):
        return self.op("dve", lambda e: e.scalar_tensor_tensor(out=out, in0=in0, scalar=scalar, in1=in1, op0=op0, op1=op1, accum_out=accum_out), r=r, w=w)


def rstd_of(kb, ss, n, nfeat, tmp, out, r, w):
    kb.ts(tmp, ss, 1.0 / nfeat, EPS, ALU.mult, ALU.add, r=r, w=w)
    kb.act(tmp, tmp, AF.Sqrt, r=w, w=w)
    kb.op("dve", lambda e: e.reciprocal(out=out, in_=tmp), r=w, w=w)


def rope_tables():
    rows = SEQ // 64
    row = np.broadcast_to(np.arange(rows, dtype=np.float32)[:, None], (rows, 64)).reshape(-1)
    col = np.broadcast_to(np.arange(64, dtype=np.float32)[None, :], (rows, 64)).reshape(-1)
    inv = (np.float32(10000.0) ** (-np.arange(8, dtype=np.float32) / np.float32(8))).astype(np.float32)
    ang = np.concatenate([row[:, None] * inv, col[:, None] * inv], axis=-1).astype(np.float32)
    return np.concatenate([np.cos(ang), np.sin(ang)], axis=-1).astype(np.float32)


def host_consts():
    c = {}
    c["ident"] = np.eye(128, dtype=np.float32)
    j = np.arange(128)
    c["triu"] = (j[:, None] <= j[None, :]).astype(np.float32)
    c["tril"] = (j[:, None] >= j[None, :]).astype(np.float32)
    sel = np.zeros((32, 32, 128), np.float32)
    for e in range(32):
        sel[e, e, :] = 1.0
    c["sel"] = sel.reshape(32, 32 * 128)
    return c


def load_consts(kb, cd, names):
    out = {}
    for nm in names:
        shp = {"ident": [128, 128], "triu": [128, 128], "tril": [128, 128], "sel": [32, 32 * 128]}[nm]
        t = kb.sb("c_" + nm, shp)
        kb.dma("sp", t[:], cd[nm][:], r=[cd[nm]], w=t)
        out[nm] = t
        if nm in ("triu", "tril"):
            tb = kb.sb("c_" + nm + "_b", shp, BF16)
            kb.copy("dve", tb[:], t[:], r=[t], w=[tb])
            out[nm + "_b"] = tb
    return out


def bcast_load(kb, name, ap1d, n, q="sp"):
    t = kb.sb(name, [128, n])
    kb.dma(q, t[:], ap1d.partition_broadcast(128), r=[], w=t)
    return t


def compute_mod(kb, cc_d, w_mod_d, b_mod_d, chunks, pool_ps, pre=None):
    res = {}
    for j in chunks:
        if pre is not None and j in pre:
            res[j] = pre[j]
        else:
            res[j] = (kb.sb("mod_l%d" % j, [128, 1024]), kb.sb("mod_c%d" % j, [128, 1024]))
    kb.push()
    cc = kb.sb("cc", [128, 8, 2])
    kb.dma("sp", cc[:], cc_d, r=[], w=cc)
    sc = kb.sb("sc", [128, 8, 2])
    kb.act(sc[:], cc[:], AF.Silu, r=[cc], w=[sc])
    SC = [kb.sb("SC%d" % i, [128, 8, 128], BF16) for i in range(2)]
    for i in range(2):
        kb.copy("dve", SC[i][:], sc[:, :, i:i + 1].broadcast_to([128, 8, 128]), r=[sc], w=[SC[i]])
    wm = [kb.sb("wm%d" % i, [128, 8, 512], BF16) for i in range(2)]
    bm = [kb.sb("bm%d" % i, [128, 512]) for i in range(2)]
    si = 0
    for j in chunks:
        tl, tc_ = res[j]
        for hf in range(2):
            c0 = j * 1024 + hf * 512
            wmt = wm[si % 2]
            bmt = bm[si % 2]
            si += 1
            kb.dma("pool", wmt[:], w_mod_d[:, c0:c0 + 512].rearrange("(k p) n -> p k n", p=128), r=[], w=wmt)
            kb.dma("sp", bmt[:], b_mod_d[c0:c0 + 512].partition_broadcast(128), r=[], w=bmt)
            for i, dst in enumerate((tl, tc_)):
                ps = pool_ps[i]
                for kc in range(8):
                    kb.mm(ps[:, 0, :], SC[i][:, kc, :], wmt[:, kc, :], kc == 0, kc == 7, r=[SC[i], wmt], w=[ps])
                kb.tt(dst[:, hf * 512:(hf + 1) * 512], ps[:, 0, :], bmt[:], ALU.add, r=[ps, bmt], w=[dst])
    kb.pop()
    return res


def phase_A(kb, io, cst):
    ident = cst["ident"]
    PS = [kb.ps("psA%d" % i, [128, 2, 512]) for i in range(4)]
    mods = compute_mod(kb, io["cc"], io["w_mod"], io["b_mod"], [0, 1], PS)
    n1 = bcast_load(kb, "n1", io["norm1_w"], 1024)
    G1 = []
    S1 = []
    for i in range(2):
        g = kb.sb("G1_%d" % i, [128, 1024])
        kb.stt(g[:], mods[1][i][:], 1.0, n1[:], ALU.add, ALU.mult, r=[mods[1][i], n1], w=[g])
        G1.append(g)
        S1.append(mods[0][i])
    w_in = kb.sb("w_in", [128, 8, D_IN], BF16)
    for kc in range(8):
        kb.dma("pool", w_in[:, kc, :], io["w_in"][kc * 128:(kc + 1) * 128, :], r=[], w=w_in)
    w_uq = kb.sb("w_uq", [128, 2, 768], BF16)
    kb.dma("pool", w_uq[:], io["w_uq"].rearrange("(k p) n -> p k n", p=128), r=[], w=w_uq)
    w_ukv = kb.sb("w_ukv", [128, 1024], BF16)
    kb.dma("pool", w_ukv[:], io["w_ukv"], r=[], w=w_ukv)
    qan = bcast_load(kb, "qan", io["q_a_norm"], 256)
    kvan = bcast_load(kb, "kvan", io["kv_a_norm"], 128)
    qnw1 = bcast_load(kb, "qnw1", io["q_norm_w"], 96)
    knw1 = bcast_load(kb, "knw1", io["k_norm_w"], 96)
    qnw = kb.sb("qnw", [128, 8, 96])
    knw = kb.sb("knw", [128, 8, 96])
    kb.ts(qnw[:], qnw1[:].unsqueeze(1).broadcast_to([128, 8, 96]), float(96 ** -0.5), None, ALU.mult, r=[qnw1], w=[qnw])
    kb.copy("dve", knw[:], knw1[:].unsqueeze(1).broadcast_to([128, 8, 96]), r=[knw1], w=[knw])
    rope = kb.sb("rope", [128, 16, 32])
    kb.dma("sp", rope[:], io["rope"].rearrange("(n p) c -> p n c", p=128), r=[], w=rope)

    if DBG < 1:
        return
    TM_SLABS = [[(O_CQ, 416), (O_MG, 16)], [(O_MV, 512)], [(O_GV, 512)]]
    FM_GROUPS = [(O_MX, 128), (O_MX + 128, 128), (O_GQ, 128), (O_GK, 128), (O_GA, 32)]
    NB_ = 2
    xt = [kb.sb("xt%d" % i, [128, 1024]) for i in range(NB_)]
    junk = [kb.sb("junk%d" % i, [128, 1024]) for i in range(NB_)]
    xm = [kb.sb("xm%d" % i, [128, 1024]) for i in range(NB_)]
    xmT = [kb.sb("xmT%d" % i, [128, 8, 128], BF16) for i in range(NB_)]
    htm = [kb.sb("htm%d" % i, [128, 1456]) for i in range(NB_)]
    hfm = [kb.sb("hfm%d" % i, [128, 5, 128]) for i in range(NB_)]
    st1 = [kb.sb("st1_%d" % i, [128, 32]) for i in range(NB_)]
    cqn = [kb.sb("cqn%d" % i, [128, 384]) for i in range(NB_)]
    cT = [kb.sb("cT%d" % i, [128, 3, 128], BF16) for i in range(NB_)]
    qf = [kb.sb("qf%d" % i, [128, 8, 96]) for i in range(NB_)]
    kf = [kb.sb("kf%d" % i, [128, 8, 96]) for i in range(NB_)]
    sq = [kb.sb("sq%d" % i, [128, 8, 96]) for i in range(NB_)]
    rt = [kb.sb("rt%d" % i, [128, 4, 8, 16]) for i in range(NB_)]
    qTs = [kb.sb("qTs%d" % i, [96, 8, 128], BF16) for i in range(NB_)]
    kTs = [kb.sb("kTs%d" % i, [96, 8, 128], BF16) for i in range(NB_)]
    vs = [kb.sb("vs%d" % i, [128, 8, 64], BF16) for i in range(NB_)]

    def head_norm_rope(src, dst_f, sqt, stt_, wbc, ropeidx, rtt, r_extra):
        kb.act(sqt[:], src, AF.Square, r=r_extra, w=[sqt])
        kb.op("dve", lambda e: e.tensor_reduce(out=stt_[:, 0:8], in_=sqt[:], axis=AX.X, op=ALU.add), r=[sqt], w=[stt_])
        rstd_of(kb, stt_[:, 0:8], 8, 96, stt_[:, 8:16], stt_[:, 16:24], r=[stt_], w=[stt_])
        kb.tt(dst_f[:], src, stt_[:, 16:24].unsqueeze(2).broadcast_to([128, 8, 96]), ALU.mult, r=r_extra + [stt_], w=[dst_f])
        kb.tt(dst_f[:], dst_f[:], wbc[:], ALU.mult, r=[dst_f, wbc], w=[dst_f])
        if ropeidx is not None:
            cos = rope[:, ropeidx, 0:16].unsqueeze(1).broadcast_to([128, 8, 16])
            sin = rope[:, ropeidx, 16:32].unsqueeze(1).broadcast_to([128, 8, 16])
            x1 = dst_f[:, :, 64:80]
            x2 = dst_f[:, :, 80:96]
            kb.tt(rtt[:, 0], x1, cos, ALU.mult, r=[dst_f, rope], w=[rtt])
            kb.tt(rtt[:, 1], x2, sin, ALU.mult, r=[dst_f, rope], w=[rtt])
            kb.tt(rtt[:, 2], x1, sin, ALU.mult, r=[dst_f, rope], w=[rtt])
            kb.tt(rtt[:, 3], x2, cos, ALU.mult, r=[dst_f, rope], w=[rtt])
            kb.tt(x1, rtt[:, 0], rtt[:, 1], ALU.subtract, r=[rtt], w=[dst_f])
            kb.tt(x2, rtt[:, 2], rtt[:, 3], ALU.add, r=[rtt], w=[dst_f])

    for ti in range(NT):
        b_ = ti % NB_
        is_ctx = ti >= 16
        mi = 1 if is_ctx else 0
        t0 = ti * 128
        X, XM, XMT, H, HF, ST = xt[b_], xm[b_], xmT[b_], htm[b_], hfm[b_], st1[b_]
        kb.dma("sp", X[:], io["x"][t0:t0 + 128, :], r=[io["x_res"]], w=X)
        kb.act(junk[b_][:], X[:], AF.Square, r=[X], w=[junk[b_], ST], accum_out=ST[:, 0:1])
        rstd_of(kb, ST[:, 0:1], 1, 1024, ST[:, 1:2], ST[:, 2:3], r=[ST], w=[ST])
        kb.stt(XM[:], X[:], ST[:, 2:3], G1[mi][:], ALU.mult, ALU.mult, r=[X, ST, G1[mi]], w=[XM])
        kb.tt(XM[:], XM[:], S1[mi][:], ALU.add, r=[XM, S1[mi]], w=[XM], eng="pool")
        if DBG < 2:
            continue
        for kc in range(8):
            kb.tr(PS[0][:, kc // 4, (kc % 4) * 128:(kc % 4 + 1) * 128], XM[:, kc * 128:(kc + 1) * 128], ident[:], r=[XM, ident], w=[PS[0]])
        kb.copy("act", XMT[:].rearrange("p (a b) t -> p a (b t)", a=2), PS[0][:], r=[PS[0]], w=[XMT])
        pcol = 0
        for si, slab in enumerate(TM_SLABS):
            pst = PS[1] if si < 2 else PS[2]
            bank = si if si < 2 else 0
            off = 0
            for (c0, wd) in slab:
                for kc in range(8):
                    kb.mm(pst[:, bank, off:off + wd], XMT[:, kc, :], w_in[:, kc, c0:c0 + wd], kc == 0, kc == 7, r=[XMT, w_in], w=[pst])
                off += wd
            kb.copy("act" if si != 1 else "dve", H[:, pcol:pcol + off], pst[:, bank, 0:off], r=[pst], w=[H])
            pcol += off
        for gi, (c0, wd) in enumerate(FM_GROUPS):
            for kc in range(8):
                kb.mm(PS[3][0:wd, gi // 4, (gi % 4) * 128:(gi % 4 + 1) * 128], w_in[:, kc, c0:c0 + wd], XMT[:, kc, :], kc == 0, kc == 7, r=[XMT, w_in], w=[PS[3]])
        kb.copy("dve", HF[:, 0:4, :].rearrange("p a t -> p (a t)"), PS[3][:, 0, :], r=[PS[3]], w=[HF])
        kb.copy("dve", HF[0:32, 4, :], PS[3][0:32, 1, 0:128], r=[PS[3]], w=[HF])
        if DBG < 3:
            continue
        kb.dma("sp", io["fm"][0:512, t0:t0 + 128].rearrange("(a p) t -> p a t", p=128), HF[:, 0:4, :], r=[HF], w=io["fm_res"])
        kb.dma("sp", io["fm"][512:544, t0:t0 + 128], HF[0:32, 4, :], r=[HF], w=io["fm_res"])
        kb.dma("sp", io["tm"][t0:t0 + 128, :], H[:, 416:1456], r=[H], w=io["tm_res"])
        if DBG < 4:
            continue
        CQ = cqn[b_]
        kb.act(junk[b_][:, 0:256], H[:, 0:256], AF.Square, r=[H], w=[junk[b_], ST], accum_out=ST[:, 4:5])
        kb.act(junk[b_][:, 256:384], H[:, 256:384], AF.Square, r=[H], w=[junk[b_], ST], accum_out=ST[:, 5:6])
        rstd_of(kb, ST[:, 4:5], 1, 256, ST[:, 6:7], ST[:, 8:9], r=[ST], w=[ST])
        rstd_of(kb, ST[:, 5:6], 1, 128, ST[:, 7:8], ST[:, 9:10], r=[ST], w=[ST])
        kb.stt(CQ[:, 0:256], H[:, 0:256], ST[:, 8:9], qan[:], ALU.mult, ALU.mult, r=[H, ST, qan], w=[CQ])
        kb.stt(CQ[:, 256:384], H[:, 256:384], ST[:, 9:10], kvan[:], ALU.mult, ALU.mult, r=[H, ST, kvan], w=[CQ])
        for kc in range(3):
            kb.tr(PS[2][:, 1, kc * 128:(kc + 1) * 128], CQ[:, kc * 128:(kc + 1) * 128], ident[:], r=[CQ, ident], w=[PS[2]])
        kb.copy("act", cT[b_][:].rearrange("p a t -> p (a t)"), PS[2][:, 1, 0:384], r=[PS[2]], w=[cT[b_]])
        if DBG < 4.1:
            continue
        for s in range(2):
            for kc in range(2):
                kb.mm(PS[0][:, s, 0:384], cT[b_][:, kc, :], w_uq[:, kc, s * 384:(s + 1) * 384], kc == 0, kc == 1, r=[cT[b_], w_uq], w=[PS[0]])
        QF, KF = qf[b_], kf[b_]
        kb.copy("act", QF[:].rearrange("p (s a) d -> p s (a d)", s=2), PS[0][:, :, 0:384], r=[PS[0]], w=[QF])
        if DBG < 4.2:
            continue
        for s in range(2):
            kb.mm(PS[1][:, s, :], cT[b_][:, 2, :], w_ukv[:, s * 512:(s + 1) * 512], True, True, r=[cT[b_], w_ukv], w=[PS[1]])
        kvv = PS[1][:].rearrange("p s (a d) -> p (s a) d", d=128)
        if DBG < 4.21:
            continue
        kb.copy("dve", KF[:, :, 0:64], kvv[:, :, 0:64], r=[PS[1]], w=[KF])
        if DBG < 4.22:
            continue
        for s in range(2):
            kb.copy("dve", vs[b_][:, 4 * s:4 * s + 4, :], PS[1][:, s, :].rearrange("p (a d) -> p a d", d=128)[:, :, 64:128], r=[PS[1]], w=[vs[b_]])
        if DBG < 4.23:
            continue
        kb.copy("dve", KF[:, :, 64:96], H[:, 384:416].unsqueeze(1).broadcast_to([128, 8, 32]), r=[H], w=[KF])
        if DBG < 4.3:
            continue
        ridx = None if is_ctx else ti
        if DBG < 4.4:
            ridx = None
        head_norm_rope(QF[:], QF, sq[b_], ST, qnw, ridx, rt[b_], [QF])
        head_norm_rope(KF[:], KF, sq[b_], ST, knw, ridx, rt[b_], [KF])
        if DBG < 5:
            continue
        for (SRC, DST, dname) in ((QF, qTs[b_], "qT"), (KF, kTs[b_], "kT")):
            for h in range(8):
                kb.tr(PS[3][0:96, h // 4, (h % 4) * 128:(h % 4 + 1) * 128], SRC[:, h, :], ident[:], r=[SRC, ident], w=[PS[3]])
            kb.copy("act", DST[:].rearrange("p (a b) t -> p a (b t)", a=2), PS[3][0:96, :, :], r=[PS[3]], w=[DST])
            kb.dma("sp", io[dname][:, :, t0:t0 + 128].rearrange("h d t -> d h t"), DST[:], r=[DST], w=io[dname + "_res"])
        kb.dma("sp", io["v"][t0:t0 + 128, :], vs[b_][:].rearrange("p h d -> p (h d)"), r=[vs[b_]], w=io["v_res"])


class IO(dict):
    pass


def declare(nc, io, name, shape, dt, kind):
    th = nc.dram_tensor(name, list(shape), dt, kind=kind)
    io[name] = th[:] if len(shape) > 0 else th
    io[name + "_res"] = Res(name)
    return th


CONST_SHAPES = {"ident": [128, 128], "triu": [128, 128], "tril": [128, 128], "sel": [32, 32 * 128]}

A_INPUTS = [("x", [NTOK, D]), ("cc", [128, 8, 2]), ("w_mod", [D, 6 * D]), ("b_mod", [6 * D]), ("norm1_w", [D]),
            ("w_in", [D, D_IN]), ("q_a_norm", [256]), ("w_uq", [256, 768]), ("kv_a_norm", [128]),
            ("w_ukv", [128, 1024]), ("q_norm_w", [96]), ("k_norm_w", [96]), ("rope", [TL, 32])]
A_OUTPUTS = [("qT", [8, 96, NTOK], BF16), ("kT", [8, 96, NTOK], BF16), ("v", [NTOK, 512], BF16),
             ("fm", [544, NTOK], F32), ("tm", [NTOK, 1040], F32)]


def build_A():
    nc = bass.Bass("TRN2", target_bir_lowering=False)
    io = IO()
    for nm, shp in A_INPUTS:
        declare(nc, io, nm, shp, F32, "ExternalInput")
    cd = {}
    for nm in ("ident",):
        th = nc.dram_tensor("c_" + nm, CONST_SHAPES[nm], F32, kind="ExternalInput")
        cd[nm] = T(th, "c_" + nm)
    for nm, shp, dt in A_OUTPUTS:
        declare(nc, io, nm, shp, dt, "ExternalOutput")
    with ExitStack() as st:
        kb = KB(nc, st)
        cst = load_consts(kb, cd, ["ident"])
        phase_A(kb, io, cst)
        kb.finish([io[nm + "_res"] for nm, _, _ in A_OUTPUTS])
        print("phase A: n_inst", kb.n_inst, "n_wait", kb.n_wait, "dsems", kb.ndsem)
    return nc


_ROPE = None
_CONSTS = None


def consts():
    global _ROPE, _CONSTS
    if _CONSTS is None:
        _CONSTS = host_consts()
        _ROPE = rope_tables()
    return _CONSTS, _ROPE


def f32(a):
    return np.ascontiguousarray(a, dtype=np.float32)


def cc_layout(c_b, c_ctx):
    cc = np.stack([c_b, c_ctx], axis=-1)
    return f32(cc.reshape(8, 128, 2).transpose(1, 0, 2))


def core_tokens(xl, xc, core):
    b, j = core // 4, core % 4
    return np.concatenate([xl[b, j * TL:(j + 1) * TL], xc[b]], axis=0)


def a_inmaps(inp, l, xl, xc):
    cst, rope = consts()
    maps = []
    for core in range(NCORE):
        b, j = core // 4, core % 4
        m = {"x": f32(core_tokens(xl, xc, core)), "cc": cc_layout(inp["c"][b], inp["c_ctx"]),
             "rope": f32(rope[j * TL:(j + 1) * TL]), "c_ident": cst["ident"]}
        for nm in ("w_mod", "b_mod", "norm1_w", "w_in", "q_a_norm", "w_uq", "kv_a_norm", "w_ukv", "q_norm_w", "k_norm_w"):
            m[nm] = f32(inp[nm][l])
        maps.append(m)
    return maps


B_T = SEQ + NCTX
NCH = B_T // 128
SEQ_F = [64, 65] + list(range(64))
SEQ_B = list(range(65, -1, -1))


def nsl(ns):
    if len(ns) == 1:
        return slice(ns[0], ns[0] + 1)
    if ns[1] == ns[0] + 1:
        return slice(ns[0], ns[-1] + 1)
    stop = ns[-1] - 1
    return slice(ns[0], stop if stop >= 0 else None, -1)


def seq_groups(seq, g):
    groups, cur = [], []
    for n in seq:
        if cur and (len(cur) == g or abs(n - cur[-1]) != 1 or (len(cur) >= 2 and (n - cur[-1]) != (cur[-1] - cur[-2]))):
            groups.append(cur)
            cur = []
        cur.append(n)
    groups.append(cur)
    return groups


def attention_B(kb, io, need_ctx):
    kb.push()
    KT = kb.sb("KT", [96, 2, B_T], BF16)
    QT = kb.sb("QT", [96, 2, SEQ], BF16)
    QC = kb.sb("QC", [96, 2, 256], BF16)
    Vst = kb.sb("Vst", [128, NCH, 128], BF16)
    V = kb.sb("V", [128, NCH, 2, 66], BF16)
    E = kb.sb("E", [65, 64], BF16)
    for hh in range(2):
        kb.dma("sp", KT[:, hh, :], io["KT"][hh], r=[], w=KT)
        kb.dma("sp", QT[:, hh, :], io["QT"][hh], r=[], w=QT)
        kb.dma("sp", QC[:, hh, :], io["QcT"][hh], r=[], w=QC)
    vsrc = io["V"]
    for i in range(0, NCH, 11):
        kb.dma("sp", Vst[:, i:i + 11, :], vsrc[:, i:i + 11, :], r=[], w=Vst)
    cf = kb.sb("cf", [128, 512])
    kb.op("dve", lambda e: e.memset(cf[:], 1.0), r=[], w=[cf])
    kb.copy("dve", V[:, :, :, 64:65], cf[:, 0:NCH * 2].rearrange("p (n h o) -> p n h o", h=2, o=1), r=[cf], w=[V])
    kb.copy("dve", V[:, :, :, 0:64], Vst[:].rearrange("p n (h d) -> p n h d", h=2), r=[Vst], w=[V])
    Ef = kb.sb("Ef", [65, 64])
    kb.op("dve", lambda e: e.memset(Ef[:], 0.0), r=[], w=[Ef])
    kb.op("dve", lambda e: e.memset(Ef[64:65, :], 1.0), r=[], w=[Ef])
    kb.copy("dve", E[:], Ef[:], r=[Ef], w=[E])
    zf = kb.sb("zf", [65, 512])
    kb.op("dve", lambda e: e.memset(zf[:], 0.0), r=[], w=[zf])
    pst = [kb.ps("pst%d" % i, [128, 512]) for i in range(2)]
    pot = [kb.ps("pot%d" % i, [128, 512]) for i in range(2)]
    pbc = kb.ps("pbc", [128, 512])
    PT = [kb.sb("PT%d" % i, [128, 512], BF16) for i in range(3)]
    osb = [kb.sb("osb%d" % i, [65, 512]) for i in range(2)]
    rd = [kb.sb("rd%d" % i, [65, 512]) for i in range(2)]
    rh = [kb.sb("rh%d" % i, [65, 512], BF16) for i in range(2)]
    rl = [kb.sb("rl%d" % i, [65, 512], BF16) for i in range(2)]
    for t in rh + rl:
        kb.copy("dve", t[:], zf[:], r=[zf], w=[t])
    oT = [kb.sb("oT%d" % i, [64, 512], BF16) for i in range(2)]
    for t in rd:
        kb.op("dve", lambda e: e.memset(t[:], 0.0), r=[], w=[t])
    cnt = {"it": 0, "blk": 0}

    def block(q_ap, qres, nq, key_tiles, hh, out_ap):
        po = pot[cnt["blk"] % 2]
        ob = osb[cnt["blk"] % 2]
        rdt = rd[cnt["blk"] % 2]
        ot = oT[cnt["blk"] % 2]
        cnt["blk"] += 1
        nk = len(key_tiles)
        for i, kt in enumerate(key_tiles):
            ps_ = pst[cnt["it"] % 2]
            pt = PT[cnt["it"] % 3]
            cnt["it"] += 1
            kb.mm(ps_[:, 0:nq], KT[:, hh, kt * 128:(kt + 1) * 128], q_ap, True, True, r=[KT, qres], w=[ps_])
            kb.act(pt[:, 0:nq], ps_[:, 0:nq], AF.Exp, r=[ps_], w=[pt])
            kb.mm(po[0:65, 0:nq], V[:, kt, hh, 0:65], pt[:, 0:nq], i == 0, i == nk - 1, r=[V, pt], w=[po])
        kb.copy("act", ob[:, 0:nq], po[0:65, 0:nq], r=[po], w=[ob])
        kb.op("dve", lambda e: e.reciprocal(out=rdt[64:65, 0:nq], in_=ob[64:65, 0:nq]), r=[ob], w=[rdt])
        rht = rh[(cnt["blk"] - 1) % 2]
        rlt = rl[(cnt["blk"] - 1) % 2]
        kb.copy("dve", rht[64:65, 0:nq], rdt[64:65, 0:nq], r=[rdt], w=[rht])
        kb.tt(rlt[64:65, 0:nq], rdt[64:65, 0:nq], rht[64:65, 0:nq], ALU.subtract, r=[rdt, rht], w=[rlt])
        kb.mm(pbc[0:64, 0:nq], E[:, :], rht[:, 0:nq], True, False, r=[E, rht], w=[pbc])
        kb.mm(pbc[0:64, 0:nq], E[:, :], rlt[:, 0:nq], False, True, r=[E, rlt], w=[pbc])
        kb.tt(ot[:, 0:nq], pbc[0:64, 0:nq], ob[0:64, 0:nq], ALU.mult, r=[ob, pbc], w=[ot])
        kb.dma("sp", out_ap, ot[:, 0:nq], r=[ot], w=io["mixT_res"])

    for hh in range(2):
        for qb in range(SEQ // 512):
            block(QT[:, hh, qb * 512:(qb + 1) * 512], QT, 512, list(range(NCH)), hh,
                  io["mixT"][hh * 64:(hh + 1) * 64, qb * 512:(qb + 1) * 512])
        if need_ctx:
            block(QC[:, hh, :], QC, 256, [64, 65], hh, io["mixT"][hh * 64:(hh + 1) * 64, SEQ:B_T])
    kb.pop()


def lin_attn(kb, cst, dk, nv, qT, kT, ktok, vp, a, a_on_G, dirn, emit):
    kb.push()
    seq = SEQ_F if dirn == 0 else SEQ_B
    mask = cst["triu"] if dirn == 0 else cst["tril"]
    Gs = kb.sb("Gs", [dk, nv, NCH])
    Ar = kb.sb("Ar", [dk, nv, NCH])
    Cs = kb.sb("Cs", [dk, nv, NCH], BF16)
    pg = [kb.ps("pg%d" % i, [128, 512]) for i in range(2)]
    pp = [kb.ps("pp%d" % i, [128, 512]) for i in range(2)]
    po = [kb.ps("po%d" % i, [128, 512]) for i in range(2)]
    PTm = [kb.sb("PTm%d" % i, [128, 128], BF16) for i in range(3)]
    gsz = 512 // nv
    for gi, s0 in enumerate(range(0, NCH, gsz)):
        pgt = pg[gi % 2]
        ss = list(range(s0, min(NCH, s0 + gsz)))
        for i, s in enumerate(ss):
            n = seq[s]
            kb.mm(pgt[0:dk, i * nv:(i + 1) * nv], ktok[:, n, :], vp[:, n, :], True, True, r=[ktok, vp], w=[pgt])
        if DBG < 10.6:
            continue
        for i, s in enumerate(ss):
            n = seq[s]
            if a_on_G:
                kb.tt(Gs[:, :, s], pgt[0:dk, i * nv:(i + 1) * nv], a[:, n:n + 1].broadcast_to([dk, nv]), ALU.mult, r=[pgt, a], w=[Gs])
            else:
                kb.copy("dve", Gs[:, :, s], pgt[0:dk, i * nv:(i + 1) * nv], r=[pgt], w=[Gs])
    if DBG < 10.61:
        kb.pop()
        return
    if dirn == 0:
        kb.copy("pool", Ar[:, :, 2:NCH], a[:, 0:64].unsqueeze(1).broadcast_to([dk, nv, 64]), r=[a], w=[Ar])
        kb.copy("pool", Ar[:, :, 0:2], a[:, 64:66].unsqueeze(1).broadcast_to([dk, nv, 2]), r=[a], w=[Ar])
    else:
        kb.copy("pool", Ar[:], a[:, ::-1].unsqueeze(1).broadcast_to([dk, nv, NCH]), r=[a], w=[Ar])
    kb.op("pool", lambda e: e.memset(Ar[:, :, 0:1], 0.0), r=[], w=[Ar])
    kb.op("dve", lambda e: e.tensor_tensor_scan(out=Cs[:].rearrange("p v s -> p (v s)"), data0=Ar[:].rearrange("p v s -> p (v s)"),
                                                 data1=Gs[:].rearrange("p v s -> p (v s)"), initial=0.0, op0=ALU.mult, op1=ALU.add),
          r=[Ar, Gs], w=[Cs])
    if DBG < 10.62:
        kb.pop()
        return
    it = 0
    for gi, ns in enumerate(seq_groups(seq, gsz)):
        pot = po[gi % 2]
        for i, n in enumerate(ns):
            s = seq.index(n)
            t0 = n * 128
            ppt = pp[it % 2]
            ptm = PTm[it % 3]
            it += 1
            kb.mm(ppt[:, 0:128], kT[:, t0:t0 + 128], qT[:, t0:t0 + 128], True, True, r=[kT, qT], w=[ppt])
            kb.tt(ptm[:], ppt[:, 0:128], mask[:], ALU.mult, r=[ppt, mask], w=[ptm])
            kb.mm(pot[:, i * nv:(i + 1) * nv], ptm[:], vp[:, n, :], True, s == 0, r=[ptm, vp], w=[pot])
            if s > 0:
                kb.mm(pot[:, i * nv:(i + 1) * nv], qT[:, t0:t0 + 128], Cs[:, :, s - 1], False, True, r=[qT, Cs], w=[pot])
        emit(pot, ns)
    kb.pop()


def out_norm_gate(kb, cst, hsum, nw_d, gate_d, gate_func, extra, mix_rows, io, name):
    kb.push()
    nw = bcast_load(kb, name + "nw", nw_d, 64)
    gt = kb.sb(name + "gt", [128, NCH, 64])
    kb.dma("sp", gt[:], gate_d, r=[], w=gt)
    sq = kb.sb(name + "sq", [128, NCH, 64])
    st = kb.sb(name + "st", [128, 3, NCH])
    kb.act(sq[:], hsum[:], AF.Square, r=[hsum], w=[sq])
    kb.op("dve", lambda e: e.tensor_reduce(out=st[:, 0, :], in_=sq[:], axis=AX.X, op=ALU.add), r=[sq], w=[st])
    rstd_of(kb, st[:, 0, :], NCH, 64, st[:, 1, :], st[:, 2, :], r=[st], w=[st])
    if DBG < 10.71:
        kb.pop()
        return
    kb.tt(hsum[:], hsum[:], st[:, 2, :].unsqueeze(2).broadcast_to([128, NCH, 64]), ALU.mult, r=[hsum, st], w=[hsum])
    kb.tt(hsum[:], hsum[:], nw[:].unsqueeze(1).broadcast_to([128, NCH, 64]), ALU.mult, r=[hsum, nw], w=[hsum])
    if extra is not None:
        kb.tt(hsum[:], hsum[:], extra[:], ALU.add, r=[hsum, extra], w=[hsum])
    kb.act(gt[:], gt[:], gate_func, r=[gt], w=[gt])
    kb.tt(hsum[:], hsum[:], gt[:], ALU.mult, r=[hsum, gt], w=[hsum])
    if DBG < 10.72:
        kb.pop()
        return
    oT = kb.sb(name + "oT", [64, B_T], BF16)
    ptr = [kb.ps(name + "ptr%d" % i, [128, 512]) for i in range(2)]
    for gi, n0 in enumerate(range(0, NCH, 4)):
        nn = min(4, NCH - n0)
        p = ptr[gi % 2]
        for i in range(nn):
            kb.tr(p[0:64, i * 128:(i + 1) * 128], hsum[:, n0 + i, :], cst["ident"][:], r=[hsum, cst["ident"]], w=[p])
        kb.copy("act", oT[:, n0 * 128:(n0 + nn) * 128], p[0:64, 0:nn * 128], r=[p], w=[oT])
    if DBG < 10.73:
        kb.pop()
        return
    for i in range(4):
        kb.dma("sp", io["mixT"][mix_rows:mix_rows + 64, i * 2112:(i + 1) * 2112], oT[:, i * 2112:(i + 1) * 2112], r=[oT], w=io["mixT_res"])
    kb.pop()


def mlstm_B(kb, io, cst):
    kb.push()
    ident = cst["ident"]
    qT = kb.sb("mqT", [64, B_T], BF16)
    kT = kb.sb("mkT", [64, B_T], BF16)
    ktok = kb.sb("mktok", [128, NCH, 64], BF16)
    xtok = kb.sb("mxtok", [128, NCH, 64])
    kb.push()
    mx = kb.sb("mx", [64, B_T])
    acc = kb.sb("macc", [64, B_T])
    xcb = kb.sb("mxcb", [64, B_T], BF16)
    cw = kb.sb("cw", [64, 5])
    cb = kb.sb("cb", [64, 1])
    wq = kb.sb("wq", [64, 64], BF16)
    wk = kb.sb("wk", [64, 64], BF16)
    kb.dma("sp", cw[:], io["cw"], r=[], w=cw)
    kb.dma("sp", cb[:], io["cb"], r=[], w=cb)
    kb.dma("pool", wq[:], io["wq"], r=[], w=wq)
    kb.dma("pool", wk[:], io["wk"], r=[], w=wk)
    for i in range(4):
        kb.dma("sp", mx[:, i * 2112:(i + 1) * 2112], io["mxT"][:, i * 2112:(i + 1) * 2112], r=[], w=mx)
    for (s0, ln) in ((0, SEQ), (SEQ, NCTX)):
        kb.ts(acc[:, s0:s0 + ln], mx[:, s0:s0 + ln], cw[:, 2:3], None, ALU.mult, r=[mx, cw], w=[acc])
        for k in (0, 1, 3, 4):
            sh = k - 2
            a0 = max(0, -sh)
            a1 = ln - max(0, sh)
            kb.stt(acc[:, s0 + a0:s0 + a1], mx[:, s0 + a0 + sh:s0 + a1 + sh], cw[:, k:k + 1], acc[:, s0 + a0:s0 + a1],
                   ALU.mult, ALU.add, r=[mx, cw, acc], w=[acc])
    if DBG < 10.1:
        return
    kb.act(acc[:], acc[:], AF.Silu, r=[acc, cb], w=[acc], bias=cb[:, 0:1])
    kb.copy("dve", xcb[:], acc[:], r=[acc], w=[xcb])
    if DBG < 10.2:
        return
    pj = [kb.ps("mpj%d" % i, [128, 512]) for i in range(2)]
    gi = 0
    for c0 in range(0, B_T, 512):
        w_ = min(512, B_T - c0)
        p = pj[gi % 2]; gi += 1
        kb.mm(p[0:64, 0:w_], wq[:], xcb[:, c0:c0 + w_], True, True, r=[wq, xcb], w=[p])
        kb.op("act", lambda e: e.mul(qT[:, c0:c0 + w_], p[0:64, 0:w_], 0.125), r=[p], w=[qT])
        p = pj[gi % 2]; gi += 1
        kb.mm(p[0:64, 0:w_], wk[:], xcb[:, c0:c0 + w_], True, True, r=[wk, xcb], w=[p])
        kb.copy("dve", kT[:, c0:c0 + w_], p[0:64, 0:w_], r=[p], w=[kT])
    if DBG < 10.3:
        return
    for n0 in range(0, NCH, 8):
        nn = min(8, NCH - n0)
        p = pj[gi % 2]; gi += 1
        for i in range(nn):
            n = n0 + i
            kb.mm(p[:, i * 64:(i + 1) * 64], xcb[:, n * 128:(n + 1) * 128], wk[:], True, True, r=[xcb, wk], w=[p])
        kb.copy("dve", ktok[:, n0:n0 + nn, :].rearrange("p n d -> p (n d)"), p[:, 0:nn * 64], r=[p], w=[ktok])
        p = pj[gi % 2]; gi += 1
        for i in range(nn):
            n = n0 + i
            kb.tr(p[:, i * 64:(i + 1) * 64], acc[:, n * 128:(n + 1) * 128], ident[0:64, 0:64], r=[acc, ident], w=[p])
        kb.copy("act", xtok[:, n0:n0 + nn, :].rearrange("p n d -> p (n d)"), p[:, 0:nn * 64], r=[p], w=[xtok])
    kb.pop()
    if DBG < 10.4:
        return
    mg = kb.sb("mg", [128, NCH, 4])
    kb.dma("sp", mg[:], io["mg"], r=[], w=mg)
    gb = bcast_load(kb, "gb", io["gb"], 4)
    gp = kb.sb("gp", [128, 4, NCH])
    for g in range(4):
        kb.ts(gp[:, g, :], mg[:, :, g], gb[:, g:g + 1], None, ALU.add, r=[mg, gb], w=[gp])
    if DBG < 10.41:
        return
    lf = kb.sb("lf", [128, 2, NCH])
    for d in range(2):
        kb.act(lf[:, d, :], gp[:, 1 + 2 * d, :], AF.Sigmoid, r=[gp], w=[lf])
    kb.act(lf[:], lf[:], AF.Ln, r=[lf], w=[lf])
    if DBG < 10.42:
        return
    ones = kb.sb("ones", [128, 64], BF16)
    onesf = kb.sb("onesf", [128, 64])
    kb.op("dve", lambda e: e.memset(onesf[:], 1.0), r=[], w=[onesf])
    kb.copy("dve", ones[:], onesf[:], r=[onesf], w=[ones])
    lfh = kb.sb("lfh", [128, 2, NCH], BF16)
    lfl = kb.sb("lfl", [128, 2, NCH], BF16)
    kb.copy("dve", lfh[:], lf[:], r=[lf], w=[lfh])
    if DBG < 10.421:
        return
    kb.tt(lfl[:], lf[:], lfh[:], ALU.subtract, r=[lf, lfh], w=[lfl])
    if DBG < 10.422:
        return
    pgt = kb.ps("mpgate", [128, 4, 128])
    rr = kb.sb("mr", [128, 2, NCH])
    uu = kb.sb("mu", [128, 2, NCH])
    aa = [kb.sb("ma%d" % d, [64, NCH]) for d in range(2)]
    for d in range(2):
        tri = cst["triu_b"] if d == 0 else cst["tril_b"]
        kb.mm(pgt[:, d, 0:NCH], tri[:], lfh[:, d, :], True, False, r=[tri, lfh], w=[pgt])
        kb.mm(pgt[:, d, 0:NCH], tri[:], lfl[:, d, :], False, True, r=[tri, lfl], w=[pgt])
        if DBG < 10.423:
            continue
        kb.mm(pgt[0:64, 2 + d, 0:NCH], ones[:], lfh[:, d, :], True, False, r=[ones, lfh], w=[pgt])
        kb.mm(pgt[0:64, 2 + d, 0:NCH], ones[:], lfl[:, d, :], False, True, r=[ones, lfl], w=[pgt])
    if DBG < 10.43:
        return
    for d in range(2):
        kb.act(rr[:, d, :], pgt[:, d, 0:NCH], AF.Exp, r=[pgt], w=[rr])
        if DBG < 10.432:
            continue
        kb.stt(uu[:, d, :], pgt[:, d, 0:NCH], -1.0, gp[:, 2 * d, :], ALU.mult, ALU.add, r=[gp, pgt], w=[uu])
        if DBG < 10.433:
            continue
        kb.act(aa[d][:], pgt[0:64, 2 + d, 0:NCH], AF.Exp, r=[pgt], w=[aa[d]])
    if DBG < 10.44:
        return
    kb.act(uu[:], uu[:], AF.Exp, r=[uu], w=[uu])
    if DBG < 10.5:
        return
    vaug = kb.sb("vaug", [128, NCH, 66])
    kb.op("dve", lambda e: e.memset(vaug[:], 1.0), r=[], w=[vaug])
    if DBG < 10.501:
        return
    hsum = kb.sb("hsum", [128, NCH, 64])
    kb.dma("sp", hsum[:], io["mv"], r=[], w=hsum)
    if DBG < 10.502:
        return
    kb.copy("dve", vaug[:, :, 0:64], hsum[:], r=[hsum], w=[vaug])
    if DBG < 10.51:
        return
    vp = kb.sb("vp", [128, NCH, 66], BF16)
    vtmp = kb.sb("vtmp", [128, NCH, 66])
    dt_ = kb.sb("mdt", [128, 4, 8])
    htmp = kb.sb("htmp", [128, 7, 64])
    for d in range(2):
        kb.tt(vtmp[:], vaug[:], uu[:, d, :].unsqueeze(2).broadcast_to([128, NCH, 66]), ALU.mult, r=[vaug, uu], w=[vtmp])
        kb.copy("dve", vp[:], vtmp[:], r=[vtmp], w=[vp])

        def emit(pot, ns, d=d):
            g = len(ns)
            sl = nsl(ns)
            pv = pot[:, 0:g * 66].rearrange("p (g v) -> p g v", v=66)
            kb.tt(dt_[:, 0, 0:g], pv[:, :, 64], rr[:, d, sl], ALU.mult, r=[pot, rr], w=[dt_])
            kb.ts(dt_[:, 1, 0:g], dt_[:, 0, 0:g], -1.0, None, ALU.mult, r=[dt_], w=[dt_])
            kb.tt(dt_[:, 1, 0:g], dt_[:, 1, 0:g], dt_[:, 0, 0:g], ALU.max, r=[dt_], w=[dt_])
            kb.ts(dt_[:, 1, 0:g], dt_[:, 1, 0:g], 1.0, None, ALU.max, r=[dt_], w=[dt_])
            kb.op("dve", lambda e: e.reciprocal(out=dt_[:, 2, 0:g], in_=dt_[:, 1, 0:g]), r=[dt_], w=[dt_])
            kb.tt(dt_[:, 3, 0:g], dt_[:, 2, 0:g], rr[:, d, sl], ALU.mult, r=[dt_, rr], w=[dt_])
            if d == 0:
                kb.tt(hsum[:, sl, :], pv[:, :, 0:64], dt_[:, 3, 0:g].unsqueeze(2).broadcast_to([128, g, 64]), ALU.mult, r=[pot, dt_], w=[hsum])
            else:
                kb.tt(htmp[:, 0:g, :], pv[:, :, 0:64], dt_[:, 3, 0:g].unsqueeze(2).broadcast_to([128, g, 64]), ALU.mult, r=[pot, dt_], w=[htmp])
                kb.tt(hsum[:, sl, :], hsum[:, sl, :], htmp[:, 0:g, :], ALU.add, r=[hsum, htmp], w=[hsum], eng="pool")
        if DBG < 10.52:
            continue
        lin_attn(kb, cst, 64, 66, qT, kT, ktok, vp, aa[d], True, d, emit)
    if DBG < 10.7:
        return
    sk = bcast_load(kb, "msk", io["msk"], 64)
    kb.tt(xtok[:], xtok[:], sk[:].unsqueeze(1).broadcast_to([128, NCH, 64]), ALU.mult, r=[xtok, sk], w=[xtok])
    out_norm_gate(kb, cst, hsum, io["mnw"], io["mo"], AF.Sigmoid, xtok, 128, io, "mo")
    kb.pop()


def gla_B(kb, io, cst):
    kb.push()
    ident = cst["ident"]
    gvb = kb.sb("gvb", [128, NCH, 64], BF16)
    osum = kb.sb("osum", [128, NCH, 64])
    kb.dma("pool", gvb[:], io["gv"], r=[], w=gvb)
    ba = kb.sb("ba", [32, 2])
    kb.dma("sp", ba[:], io["ba"], r=[], w=ba)
    NP = 22
    rst = kb.sb("rst", [32, NP, 128])
    kb.op("pool", lambda e: e.memset(rst[:], 1.0), r=[], w=[rst])
    kb.op("pool", lambda e: e.memset(rst[:, :, 0:1], 0.0), r=[], w=[rst])
    qTt = kb.sb("gqT", [32, B_T], BF16)
    kTt = kb.sb("gkT", [32, B_T], BF16)
    ktok = kb.sb("gktok", [128, NCH, 32], BF16)
    a = kb.sb("ga", [32, NCH])
    for d in range(2):
        wa = kb.sb("wa%d" % d, [16, 32], BF16)
        kb.dma("pool", wa[:], io["wa"][d], r=[], w=wa)
        for n0 in range(0, NCH, NP):
            kb.push()
            c0 = n0 * 128
            cw_ = NP * 128
            ga = kb.sb("gain", [16, cw_], BF16)
            gq = kb.sb("gq", [32, cw_])
            gk = kb.sb("gk", [32, cw_])
            kb.dma("pool", ga[:], io["gaT"][d][:, c0:c0 + cw_], r=[], w=ga)
            kb.dma("sp", gq[:], io["gqT"][:, c0:c0 + cw_], r=[], w=gq)
            kb.dma("sp", gk[:], io["gkT"][:, c0:c0 + cw_], r=[], w=gk)
            la = kb.sb("la", [32, NP, 128])
            P = kb.sb("P", [32, NP, 128])
            Dm = kb.sb("Dm", [32, NP, 128])
            Ex = kb.sb("Ex", [32, NP, 128])
            khat = kb.sb("khat", [32, NP, 128])
            laf = la[:].rearrange("p n t -> p (n t)")
            Exf = Ex[:].rearrange("p n t -> p (n t)")
            pp = [kb.ps("gpp%d" % i, [128, 512]) for i in range(2)]
            for gi, x0 in enumerate(range(0, cw_, 512)):
                w_ = min(512, cw_ - x0)
                p = pp[gi % 2]
                kb.mm(p[0:32, 0:w_], wa[:], ga[:, x0:x0 + w_], True, True, r=[wa, ga], w=[p])
                kb.act(laf[:, x0:x0 + w_], p[0:32, 0:w_], AF.Sigmoid, r=[p, ba], w=[la], bias=ba[:, d:d + 1])
            kb.act(la[:], la[:], AF.Ln, r=[la], w=[la])
            kb.op("dve", lambda e: e.tensor_tensor_scan(out=P[:].rearrange("p n t -> p (n t)"), data0=rst[:].rearrange("p n t -> p (n t)"),
                                                         data1=laf, initial=0.0, op0=ALU.mult, op1=ALU.add), r=[rst, la], w=[P])
            kb.act(a[:, n0:n0 + NP], P[:, :, 127], AF.Exp, r=[P], w=[a], scale=1.0 / 16)
            kb.tt(Dm[:], P[:, :, 127:128].broadcast_to([32, NP, 128]), P[:], ALU.subtract, r=[P], w=[Dm])
            if d == 0:
                Bq = P
                Bke = Dm
            else:
                kb.tt(Dm[:], Dm[:], la[:], ALU.add, r=[Dm, la], w=[Dm])
                kb.tt(P[:], P[:], la[:], ALU.subtract, r=[P, la], w=[P])
                Bq = Dm
                Bke = P
            kb.act(Ex[:], Bq[:], AF.Exp, r=[Bq], w=[Ex], scale=1.0 / 16)
            kb.stt(qTt[:, c0:c0 + cw_], gq[:], float(32 ** -0.5), Exf, ALU.mult, ALU.mult, r=[gq, Ex], w=[qTt])
            kb.act(Ex[:], Bq[:], AF.Exp, r=[Bq], w=[Ex], scale=-1.0 / 16)
            kb.tt(kTt[:, c0:c0 + cw_], gk[:], Exf, ALU.mult, r=[gk, Ex], w=[kTt])
            kb.act(Ex[:], Bke[:], AF.Exp, r=[Bke], w=[Ex], scale=1.0 / 16)
            kb.tt(khat[:].rearrange("p n t -> p (n t)"), gk[:], Exf, ALU.mult, r=[gk, Ex], w=[khat])
            for gi, m0 in enumerate(range(0, NP, 16)):
                nn = min(16, NP - m0)
                p = pp[gi % 2]
                for i in range(nn):
                    kb.tr(p[:, i * 32:(i + 1) * 32], khat[:, m0 + i, :], ident[0:32, 0:32], r=[khat, ident], w=[p])
                kb.copy("dve", ktok[:, n0 + m0:n0 + m0 + nn, :].rearrange("p n d -> p (n d)"), p[:, 0:nn * 32], r=[p], w=[ktok])
            kb.pop()

        def emit(pot, ns, d=d):
            g = len(ns)
            sl = nsl(ns)
            pv = pot[:, 0:g * 64].rearrange("p (g v) -> p g v", v=64)
            if d == 0:
                kb.copy("dve", osum[:, sl, :], pv, r=[pot], w=[osum])
            else:
                kb.tt(osum[:, sl, :], pv, osum[:, sl, :], ALU.add, r=[osum, pot], w=[osum])
        lin_attn(kb, cst, 32, 64, qTt, kTt, ktok, gvb, a, False, d, emit)
    out_norm_gate(kb, cst, osum, io["gnw"], io["gr"], AF.Silu, None, 192, io, "go")
    kb.pop()


B_INPUTS = [("QT", [2, 96, SEQ], BF16), ("QcT", [2, 96, NCTX], BF16), ("KT", [2, 96, B_T], BF16), ("V", [128, NCH, 128], BF16),
            ("mxT", [64, B_T], F32), ("gqT", [32, B_T], F32), ("gkT", [32, B_T], F32), ("gaT", [2, 16, B_T], F32),
            ("mg", [128, NCH, 4], F32), ("mv", [128, NCH, 64], F32), ("mo", [128, NCH, 64], F32), ("gv", [128, NCH, 64], F32), ("gr", [128, NCH, 64], F32),
            ("cw", [64, 5], F32), ("cb", [64, 1], F32), ("wq", [64, 64], F32), ("wk", [64, 64], F32), ("gb", [4], F32),
            ("mnw", [64], F32), ("msk", [64], F32), ("wa", [2, 16, 32], F32), ("ba", [32, 2], F32), ("gnw", [64], F32)]


def build_B(need_ctx=True, parts=("attn", "mlstm", "gla")):
    nc = bass.Bass("TRN2", target_bir_lowering=False)
    io = IO()
    for nm, shp, dt in B_INPUTS:
        declare(nc, io, nm, shp, dt, "ExternalInput")
    cd = {}
    for nm in ("ident", "triu", "tril"):
        th = nc.dram_tensor("c_" + nm, CONST_SHAPES[nm], F32, kind="ExternalInput")
        cd[nm] = T(th, "c_" + nm)
    declare(nc, io, "mixT", [256, B_T], BF16, "ExternalOutput")
    with ExitStack() as st:
        kb = KB(nc, st)
        cst = load_consts(kb, cd, ["ident", "triu", "tril"])
        if "mlstm" in parts:
            mlstm_B(kb, io, cst)
        if "gla" in parts:
            gla_B(kb, io, cst)
        if "attn" in parts:
            attention_B(kb, io, need_ctx)
        while len(kb.stacks) > 1:
            kb.stacks.pop().close()
        kb.finish([io["mixT_res"]])
        print("phase B: n_inst", kb.n_inst, "n_wait", kb.n_wait, "dsems", kb.ndsem)
    return nc


def b_inmaps(inp, l, aout):
    cst, _ = consts()
    maps = []
    for b in range(NB):
        cs = [aout[4 * b + j] for j in range(4)]

        def cat_t(name, axis):
            parts = [np.take(c[name], np.arange(0, TL), axis=axis) for c in cs]
            parts.append(np.take(cs[0][name], np.arange(TL, NTOK), axis=axis))
            return np.concatenate(parts, axis=axis)
        qT = cat_t("qT", 2)
        kT = cat_t("kT", 2)
        v = cat_t("v", 0)
        fm = cat_t("fm", 1)
        tm = cat_t("tm", 0)
        for j in range(4):
            def pm(a):
                return np.ascontiguousarray(a.reshape(NCH, 128, a.shape[-1]).transpose(1, 0, 2))
            m = {"QT": np.ascontiguousarray(qT[2 * j:2 * j + 2, :, 0:SEQ]), "QcT": np.ascontiguousarray(qT[2 * j:2 * j + 2, :, SEQ:]),
                 "KT": np.ascontiguousarray(kT[2 * j:2 * j + 2]), "V": pm(v[:, 128 * j:128 * j + 128]),
                 "mxT": f32(fm[64 * j:64 * j + 64]), "gqT": f32(fm[256 + 32 * j:256 + 32 * j + 32]),
                 "gkT": f32(fm[384 + 32 * j:384 + 32 * j + 32]), "gaT": f32(fm[512:544].reshape(2, 16, B_T)),
                 "mg": pm(f32(tm[:, [j, 4 + j, 8 + j, 12 + j]])), "mv": pm(f32(tm[:, 16 + 64 * j:16 + 64 * j + 64])),
                 "mo": pm(f32(tm[:, 272 + 64 * j:272 + 64 * j + 64])), "gv": pm(f32(tm[:, 528 + 64 * j:528 + 64 * j + 64])),
                 "gr": pm(f32(tm[:, 784 + 64 * j:784 + 64 * j + 64])),
                 "cw": f32(inp["ml_conv_w"][l][:, 64 * j:64 * j + 64].T), "cb": f32(inp["ml_conv_b"][l][64 * j:64 * j + 64][:, None]),
                 "wq": f32(inp["ml_wq"][l][j]), "wk": f32(inp["ml_wk"][l][j]),
                 "gb": f32(inp["ml_gate_b"][l][[j, 4 + j, 8 + j, 12 + j]]),
                 "mnw": f32(inp["ml_norm_w"][l][64 * j:64 * j + 64]), "msk": f32(inp["ml_skip"][l][64 * j:64 * j + 64]),
                 "wa": f32(inp["gla_wa"][l][:, :, 32 * j:32 * j + 32]), "ba": f32(inp["gla_ba"][l][:, 32 * j:32 * j + 32].T),
                 "gnw": f32(inp["gla_norm_w"][l][64 * j:64 * j + 64]),
                 "c_ident": cst["ident"], "c_triu": cst["triu"], "c_tril": cst["tril"]}
            maps.append(m)
    return maps


def phase_C(kb, io, cst):
    ident = cst["ident"]
    NE = 32
    x1 = kb.sb("x1_all", [128, NT, 1024])
    h2T = kb.sb("h2T_all", [128, 8, NTOK], BF16)
    gTh = kb.sb("gTh", [32, NTOK], BF16)
    gTl = kb.sb("gTl", [32, NTOK], BF16)
    selb = kb.sb("selb", [32, NE * 128], BF16)
    kb.dma("pool", selb[:], cst["sel_d"], r=[], w=selb)
    gate2 = [kb.sb("gate2_%d" % i, [128, 1024]) for i in range(2)]
    kb.push()
    PS = [kb.ps("psC%d" % i, [128, 2, 512]) for i in range(2)]
    plg = kb.ps("plg", [128, 512])
    pgt = kb.ps("pgtC", [128, 512])
    mods = compute_mod(kb, io["cc"], io["w_mod"], io["b_mod"], [2, 3, 4, 5], PS, pre={5: (gate2[0], gate2[1])})
    n2 = bcast_load(kb, "n2", io["norm2_w"], 1024)
    G2 = []
    for i in range(2):
        g = mods[4][i]
        kb.stt(g[:], g[:], 1.0, n2[:], ALU.add, ALU.mult, r=[g, n2], w=[g])
        G2.append(g)
    gate1 = mods[2]
    S2 = mods[3]
    w_out = kb.sb("w_out", [128, 8, 1024], BF16)
    for kc in range(8):
        kb.dma("pool", w_out[:, kc, :], io["w_out"][kc * 128:(kc + 1) * 128, :], r=[], w=w_out)
    wr = kb.sb("wr", [128, 8, 36])
    kb.dma("sp", wr[:, :, 0:4], io["w_grp"].rearrange("(k p) n -> p k n", p=128), r=[], w=wr)
    kb.dma("sp", wr[:, :, 4:36], io["w_erouter"].rearrange("(k p) n -> p k n", p=128), r=[], w=wr)
    wrh = kb.sb("wrh", [128, 8, 36], BF16)
    wrl = kb.sb("wrl", [128, 8, 36], BF16)
    kb.copy("dve", wrh[:], wr[:], r=[wr], w=[wrh])
    kb.tt(wrl[:], wr[:], wrh[:], ALU.subtract, r=[wr, wrh], w=[wrl])
    rb = kb.sb("rb", [128, 36])
    kb.dma("sp", rb[:, 0:4], io["b_grp"].partition_broadcast(128), r=[], w=rb)
    kb.dma("sp", rb[:, 4:36], io["b_erouter"].partition_broadcast(128), r=[], w=rb)
    NB_ = 2
    xt = [kb.sb("xtC%d" % i, [128, 1024]) for i in range(NB_)]
    mT = [kb.sb("mTC%d" % i, [128, 8, 128], BF16) for i in range(NB_)]
    tmp = [kb.sb("tmpC", [128, 1024])] * NB_
    h2 = [kb.sb("h2C", [128, 1024])] * NB_
    h2l = [kb.sb("h2l", [128, 8, 128], BF16)] * NB_
    st = [kb.sb("stC%d" % i, [128, 16]) for i in range(NB_)]
    lg = [kb.sb("lg%d" % i, [128, 36]) for i in range(NB_)]
    rw = [kb.sb("rw%d" % i, [128, 6, 32]) for i in range(NB_)]
    m8 = [kb.sb("m8_%d" % i, [128, 8]) for i in range(NB_)]
    for ti in range(NT):
        b_ = ti % NB_
        mi = 1 if ti >= 16 else 0
        t0 = ti * 128
        X, MT, TMP, H2, H2L, ST, LG, RW, M8 = xt[b_], mT[b_], tmp[b_], h2[b_], h2l[b_], st[b_], lg[b_], rw[b_], m8[b_]
        kb.dma("sp", X[:], io["x"][t0:t0 + 128, :], r=[], w=X)
        kb.dma("sp", MT[:], io["mixT"][:, t0:t0 + 128].rearrange("(k p) t -> p k t", p=128), r=[], w=MT)
        for hf in range(2):
            for kc in range(8):
                kb.mm(PS[0][:, hf, :], MT[:, kc, :], w_out[:, kc, hf * 512:(hf + 1) * 512], kc == 0, kc == 7, r=[MT, w_out], w=[PS[0]])
        X1 = x1[:, ti, :]
        kb.tt(TMP[:], PS[0][:].rearrange("p a b -> p (a b)"), gate1[mi][:], ALU.mult, r=[PS[0], gate1[mi]], w=[TMP])
        kb.tt(X1, TMP[:], X[:], ALU.add, r=[TMP, X], w=[x1], eng="pool")
        kb.act(TMP[:], X1, AF.Square, r=[x1], w=[TMP, ST], accum_out=ST[:, 0:1])
        rstd_of(kb, ST[:, 0:1], 1, 1024, ST[:, 1:2], ST[:, 2:3], r=[ST], w=[ST])
        kb.stt(H2[:], X1, ST[:, 2:3], G2[mi][:], ALU.mult, ALU.mult, r=[x1, ST, G2[mi]], w=[H2])
        kb.tt(H2[:], H2[:], S2[mi][:], ALU.add, r=[H2, S2[mi]], w=[H2], eng="pool")
        for kc in range(8):
            kb.tr(PS[1][:, kc // 4, (kc % 4) * 128:(kc % 4 + 1) * 128], H2[:, kc * 128:(kc + 1) * 128], ident[:], r=[H2, ident], w=[PS[1]])
        hi = h2T[:, :, t0:t0 + 128]
        for a in range(2):
            kb.copy("act", h2T[:, 4 * a:4 * a + 4, t0:t0 + 128], PS[1][:, a, :].rearrange("p (b t) -> p b t", t=128), r=[PS[1]], w=[h2T])
        for a in range(2):
            kb.tt(H2L[:, 4 * a:4 * a + 4, :], PS[1][:, a, :].rearrange("p (b t) -> p b t", t=128), h2T[:, 4 * a:4 * a + 4, t0:t0 + 128],
                  ALU.subtract, r=[PS[1], h2T], w=[H2L])
        n = 0
        for kc in range(8):
            for (l_, r_, lr, rr_) in ((hi[:, kc, :], wrh[:, kc, :], h2T, wrh), (H2L[:, kc, :], wrh[:, kc, :], H2L, wrh), (hi[:, kc, :], wrl[:, kc, :], h2T, wrl)):
                kb.mm(plg[:, 0:36], l_, r_, n == 0, n == 23, r=[lr, rr_], w=[plg])
                n += 1
        kb.tt(LG[:], plg[:, 0:36], rb[:], ALU.add, r=[plg, rb], w=[LG])
        kb.op("dve", lambda e: e.tensor_reduce(out=ST[:, 4:5], in_=LG[:, 0:4], axis=AX.X, op=ALU.max), r=[LG], w=[ST])
        kb.tt(RW[:, 0, 0:4], LG[:, 0:4], ST[:, 4:5].broadcast_to([128, 4]), ALU.is_equal, r=[LG, ST], w=[RW])
        kb.ts(ST[:, 5:6], ST[:, 4:5], -1.0, None, ALU.mult, r=[ST], w=[ST])
        kb.act(RW[:, 1, 0:4], LG[:, 0:4], AF.Exp, r=[LG, ST], w=[RW, ST], bias=ST[:, 5:6], accum_out=ST[:, 6:7])
        kb.op("dve", lambda e: e.reciprocal(out=ST[:, 7:8], in_=ST[:, 6:7]), r=[ST], w=[ST])
        kb.ts(RW[:, 2, 0:4], RW[:, 0, 0:4], 1e9, -1e9, ALU.mult, ALU.add, r=[RW], w=[RW])
        EM = RW[:, 3, :]
        kb.tt(RW[:, 3, :].rearrange("p (g e) -> p g e", e=8), LG[:, 4:36].rearrange("p (g e) -> p g e", e=8),
              RW[:, 2, 0:4].unsqueeze(2).broadcast_to([128, 4, 8]), ALU.add, r=[LG, RW], w=[RW])
        kb.op("dve", lambda e: e.max(out=M8[:], in_=EM), r=[RW], w=[M8])
        kb.tt(ST[:, 8:9], M8[:, 1:2], M8[:, 0:1], ALU.subtract, r=[M8], w=[ST])
        kb.act(ST[:, 8:9], ST[:, 8:9], AF.Exp, r=[ST], w=[ST])
        kb.ts(ST[:, 8:9], ST[:, 8:9], 1.0, None, ALU.add, r=[ST], w=[ST])
        kb.op("dve", lambda e: e.reciprocal(out=ST[:, 9:10], in_=ST[:, 8:9]), r=[ST], w=[ST])
        kb.ts(ST[:, 10:11], ST[:, 9:10], -1.0, 1.0, ALU.mult, ALU.add, r=[ST], w=[ST])
        kb.tt(ST[:, 11:12], ST[:, 9:10], ST[:, 7:8], ALU.mult, r=[ST], w=[ST])
        kb.tt(ST[:, 12:13], ST[:, 10:11], ST[:, 7:8], ALU.mult, r=[ST], w=[ST])
        kb.tt(RW[:, 4, :], EM, M8[:, 0:1].broadcast_to([128, 32]), ALU.is_equal, r=[RW, M8], w=[RW])
        kb.ts(RW[:, 4, :], RW[:, 4, :], ST[:, 11:12], None, ALU.mult, r=[RW, ST], w=[RW])
        kb.tt(RW[:, 5, :], EM, M8[:, 1:2].broadcast_to([128, 32]), ALU.is_equal, r=[RW, M8], w=[RW])
        kb.ts(RW[:, 5, :], RW[:, 5, :], ST[:, 12:13], None, ALU.mult, r=[RW, ST], w=[RW])
        kb.tt(RW[:, 4, :], RW[:, 4, :], RW[:, 5, :], ALU.add, r=[RW], w=[RW])
        kb.tr(pgt[0:32, 0:128], RW[:, 4, :], ident[:], r=[RW, ident], w=[pgt])
        kb.copy("dve", gTh[:, t0:t0 + 128], pgt[0:32, 0:128], r=[pgt], w=[gTh])
        kb.tt(gTl[:, t0:t0 + 128], pgt[0:32, 0:128], gTh[:, t0:t0 + 128], ALU.subtract, r=[pgt, gTh], w=[gTl])
    kb.pop()
    kb.push()
    NSLOT = 4
    EG = 2
    wg = [kb.sb("wg%d" % i, [128, 8, 256], BF16) for i in range(NSLOT)]
    wu = [kb.sb("wu%d" % i, [128, 8, 256], BF16) for i in range(NSLOT)]
    wd = [kb.sb("wd%d" % i, [128, 2, 1024], BF16) for i in range(NSLOT)]
    pgu = [kb.ps("pgu%d" % i, [128, 2, 256]) for i in range(2)]
    pbc = kb.ps("pbcC", [128, 512])
    pacc = [kb.ps("pacc%d" % i, [128, 512]) for i in range(4)]
    sg = [kb.sb("sg%d" % i, [128, 256], BF16) for i in range(2)]
    tu = [kb.sb("tu%d" % i, [128, 256]) for i in range(2)]
    aT = [kb.sb("aT%d" % i, [128, 256], BF16) for i in range(4)]
    fl = [kb.sb("fl%d" % i, [128, 512]) for i in range(2)]
    TG = 256
    it = {"f": 0, "a": 0, "fl": 0}

    def load_expert(e):
        s_ = e % NSLOT
        kb.dma("pool", wg[s_][:], io["w_gate"][e].rearrange("(k p) f -> p k f", p=128), r=[], w=wg[s_])
        kb.dma("pool", wu[s_][:], io["w_up"][e].rearrange("(k p) f -> p k f", p=128), r=[], w=wu[s_])
        kb.dma("pool", wd[s_][:], io["w_down"][e].rearrange("(c p) d -> p c d", p=128), r=[], w=wd[s_])

    for e in range(min(NSLOT, NE)):
        load_expert(e)
    for g0 in range(0, NE, EG):
        for tg in range(NTOK // TG):
            c0 = tg * TG
            mi = 1 if tg >= 8 else 0
            first = True
            for e in range(g0, g0 + EG):
                s_ = e % NSLOT
                kb.mm(pbc[:, 0:TG], selb[:, e * 128:(e + 1) * 128], gTh[:, c0:c0 + TG], True, False, r=[selb, gTh], w=[pbc])
                kb.mm(pbc[:, 0:TG], selb[:, e * 128:(e + 1) * 128], gTl[:, c0:c0 + TG], False, True, r=[selb, gTl], w=[pbc])
                ats = []
                for fc in range(2):
                    p = pgu[it["f"] % 2]
                    sgt = sg[it["f"] % 2]
                    tut = tu[it["f"] % 2]
                    it["f"] += 1
                    at = aT[it["a"] % 4]
                    it["a"] += 1
                    for kc in range(8):
                        kb.mm(p[:, 0, :], wg[s_][:, kc, fc * 128:(fc + 1) * 128], h2T[:, kc, c0:c0 + TG], kc == 0, kc == 7, r=[wg[s_], h2T], w=[p])
                    for kc in range(8):
                        kb.mm(p[:, 1, :], wu[s_][:, kc, fc * 128:(fc + 1) * 128], h2T[:, kc, c0:c0 + TG], kc == 0, kc == 7, r=[wu[s_], h2T], w=[p])
                    kb.act(sgt[:], p[:, 0, :], AF.Silu, r=[p], w=[sgt])
                    kb.tt(tut[:], p[:, 1, :], sgt[:], ALU.mult, r=[p, sgt], w=[tut])
                    kb.tt(at[:], pbc[:, 0:TG], tut[:], ALU.mult, r=[pbc, tut], w=[at])
                    ats.append(at)
                for fc in range(2):
                    last = (e == g0 + EG - 1) and fc == 1
                    for sub in range(2):
                        for hf in range(2):
                            kb.mm(pacc[sub * 2 + hf][:, :], ats[fc][:, sub * 128:(sub + 1) * 128], wd[s_][:, fc, hf * 512:(hf + 1) * 512],
                                  first and fc == 0, last, r=[ats[fc], wd[s_]], w=[pacc[sub * 2 + hf]])
                first = False
            for sub in range(2):
                ti = tg * 2 + sub
                for hf in range(2):
                    f = fl[it["fl"] % 2]
                    it["fl"] += 1
                    kb.tt(f[:], pacc[sub * 2 + hf][:, :], gate2[mi][:, hf * 512:(hf + 1) * 512], ALU.mult, r=[pacc[sub * 2 + hf], gate2[mi]], w=[f])
                    kb.tt(x1[:, ti, hf * 512:(hf + 1) * 512], x1[:, ti, hf * 512:(hf + 1) * 512], f[:], ALU.add, r=[x1, f], w=[x1], eng="pool")
        for e in range(g0 + NSLOT, min(g0 + NSLOT + EG, NE)):
            load_expert(e)
    for ti in range(NT):
        kb.dma("sp", io["xo"][ti * 128:(ti + 1) * 128, :], x1[:, ti, :], r=[x1], w=io["xo_res"])
    kb.pop()


C_INPUTS = [("x", [NTOK, D], F32), ("mixT", [D, NTOK], BF16), ("cc", [128, 8, 2], F32), ("w_mod", [D, 6 * D], F32), ("b_mod", [6 * D], F32),
            ("w_out", [D, D], F32), ("norm2_w", [D], F32), ("w_grp", [D, 4], F32), ("b_grp", [4], F32),
            ("w_erouter", [D, 32], F32), ("b_erouter", [32], F32), ("w_gate", [32, D, 256], F32), ("w_up", [32, D, 256], F32),
            ("w_down", [32, 256, D], F32)]


def build_C():
    nc = bass.Bass("TRN2", target_bir_lowering=False)
    io = IO()
    for nm, shp, dt in C_INPUTS:
        declare(nc, io, nm, shp, dt, "ExternalInput")
    cd = {}
    for nm in ("ident", "sel"):
        th = nc.dram_tensor("c_" + nm, CONST_SHAPES[nm], F32, kind="ExternalInput")
        cd[nm] = T(th, "c_" + nm)
    declare(nc, io, "xo", [NTOK, D], F32, "ExternalOutput")
    with ExitStack() as st:
        kb = KB(nc, st)
        cst = load_consts(kb, cd, ["ident"])
        cst["sel_d"] = cd["sel"][:]
        phase_C(kb, io, cst)
        while len(kb.stacks) > 1:
            kb.stacks.pop().close()
        kb.finish([io["xo_res"]])
        print("phase C: n_inst", kb.n_inst, "n_wait", kb.n_wait, "dsems", kb.ndsem)
    return nc


def c_inmaps(inp, l, xl, xc, bout):
    cst, _ = consts()
    maps = []
    for b in range(NB):
        full = np.zeros((D, B_T), dtype=bout[0].dtype)
        for j in range(4):
            m = bout[4 * b + j]
            full[128 * j:128 * j + 128] = m[0:128]
            full[512 + 64 * j:512 + 64 * j + 64] = m[128:192]
            full[768 + 64 * j:768 + 64 * j + 64] = m[192:256]
        for j in range(4):
            core = 4 * b + j
            cols = np.concatenate([np.arange(j * TL, (j + 1) * TL), np.arange(SEQ, B_T)])
            mm_ = {"x": f32(core_tokens(xl, xc, core)), "mixT": np.ascontiguousarray(full[:, cols]),
                   "cc": cc_layout(inp["c"][b], inp["c_ctx"]), "c_ident": cst["ident"], "c_sel": cst["sel"]}
            for nm in ("w_mod", "b_mod", "w_out", "norm2_w", "w_grp", "b_grp", "w_erouter", "b_erouter", "w_gate", "w_up", "w_down"):
                mm_[nm] = f32(inp[nm][l])
            maps.append(mm_)
    return maps


def _run(nc, maps):
    return run_bass_kernel_spmd(nc, maps, core_ids=list(range(NCORE))).results


def kernel(**inputs):
    inp = {k: np.asarray(v) for k, v in inputs.items()}
    xl = f32(inp["x"])
    xc = f32(inp["ctx"])
    for l in range(DEPTH):
        ra = _run(build_A(), a_inmaps(inp, l, xl, xc))
        aout = [{k: np.asarray(r[k]) for k in ("qT", "kT", "v", "fm", "tm")} for r in ra]
        del ra
        rb = _run(build_B(True), b_inmaps(inp, l, aout))
        bout = [np.asarray(r["mixT"]) for r in rb]
        del rb, aout
        rc = _run(build_C(), c_inmaps(inp, l, xl, xc, bout))
        xl_n = np.empty_like(xl)
        xc_n = np.empty_like(xc)
        for core in range(NCORE):
            b, j = core // 4, core % 4
            xo = np.asarray(rc[core]["xo"], dtype=np.float32)
            xl_n[b, j * TL:(j + 1) * TL] = xo[:TL]
            if j == 0:
                xc_n[b] = xo[TL:]
        xl, xc = xl_n, xc_n
        del rc, bout
    return xl
```

```python
import numpy as np
import ml_dtypes
from contextlib import ExitStack
import concourse.bass as bass
import concourse.mybir as mybir
from concourse.bass_utils import run_bass_kernel_spmd

F32 = mybir.dt.float32
BF16 = mybir.dt.bfloat16
AF = mybir.ActivationFunctionType
ALU = mybir.AluOpType
AX = mybir.AxisListType

D = 1024
NB = 2
SEQ = 8192
DEPTH = 4
NCTX = 256
EPS = 1e-6
NCORE = 8
TL = 2048
NT = 18
NTOK = NT * 128
D_IN = 2000
O_CQ, O_CKV, O_KR, O_MX, O_MV, O_MO, O_MG, O_GQ, O_GK, O_GV, O_GR, O_GA = (
    0, 256, 384, 416, 672, 928, 1184, 1200, 1328, 1456, 1712, 1968)

SAME_ENG_SYNC = True
import os
DBG = float(os.environ.get('KDBG', '99'))


class Res:
    __slots__ = ("name", "w", "r", "dsem", "dcnt", "dkey", "excl")

    def __init__(self, name):
        self.name = name
        self.excl = False
        self.w = None
        self.r = {}
        self.dsem = None
        self.dcnt = 0
        self.dkey = None


class T:
    def __init__(self, th, name):
        self.t = th
        self.res = Res(name)
        self.name = name

    def __getitem__(self, idx):
        return self.t[idx]


def _res(x):
    return x.res if isinstance(x, T) else x


class KB:
    def __init__(self, nc, stack):
        self.nc = nc
        self.st = stack
        self.eng = {"pe": nc.tensor, "dve": nc.vector, "act": nc.scalar,
                    "pool": nc.gpsimd, "sp": nc.sync}
        self.semh = {}
        self.cnt = {}
        for k in self.eng:
            self.semh[k] = stack.enter_context(nc.semaphore("s_" + k))
            self.cnt[k] = 0
        self.waited = {k: {} for k in self.eng}
        self.ndsem = 0
        self.n_inst = 0
        self.n_wait = 0
        self.uid = 0
        self.stacks = [stack]
        self.dres = []

    def sb(self, name, shape, dt=F32):
        self.uid += 1
        nm = "%s_%d" % (name, self.uid)
        return T(self.stacks[-1].enter_context(self.nc.sbuf_tensor(nm, list(shape), dt)), nm)

    def ps(self, name, shape, dt=F32):
        self.uid += 1
        nm = "%s_%d" % (name, self.uid)
        t = T(self.stacks[-1].enter_context(self.nc.psum_tensor(nm, list(shape), dt)), nm)
        t.res.excl = True
        return t

    def push(self):
        self.stacks.append(ExitStack())

    def pop(self):
        self.barrier()
        self.stacks.pop().close()

    def barrier(self):
        for e in self.eng:
            deps = {k: self.cnt[k] for k in self.eng if k != e and self.cnt[k] > 0}
            for res in self.dres:
                deps[res.dkey] = res.dcnt
            self._wait(e, deps)

    def _wait(self, e, deps):
        for key, val in deps.items():
            if self.waited[e].get(key, 0) >= val:
                continue
            if key == e and (e == "pe" or not SAME_ENG_SYNC):
                continue
            self.eng[e].wait_ge(self.semh[key], val)
            self.waited[e][key] = val
            self.n_wait += 1

    def _collect(self, reads, writes, dma_write=None):
        deps = {}

        def add(ev):
            if ev is None:
                return
            k, v = ev
            if deps.get(k, 0) < v:
                deps[k] = v

        def cur(res):
            if res.w is None:
                return None
            if res.w[0] == res.dkey:
                return (res.dkey, res.dcnt)
            return res.w

        for r in reads:
            add(cur(r))
        for w in writes:
            if dma_write is not None and w is dma_write and w.w is not None \
                    and w.w[0] == w.dkey and not w.r:
                continue
            add(cur(w))
            for k, v in w.r.items():
                add((k, v))
        return deps

    def op(self, e, fn, r=(), w=()):
        reads = [_res(x) for x in r]
        writes = [_res(x) for x in w]
        ex = [x for x in reads if x.excl and x not in writes]
        if ex:
            reads = [x for x in reads if not x.excl]
            writes = writes + ex
        self._wait(e, self._collect(reads, writes))
        ins = fn(self.eng[e])
        self.cnt[e] += 1
        ins.then_inc(self.semh[e], 1)
        self.n_inst += 1
        ev = (e, self.cnt[e])
        for rr in reads:
            if rr.r.get(e, 0) < ev[1]:
                rr.r[e] = ev[1]
        for ww in writes:
            ww.w = ev
            ww.r = {}
        return ins

    def dma(self, q, out, in_, r=(), w=None, **kw):
        reads = [_res(x) for x in r]
        wres = _res(w)
        if wres.dsem is None:
            wres.dkey = "d%d" % self.ndsem
            self.ndsem += 1
            wres.dsem = self.st.enter_context(self.nc.semaphore(wres.dkey))
            self.semh[wres.dkey] = wres.dsem
            self.dres.append(wres)
        self._wait(q, self._collect(reads, [wres], dma_write=wres))
        ins = self.eng[q].dma_start(out=out, in_=in_, **kw)
        wres.dcnt += 16
        ins.then_inc(wres.dsem, 16)
        self.n_inst += 1
        ev = (wres.dkey, wres.dcnt)
        for rr in reads:
            if rr.r.get(ev[0], 0) < ev[1]:
                rr.r[ev[0]] = ev[1]
        wres.w = ev
        wres.r = {}
        return ins

    def finish(self, outs, e="sp"):
        self._wait(e, self._collect([_res(x) for x in outs], []))

    def mm(self, out, lhsT, rhs, start, stop, r, w):
        return self.op("pe", lambda e: e.matmul(out, lhsT=lhsT, rhs=rhs, start=start, stop=stop), r=r, w=w)

    def tr(self, out, in_, ident, r, w):
        return self.op("pe", lambda e: e.transpose(out=out, in_=in_, identity=ident), r=r, w=w)

    def copy(self, eng, out, in_, r, w):
        if eng == "act":
            return self.op("act", lambda e: e.copy(out=out, in_=in_), r=r, w=w)
        return self.op(eng, lambda e: e.tensor_copy(out=out, in_=in_), r=r, w=w)

    def act(self, out, in_, func, r, w, **kw):
        return self.op("act", lambda e: e.activation(out=out, in_=in_, func=func, **kw), r=r, w=w)

    def tt(self, out, in0, in1, op, r, w, eng="dve"):
        return self.op(eng, lambda e: e.tensor_tensor(out=out, in0=in0, in1=in1, op=op), r=r, w=w)

    def ts(self, out, in0, s1, s2, op0, op1=None, r=(), w=(), eng="dve"):
        if op1 is None:
            return self.op(eng, lambda e: e.tensor_scalar(out=out, in0=in0, scalar1=s1, scalar2=None, op0=op0), r=r, w=w)
        return self.op(eng, lambda e: e.tensor_scalar(out=out, in0=in0, scalar1=s1, scalar2=s2, op0=op0, op1=op1), r=r, w=w)

    def stt(self, out, in0, scalar, in1, op0, op1, r, w, accum_out=None):
        return self.op("dve", lambda e: e.scalar_tensor_tensor(out=out, in0=in0, scalar=scalar, in1=in1, op0=op0, op1=op1, accum_out=accum_out), r=r, w=w)


def rstd_of(kb, ss, n, nfeat, tmp, out, r, w):
    kb.ts(tmp, ss, 1.0 / nfeat, EPS, ALU.mult, ALU.add, r=r, w=w)
    kb.act(tmp, tmp, AF.Sqrt, r=w, w=w)
    kb.op("dve", lambda e: e.reciprocal(out=out, in_=tmp), r=w, w=w)


def rope_tables():
    rows = SEQ // 64
    row = np.broadcast_to(np.arange(rows, dtype=np.float32)[:, None], (rows, 64)).reshape(-1)
    col = np.broadcast_to(np.arange(64, dtype=np.float32)[None, :], (rows, 64)).reshape(-1)
    inv = (np.float32(10000.0) ** (-np.arange(8, dtype=np.float32) / np.float32(8))).astype(np.float32)
    ang = np.concatenate([row[:, None] * inv, col[:, None] * inv], axis=-1).astype(np.float32)
    return np.concatenate([np.cos(ang), np.sin(ang)], axis=-1).astype(np.float32)


def host_consts():
    c = {}
    c["ident"] = np.eye(128, dtype=np.float32)
    j = np.arange(128)
    c["triu"] = (j[:, None] <= j[None, :]).astype(np.float32)
    c["tril"] = (j[:, None] >= j[None, :]).astype(np.float32)
    sel = np.zeros((32, 32, 128), np.float32)
    for e in range(32):
        sel[e, e, :] = 1.0
    c["sel"] = sel.reshape(32, 32 * 128)
    return c


def load_consts(kb, cd, names):
    out = {}
    for nm in names:
        shp = {"ident": [128, 128], "triu": [128, 128], "tril": [128, 128], "sel": [32, 32 * 128]}[nm]
        t = kb.sb("c_" + nm, shp)
        kb.dma("sp", t[:], cd[nm][:], r=[cd[nm]], w=t)
        out[nm] = t
        if nm in ("triu", "tril"):
            tb = kb.sb("c_" + nm + "_b", shp, BF16)
            kb.copy("dve", tb[:], t[:], r=[t], w=[tb])
            out[nm + "_b"] = tb
    return out


def bcast_load(kb, name, ap1d, n, q="sp"):
    t = kb.sb(name, [128, n])
    kb.dma(q, t[:], ap1d.partition_broadcast(128), r=[], w=t)
    return t


def compute_mod(kb, cc_d, w_mod_d, b_mod_d, chunks, pool_ps, pre=None):
    res = {}
    for j in chunks:
        if pre is not None and j in pre:
            res[j] = pre[j]
        else:
            res[j] = (kb.sb("mod_l%d" % j, [128, 1024]), kb.sb("mod_c%d" % j, [128, 1024]))
    kb.push()
    cc = kb.sb("cc", [128, 8, 2])
    kb.dma("sp", cc[:], cc_d, r=[], w=cc)
    sc = kb.sb("sc", [128, 8, 2])
    kb.act(sc[:], cc[:], AF.Silu, r=[cc], w=[sc])
    SC = [kb.sb("SC%d" % i, [128, 8, 128], BF16) for i in range(2)]
    for i in range(2):
        kb.copy("dve", SC[i][:], sc[:, :, i:i + 1].broadcast_to([128, 8, 128]), r=[sc], w=[SC[i]])
    wm = [kb.sb("wm%d" % i, [128, 8, 512], BF16) for i in range(2)]
    bm = [kb.sb("bm%d" % i, [128, 512]) for i in range(2)]
    si = 0
    for j in chunks:
        tl, tc_ = res[j]
        for hf in range(2):
            c0 = j * 1024 + hf * 512
            wmt = wm[si % 2]
            bmt = bm[si % 2]
            si += 1
            kb.dma("pool", wmt[:], w_mod_d[:, c0:c0 + 512].rearrange("(k p) n -> p k n", p=128), r=[], w=wmt)
            kb.dma("sp", bmt[:], b_mod_d[c0:c0 + 512].partition_broadcast(128), r=[], w=bmt)
            for i, dst in enumerate((tl, tc_)):
                ps = pool_ps[i]
                for kc in range(8):
                    kb.mm(ps[:, 0, :], SC[i][:, kc, :], wmt[:, kc, :], kc == 0, kc == 7, r=[SC[i], wmt], w=[ps])
                kb.tt(dst[:, hf * 512:(hf + 1) * 512], ps[:, 0, :], bmt[:], ALU.add, r=[ps, bmt], w=[dst])
    kb.pop()
    return res


def phase_A(kb, io, cst):
    ident = cst["ident"]
    PS = [kb.ps("psA%d" % i, [128, 2, 512]) for i in range(4)]
    mods = compute_mod(kb, io["cc"], io["w_mod"], io["b_mod"], [0, 1], PS)
    n1 = bcast_load(kb, "n1", io["norm1_w"], 1024)
    G1 = []
    S1 = []
    for i in range(2):
        g = kb.sb("G1_%d" % i, [128, 1024])
        kb.stt(g[:], mods[1][i][:], 1.0, n1[:], ALU.add, ALU.mult, r=[mods[1][i], n1], w=[g])
        G1.append(g)
        S1.append(mods[0][i])
    w_in = kb.sb("w_in", [128, 8, D_IN], BF16)
    for kc in range(8):
        kb.dma("pool", w_in[:, kc, :], io["w_in"][kc * 128:(kc + 1) * 128, :], r=[], w=w_in)
    w_uq = kb.sb("w_uq", [128, 2, 768], BF16)
    kb.dma("pool", w_uq[:], io["w_uq"].rearrange("(k p) n -> p k n", p=128), r=[], w=w_uq)
    w_ukv = kb.sb("w_ukv", [128, 1024], BF16)
    kb.dma("pool", w_ukv[:], io["w_ukv"], r=[], w=w_ukv)
    qan = bcast_load(kb, "qan", io["q_a_norm"], 256)
    kvan = bcast_load(kb, "kvan", io["kv_a_norm"], 128)
    qnw1 = bcast_load(kb, "qnw1", io["q_norm_w"], 96)
    knw1 = bcast_load(kb, "knw1", io["k_norm_w"], 96)
    qnw = kb.sb("qnw", [128, 8, 96])
    knw = kb.sb("knw", [128, 8, 96])
    kb.ts(qnw[:], qnw1[:].unsqueeze(1).broadcast_to([128, 8, 96]), float(96 ** -0.5), None, ALU.mult, r=[qnw1], w=[qnw])
    kb.copy("dve", knw[:], knw1[:].unsqueeze(1).broadcast_to([128, 8, 96]), r=[knw1], w=[knw])
    rope = kb.sb("rope", [128, 16, 32])
    kb.dma("sp", rope[:], io["rope"].rearrange("(n p) c -> p n c", p=128), r=[], w=rope)

    if DBG < 1:
        return
    TM_SLABS = [[(O_CQ, 416), (O_MG, 16)], [(O_MV, 512)], [(O_GV, 512)]]
    FM_GROUPS = [(O_MX, 128), (O_MX + 128, 128), (O_GQ, 128), (O_GK, 128), (O_GA, 32)]
    NB_ = 2
    xt = [kb.sb("xt%d" % i, [128, 1024]) for i in range(NB_)]
    junk = [kb.sb("junk%d" % i, [128, 1024]) for i in range(NB_)]
    xm = [kb.sb("xm%d" % i, [128, 1024]) for i in range(NB_)]
    xmT = [kb.sb("xmT%d" % i, [128, 8, 128], BF16) for i in range(NB_)]
    htm = [kb.sb("htm%d" % i, [128, 1456]) for i in range(NB_)]
    hfm = [kb.sb("hfm%d" % i, [128, 5, 128]) for i in range(NB_)]
    st1 = [kb.sb("st1_%d" % i, [128, 32]) for i in range(NB_)]
    cqn = [kb.sb("cqn%d" % i, [128, 384]) for i in range(NB_)]
    cT = [kb.sb("cT%d" % i, [128, 3, 128], BF16) for i in range(NB_)]
    qf = [kb.sb("qf%d" % i, [128, 8, 96]) for i in range(NB_)]
    kf = [kb.sb("kf%d" % i, [128, 8, 96]) for i in range(NB_)]
    sq = [kb.sb("sq%d" % i, [128, 8, 96]) for i in range(NB_)]
    rt = [kb.sb("rt%d" % i, [128, 4, 8, 16]) for i in range(NB_)]
    qTs = [kb.sb("qTs%d" % i, [96, 8, 128], BF16) for i in range(NB_)]
    kTs = [kb.sb("kTs%d" % i, [96, 8, 128], BF16) for i in range(NB_)]
    vs = [kb.sb("vs%d" % i, [128, 8, 64], BF16) for i in range(NB_)]

    def head_norm_rope(src, dst_f, sqt, stt_, wbc, ropeidx, rtt, r_extra):
        kb.act(sqt[:], src, AF.Square, r=r_extra, w=[sqt])
        kb.op("dve", lambda e: e.tensor_reduce(out=stt_[:, 0:8], in_=sqt[:], axis=AX.X, op=ALU.add), r=[sqt], w=[stt_])
        rstd_of(kb, stt_[:, 0:8], 8, 96, stt_[:, 8:16], stt_[:, 16:24], r=[stt_], w=[stt_])
        kb.tt(dst_f[:], src, stt_[:, 16:24].unsqueeze(2).broadcast_to([128, 8, 96]), ALU.mult, r=r_extra + [stt_], w=[dst_f])
        kb.tt(dst_f[:], dst_f[:], wbc[:], ALU.mult, r=[dst_f, wbc], w=[dst_f])
        if ropeidx is not None:
            cos = rope[:, ropeidx, 0:16].unsqueeze(1).broadcast_to([128, 8, 16])
            sin = rope[:, ropeidx, 16:32].unsqueeze(1).broadcast_to([128, 8, 16])
            x1 = dst_f[:, :, 64:80]
            x2 = dst_f[:, :, 80:96]
            kb.tt(rtt[:, 0], x1, cos, ALU.mult, r=[dst_f, rope], w=[rtt])
            kb.tt(rtt[:, 1], x2, sin, ALU.mult, r=[dst_f, rope], w=[rtt])
            kb.tt(rtt[:, 2], x1, sin, ALU.mult, r=[dst_f, rope], w=[rtt])
            kb.tt(rtt[:, 3], x2, cos, ALU.mult, r=[dst_f, rope], w=[rtt])
            kb.tt(x1, rtt[:, 0], rtt[:, 1], ALU.subtract, r=[rtt], w=[dst_f])
            kb.tt(x2, rtt[:, 2], rtt[:, 3], ALU.add, r=[rtt], w=[dst_f])

    for ti in range(NT):
        b_ = ti % NB_
        is_ctx = ti >= 16
        mi = 1 if is_ctx else 0
        t0 = ti * 128
        X, XM, XMT, H, HF, ST = xt[b_], xm[b_], xmT[b_], htm[b_], hfm[b_], st1[b_]
        kb.dma("sp", X[:], io["x"][t0:t0 + 128, :], r=[io["x_res"]], w=X)
        kb.act(junk[b_][:], X[:], AF.Square, r=[X], w=[junk[b_], ST], accum_out=ST[:, 0:1])
        rstd_of(kb, ST[:, 0:1], 1, 1024, ST[:, 1:2], ST[:, 2:3], r=[ST], w=[ST])
        kb.stt(XM[:], X[:], ST[:, 2:3], G1[mi][:], ALU.mult, ALU.mult, r=[X, ST, G1[mi]], w=[XM])
        kb.tt(XM[:], XM[:], S1[mi][:], ALU.add, r=[XM, S1[mi]], w=[XM], eng="pool")
        if DBG < 2:
            continue
        for kc in range(8):
            kb.tr(PS[0][:, kc // 4, (kc % 4) * 128:(kc % 4 + 1) * 128], XM[:, kc * 128:(kc + 1) * 128], ident[:], r=[XM, ident], w=[PS[0]])
        kb.copy("act", XMT[:].rearrange("p (a b) t -> p a (b t)", a=2), PS[0][:], r=[PS[0]], w=[XMT])
        pcol = 0
        for si, slab in enumerate(TM_SLABS):
            pst = PS[1] if si < 2 else PS[2]
            bank = si if si < 2 else 0
            off = 0
            for (c0, wd) in slab:
                for kc in range(8):
                    kb.mm(pst[:, bank, off:off + wd], XMT[:, kc, :], w_in[:, kc, c0:c0 + wd], kc == 0, kc == 7, r=[XMT, w_in], w=[pst])
                off += wd
            kb.copy("act" if si != 1 else "dve", H[:, pcol:pcol + off], pst[:, bank, 0:off], r=[pst], w=[H])
            pcol += off
        for gi, (c0, wd) in enumerate(FM_GROUPS):
            for kc in range(8):
                kb.mm(PS[3][0:wd, gi // 4, (gi % 4) * 128:(gi % 4 + 1) * 128], w_in[:, kc, c0:c0 + wd], XMT[:, kc, :], kc == 0, kc == 7, r=[XMT, w_in], w=[PS[3]])
        kb.copy("dve", HF[:, 0:4, :].rearrange("p a t -> p (a t)"), PS[3][:, 0, :], r=[PS[3]], w=[HF])
        kb.copy("dve", HF[0:32, 4, :], PS[3][0:32, 1, 0:128], r=[PS[3]], w=[HF])
        if DBG < 3:
            continue
        kb.dma("sp", io["fm"][0:512, t0:t0 + 128].rearrange("(a p) t -> p a t", p=128), HF[:, 0:4, :], r=[HF], w=io["fm_res"])
        kb.dma("sp", io["fm"][512:544, t0:t0 + 128], HF[0:32, 4, :], r=[HF], w=io["fm_res"])
        kb.dma("sp", io["tm"][t0:t0 + 128, :], H[:, 416:1456], r=[H], w=io["tm_res"])
        if DBG < 4:
            continue
        CQ = cqn[b_]
        kb.act(junk[b_][:, 0:256], H[:, 0:256], AF.Square, r=[H], w=[junk[b_], ST], accum_out=ST[:, 4:5])
        kb.act(junk[b_][:, 256:384], H[:, 256:384], AF.Square, r=[H], w=[junk[b_], ST], accum_out=ST[:, 5:6])
        rstd_of(kb, ST[:, 4:5], 1, 256, ST[:, 6:7], ST[:, 8:9], r=[ST], w=[ST])
        rstd_of(kb, ST[:, 5:6], 1, 128, ST[:, 7:8], ST[:, 9:10], r=[ST], w=[ST])
        kb.stt(CQ[:, 0:256], H[:, 0:256], ST[:, 8:9], qan[:], ALU.mult, ALU.mult, r=[H, ST, qan], w=[CQ])
        kb.stt(CQ[:, 256:384], H[:, 256:384], ST[:, 9:10], kvan[:], ALU.mult, ALU.mult, r=[H, ST, kvan], w=[CQ])
        for kc in range(3):
            kb.tr(PS[2][:, 1, kc * 128:(kc + 1) * 128], CQ[:, kc * 128:(kc + 1) * 128], ident[:], r=[CQ, ident], w=[PS[2]])
        kb.copy("act", cT[b_][:].rearrange("p a t -> p (a t)"), PS[2][:, 1, 0:384], r=[PS[2]], w=[cT[b_]])
        if DBG < 4.1:
            continue
        for s in range(2):
            for kc in range(2):
                kb.mm(PS[0][:, s, 0:384], cT[b_][:, kc, :], w_uq[:, kc, s * 384:(s + 1) * 384], kc == 0, kc == 1, r=[cT[b_], w_uq], w=[PS[0]])
        QF, KF = qf[b_], kf[b_]
        kb.copy("act", QF[:].rearrange("p (s a) d -> p s (a d)", s=2), PS[0][:, :, 0:384], r=[PS[0]], w=[QF])
        if DBG < 4.2:
            continue
        for s in range(2):
            kb.mm(PS[1][:, s, :], cT[b_][:, 2, :], w_ukv[:, s * 512:(s + 1) * 512], True, True, r=[cT[b_], w_ukv], w=[PS[1]])
        kvv = PS[1][:].rearrange("p s (a d) -> p (s a) d", d=128)
        if DBG < 4.21:
            continue
        kb.copy("dve", KF[:, :, 0:64], kvv[:, :, 0:64], r=[PS[1]], w=[KF])
        if DBG < 4.22:
            continue
        for s in range(2):
            kb.copy("dve", vs[b_][:, 4 * s:4 * s + 4, :], PS[1][:, s, :].rearrange("p (a d) -> p a d", d=128)[:, :, 64:128], r=[PS[1]], w=[vs[b_]])
        if DBG < 4.23:
            continue
        kb.copy("dve", KF[:, :, 64:96], H[:, 384:416].unsqueeze(1).broadcast_to([128, 8, 32]), r=[H], w=[KF])
        if DBG < 4.3:
            continue
        ridx = None if is_ctx else ti
        if DBG < 4.4:
            ridx = None
        head_norm_rope(QF[:], QF, sq[b_], ST, qnw, ridx, rt[b_], [QF])
        head_norm_rope(KF[:], KF, sq[b_], ST, knw, ridx, rt[b_], [KF])
        if DBG < 5:
            continue
        for (SRC, DST, dname) in ((QF, qTs[b_], "qT"), (KF, kTs[b_], "kT")):
            for h in range(8):
                kb.tr(PS[3][0:96, h // 4, (h % 4) * 128:(h % 4 + 1) * 128], SRC[:, h, :], ident[:], r=[SRC, ident], w=[PS[3]])
            kb.copy("act", DST[:].rearrange("p (a b) t -> p a (b t)", a=2), PS[3][0:96, :, :], r=[PS[3]], w=[DST])
            kb.dma("sp", io[dname][:, :, t0:t0 + 128].rearrange("h d t -> d h t"), DST[:], r=[DST], w=io[dname + "_res"])
        kb.dma("sp", io["v"][t0:t0 + 128, :], vs[b_][:].rearrange("p h d -> p (h d)"), r=[vs[b_]], w=io["v_res"])


class IO(dict):
    pass


def declare(nc, io, name, shape, dt, kind):
    th = nc.dram_tensor(name, list(shape), dt, kind=kind)
    io[name] = th[:] if len(shape) > 0 else th
    io[name + "_res"] = Res(name)
    return th


CONST_SHAPES = {"ident": [128, 128], "triu": [128, 128], "tril": [128, 128], "sel": [32, 32 * 128]}

A_INPUTS = [("x", [NTOK, D]), ("cc", [128, 8, 2]), ("w_mod", [D, 6 * D]), ("b_mod", [6 * D]), ("norm1_w", [D]),
            ("w_in", [D, D_IN]), ("q_a_norm", [256]), ("w_uq", [256, 768]), ("kv_a_norm", [128]),
            ("w_ukv", [128, 1024]), ("q_norm_w", [96]), ("k_norm_w", [96]), ("rope", [TL, 32])]
A_OUTPUTS = [("qT", [8, 96, NTOK], BF16), ("kT", [8, 96, NTOK], BF16), ("v", [NTOK, 512], BF16),
             ("fm", [544, NTOK], F32), ("tm", [NTOK, 1040], F32)]


def build_A():
    nc = bass.Bass("TRN2", target_bir_lowering=False)
    io = IO()
    for nm, shp in A_INPUTS:
        declare(nc, io, nm, shp, F32, "ExternalInput")
    cd = {}
    for nm in ("ident",):
        th = nc.dram_tensor("c_" + nm, CONST_SHAPES[nm], F32, kind="ExternalInput")
        cd[nm] = T(th, "c_" + nm)
    for nm, shp, dt in A_OUTPUTS:
        declare(nc, io, nm, shp, dt, "ExternalOutput")
    with ExitStack() as st:
        kb = KB(nc, st)
        cst = load_consts(kb, cd, ["ident"])
        phase_A(kb, io, cst)
        kb.finish([io[nm + "_res"] for nm, _, _ in A_OUTPUTS])
        print("phase A: n_inst", kb.n_inst, "n_wait", kb.n_wait, "dsems", kb.ndsem)
    return nc


_ROPE = None
_CONSTS = None


def consts():
    global _ROPE, _CONSTS
    if _CONSTS is None:
        _CONSTS = host_consts()
        _ROPE = rope_tables()
    return _CONSTS, _ROPE


def f32(a):
    return np.ascontiguousarray(a, dtype=np.float32)


def cc_layout(c_b, c_ctx):
    cc = np.stack([c_b, c_ctx], axis=-1)
    return f32(cc.reshape(8, 128, 2).transpose(1, 0, 2))


def core_tokens(xl, xc, core):
    b, j = core // 4, core % 4
    return np.concatenate([xl[b, j * TL:(j + 1) * TL], xc[b]], axis=0)


def a_inmaps(inp, l, xl, xc):
    cst, rope = consts()
    maps = []
    for core in range(NCORE):
        b, j = core // 4, core % 4
        m = {"x": f32(core_tokens(xl, xc, core)), "cc": cc_layout(inp["c"][b], inp["c_ctx"]),
             "rope": f32(rope[j * TL:(j + 1) * TL]), "c_ident": cst["ident"]}
        for nm in ("w_mod", "b_mod", "norm1_w", "w_in", "q_a_norm", "w_uq", "kv_a_norm", "w_ukv", "q_norm_w", "k_norm_w"):
            m[nm] = f32(inp[nm][l])
        maps.append(m)
    return maps


B_T = SEQ + NCTX
NCH = B_T // 128
SEQ_F = [64, 65] + list(range(64))
SEQ_B = list(range(65, -1, -1))


def nsl(ns):
    if len(ns) == 1:
        return slice(ns[0], ns[0] + 1)
    if ns[1] == ns[0] + 1:
        return slice(ns[0], ns[-1] + 1)
    stop = ns[-1] - 1
    return slice(ns[0], stop if stop >= 0 else None, -1)


def seq_groups(seq, g):
    groups, cur = [], []
    for n in seq:
        if cur and (len(cur) == g or abs(n - cur[-1]) != 1 or (len(cur) >= 2 and (n - cur[-1]) != (cur[-1] - cur[-2]))):
            groups.append(cur)
            cur = []
        cur.append(n)
    groups.append(cur)
    return groups


def attention_B(kb, io, need_ctx):
    kb.push()
    KT = kb.sb("KT", [96, 2, B_T], BF16)
    QT = kb.sb("QT", [96, 2, SEQ], BF16)
    QC = kb.sb("QC", [96, 2, 256], BF16)
    Vst = kb.sb("Vst", [128, NCH, 128], BF16)
    V = kb.sb("V", [128, NCH, 2, 66], BF16)
    E = kb.sb("E", [65, 64], BF16)
    for hh in range(2):
        kb.dma("sp", KT[:, hh, :], io["KT"][hh], r=[], w=KT)
        kb.dma("sp", QT[:, hh, :], io["QT"][hh], r=[], w=QT)
        kb.dma("sp", QC[:, hh, :], io["QcT"][hh], r=[], w=QC)
    vsrc = io["V"]
    for i in range(0, NCH, 11):
        kb.dma("sp", Vst[:, i:i + 11, :], vsrc[:, i:i + 11, :], r=[], w=Vst)
    cf = kb.sb("cf", [128, 512])
    kb.op("dve", lambda e: e.memset(cf[:], 1.0), r=[], w=[cf])
    kb.copy("dve", V[:, :, :, 64:65], cf[:, 0:NCH * 2].rearrange("p (n h o) -> p n h o", h=2, o=1), r=[cf], w=[V])
    kb.copy("dve", V[:, :, :, 0:64], Vst[:].rearrange("p n (h d) -> p n h d", h=2), r=[Vst], w=[V])
    Ef = kb.sb("Ef", [65, 64])
    kb.op("dve", lambda e: e.memset(Ef[:], 0.0), r=[], w=[Ef])
    kb.op("dve", lambda e: e.memset(Ef[64:65, :], 1.0), r=[], w=[Ef])
    kb.copy("dve", E[:], Ef[:], r=[Ef], w=[E])
    zf = kb.sb("zf", [65, 512])
    kb.op("dve", lambda e: e.memset(zf[:], 0.0), r=[], w=[zf])
    pst = [kb.ps("pst%d" % i, [128, 512]) for i in range(2)]
    pot = [kb.ps("pot%d" % i, [128, 512]) for i in range(2)]
    pbc = kb.ps("pbc", [128, 512])
    PT = [kb.sb("PT%d" % i, [128, 512], BF16) for i in range(3)]
    osb = [kb.sb("osb%d" % i, [65, 512]) for i in range(2)]
    rd = [kb.sb("rd%d" % i, [65, 512]) for i in range(2)]
    rh = [kb.sb("rh%d" % i, [65, 512], BF16) for i in range(2)]
    rl = [kb.sb("rl%d" % i, [65, 512], BF16) for i in range(2)]
    for t in rh + rl:
        kb.copy("dve", t[:], zf[:], r=[zf], w=[t])
    oT = [kb.sb("oT%d" % i, [64, 512], BF16) for i in range(2)]
    for t in rd:
        kb.op("dve", lambda e: e.memset(t[:], 0.0), r=[], w=[t])
    cnt = {"it": 0, "blk": 0}

    pending = []

    def finalize(po, ob, rdt, rht, rlt, ot, nq, out_ap):
        kb.copy("act", ob[:, 0:nq], po[0:65, 0:nq], r=[po], w=[ob])
        kb.op("dve", lambda e: e.reciprocal(out=rdt[64:65, 0:nq], in_=ob[64:65, 0:nq]), r=[ob], w=[rdt])
        kb.copy("dve", rht[64:65, 0:nq], rdt[64:65, 0:nq], r=[rdt], w=[rht])
        kb.tt(rlt[64:65, 0:nq], rdt[64:65, 0:nq], rht[64:65, 0:nq], ALU.subtract, r=[rdt, rht], w=[rlt])
        kb.mm(pbc[0:64, 0:nq], E[:, :], rht[:, 0:nq], True, False, r=[E, rht], w=[pbc])
        kb.mm(pbc[0:64, 0:nq], E[:, :], rlt[:, 0:nq], False, True, r=[E, rlt], w=[pbc])
        kb.tt(ot[:, 0:nq], pbc[0:64, 0:nq], ob[0:64, 0:nq], ALU.mult, r=[ob, pbc], w=[ot])
        kb.dma("sp", out_ap, ot[:, 0:nq], r=[ot], w=io["mixT_res"])

    def block(q_ap, qres, nq, key_tiles, hh, out_ap):
        k_ = cnt["blk"] % 2
        po, ob, rdt, rht, rlt, ot = pot[k_], osb[k_], rd[k_], rh[k_], rl[k_], oT[k_]
        cnt["blk"] += 1
        nk = len(key_tiles)
        prev = None
        for i, kt in enumerate(key_tiles):
            ps_ = pst[cnt["it"] % 2]
            pt = PT[cnt["it"] % 3]
            cnt["it"] += 1
            kb.mm(ps_[:, 0:nq], KT[:, hh, kt * 128:(kt + 1) * 128], q_ap, True, True, r=[KT, qres], w=[ps_])
            kb.act(pt[:, 0:nq], ps_[:, 0:nq], AF.Exp, r=[ps_], w=[pt])
            if prev is not None:
                pi, pkt, ppt = prev
                kb.mm(po[0:65, 0:nq], V[:, pkt, hh, 0:65], ppt[:, 0:nq], pi == 0, False, r=[V, ppt], w=[po])
            prev = (i, kt, pt)
            if i == 2 and pending:
                finalize(*pending.pop())
        pi, pkt, ppt = prev
        kb.mm(po[0:65, 0:nq], V[:, pkt, hh, 0:65], ppt[:, 0:nq], pi == 0, True, r=[V, ppt], w=[po])
        if pending:
            finalize(*pending.pop())
        pending.append((po, ob, rdt, rht, rlt, ot, nq, out_ap))

    for hh in range(2):
        for qb in range(SEQ // 512):
            block(QT[:, hh, qb * 512:(qb + 1) * 512], QT, 512, list(range(NCH)), hh,
                  io["mixT"][hh * 64:(hh + 1) * 64, qb * 512:(qb + 1) * 512])
        if need_ctx:
            block(QC[:, hh, :], QC, 256, [64, 65], hh, io["mixT"][hh * 64:(hh + 1) * 64, SEQ:B_T])
    while pending:
        finalize(*pending.pop())
    kb.pop()


def lin_attn(kb, cst, dk, nv, qT, kT, ktok, vp, a, a_on_G, dirn, emit):
    kb.push()
    seq = SEQ_F if dirn == 0 else SEQ_B
    mask = cst["triu"] if dirn == 0 else cst["tril"]
    Gs = kb.sb("Gs", [dk, nv, NCH])
    Ar = kb.sb("Ar", [dk, nv, NCH])
    Cs = kb.sb("Cs", [dk, nv, NCH], BF16)
    pg = [kb.ps("pg%d" % i, [128, 512]) for i in range(2)]
    pp = [kb.ps("pp%d" % i, [128, 512]) for i in range(2)]
    po = [kb.ps("po%d" % i, [128, 512]) for i in range(2)]
    PTm = [kb.sb("PTm%d" % i, [128, 128], BF16) for i in range(3)]
    gsz = 512 // nv
    for gi, s0 in enumerate(range(0, NCH, gsz)):
        pgt = pg[gi % 2]
        ss = list(range(s0, min(NCH, s0 + gsz)))
        for i, s in enumerate(ss):
            n = seq[s]
            kb.mm(pgt[0:dk, i * nv:(i + 1) * nv], ktok[:, n, :], vp[:, n, :], True, True, r=[ktok, vp], w=[pgt])
        if DBG < 10.6:
            continue
        for i, s in enumerate(ss):
            n = seq[s]
            if a_on_G:
                kb.tt(Gs[:, :, s], pgt[0:dk, i * nv:(i + 1) * nv], a[:, n:n + 1].broadcast_to([dk, nv]), ALU.mult, r=[pgt, a], w=[Gs])
            else:
                kb.copy("dve", Gs[:, :, s], pgt[0:dk, i * nv:(i + 1) * nv], r=[pgt], w=[Gs])
    if DBG < 10.61:
        kb.pop()
        return
    if dirn == 0:
        kb.copy("pool", Ar[:, :, 2:NCH], a[:, 0:64].unsqueeze(1).broadcast_to([dk, nv, 64]), r=[a], w=[Ar])
        kb.copy("pool", Ar[:, :, 0:2], a[:, 64:66].unsqueeze(1).broadcast_to([dk, nv, 2]), r=[a], w=[Ar])
    else:
        kb.copy("pool", Ar[:], a[:, ::-1].unsqueeze(1).broadcast_to([dk, nv, NCH]), r=[a], w=[Ar])
    kb.op("pool", lambda e: e.memset(Ar[:, :, 0:1], 0.0), r=[], w=[Ar])
    kb.op("dve", lambda e: e.tensor_tensor_scan(out=Cs[:].rearrange("p v s -> p (v s)"), data0=Ar[:].rearrange("p v s -> p (v s)"),
                                                 data1=Gs[:].rearrange("p v s -> p (v s)"), initial=0.0, op0=ALU.mult, op1=ALU.add),
          r=[Ar, Gs], w=[Cs])
    if DBG < 10.62:
        kb.pop()
        return
    it = 0

    def issue_pv(pot, i, n, ptm):
        s = seq.index(n)
        t0 = n * 128
        kb.mm(pot[:, i * nv:(i + 1) * nv], ptm[:], vp[:, n, :], True, s == 0, r=[ptm, vp], w=[pot])
        if s > 0:
            kb.mm(pot[:, i * nv:(i + 1) * nv], qT[:, t0:t0 + 128], Cs[:, :, s - 1], False, True, r=[qT, Cs], w=[pot])

    for gi, ns in enumerate(seq_groups(seq, gsz)):
        pot = po[gi % 2]
        prev = None
        for i, n in enumerate(ns):
            t0 = n * 128
            ppt = pp[it % 2]
            ptm = PTm[it % 3]
            it += 1
            kb.mm(ppt[:, 0:128], kT[:, t0:t0 + 128], qT[:, t0:t0 + 128], True, True, r=[kT, qT], w=[ppt])
            kb.tt(ptm[:], ppt[:, 0:128], mask[:], ALU.mult, r=[ppt, mask], w=[ptm])
            if prev is not None:
                issue_pv(pot, *prev)
            prev = (i, n, ptm)
        issue_pv(pot, *prev)
        emit(pot, ns)
    kb.pop()


def out_norm_gate(kb, cst, hsum, nw_d, gate_d, gate_func, extra, mix_rows, io, name):
    kb.push()
    nw = bcast_load(kb, name + "nw", nw_d, 64)
    gt = kb.sb(name + "gt", [128, NCH, 64])
    kb.dma("sp", gt[:], gate_d, r=[], w=gt)
    sq = kb.sb(name + "sq", [128, NCH, 64])
    st = kb.sb(name + "st", [128, 3, NCH])
    kb.act(sq[:], hsum[:], AF.Square, r=[hsum], w=[sq])
    kb.op("dve", lambda e: e.tensor_reduce(out=st[:, 0, :], in_=sq[:], axis=AX.X, op=ALU.add), r=[sq], w=[st])
    rstd_of(kb, st[:, 0, :], NCH, 64, st[:, 1, :], st[:, 2, :], r=[st], w=[st])
    if DBG < 10.71:
        kb.pop()
        return
    kb.tt(hsum[:], hsum[:], st[:, 2, :].unsqueeze(2).broadcast_to([128, NCH, 64]), ALU.mult, r=[hsum, st], w=[hsum])
    kb.tt(hsum[:], hsum[:], nw[:].unsqueeze(1).broadcast_to([128, NCH, 64]), ALU.mult, r=[hsum, nw], w=[hsum])
    if extra is not None:
        kb.tt(hsum[:], hsum[:], extra[:], ALU.add, r=[hsum, extra], w=[hsum])
    kb.act(gt[:], gt[:], gate_func, r=[gt], w=[gt])
    kb.tt(hsum[:], hsum[:], gt[:], ALU.mult, r=[hsum, gt], w=[hsum])
    if DBG < 10.72:
        kb.pop()
        return
    oT = kb.sb(name + "oT", [64, B_T], BF16)
    ptr = [kb.ps(name + "ptr%d" % i, [128, 512]) for i in range(2)]
    for gi, n0 in enumerate(range(0, NCH, 4)):
        nn = min(4, NCH - n0)
        p = ptr[gi % 2]
        for i in range(nn):
            kb.tr(p[0:64, i * 128:(i + 1) * 128], hsum[:, n0 + i, :], cst["ident"][:], r=[hsum, cst["ident"]], w=[p])
        kb.copy("act", oT[:, n0 * 128:(n0 + nn) * 128], p[0:64, 0:nn * 128], r=[p], w=[oT])
    if DBG < 10.73:
        kb.pop()
        return
    for i in range(4):
        kb.dma("sp", io["mixT"][mix_rows:mix_rows + 64, i * 2112:(i + 1) * 2112], oT[:, i * 2112:(i + 1) * 2112], r=[oT], w=io["mixT_res"])
    kb.pop()


def mlstm_B(kb, io, cst):
    kb.push()
    ident = cst["ident"]
    qT = kb.sb("mqT", [64, B_T], BF16)
    kT = kb.sb("mkT", [64, B_T], BF16)
    ktok = kb.sb("mktok", [128, NCH, 64], BF16)
    xtok = kb.sb("mxtok", [128, NCH, 64])
    kb.push()
    mx = kb.sb("mx", [64, B_T])
    acc = kb.sb("macc", [64, B_T])
    xcb = kb.sb("mxcb", [64, B_T], BF16)
    cw = kb.sb("cw", [64, 5])
    cb = kb.sb("cb", [64, 1])
    wq = kb.sb("wq", [64, 64], BF16)
    wk = kb.sb("wk", [64, 64], BF16)
    kb.dma("sp", cw[:], io["cw"], r=[], w=cw)
    kb.dma("sp", cb[:], io["cb"], r=[], w=cb)
    kb.dma("pool", wq[:], io["wq"], r=[], w=wq)
    kb.dma("pool", wk[:], io["wk"], r=[], w=wk)
    for i in range(4):
        kb.dma("sp", mx[:, i * 2112:(i + 1) * 2112], io["mxT"][:, i * 2112:(i + 1) * 2112], r=[], w=mx)
    for (s0, ln) in ((0, SEQ), (SEQ, NCTX)):
        kb.ts(acc[:, s0:s0 + ln], mx[:, s0:s0 + ln], cw[:, 2:3], None, ALU.mult, r=[mx, cw], w=[acc])
        for k in (0, 1, 3, 4):
            sh = k - 2
            a0 = max(0, -sh)
            a1 = ln - max(0, sh)
            kb.stt(acc[:, s0 + a0:s0 + a1], mx[:, s0 + a0 + sh:s0 + a1 + sh], cw[:, k:k + 1], acc[:, s0 + a0:s0 + a1],
                   ALU.mult, ALU.add, r=[mx, cw, acc], w=[acc])
    if DBG < 10.1:
        return
    kb.act(acc[:], acc[:], AF.Silu, r=[acc, cb], w=[acc], bias=cb[:, 0:1])
    kb.copy("dve", xcb[:], acc[:], r=[acc], w=[xcb])
    if DBG < 10.2:
        return
    pj = [kb.ps("mpj%d" % i, [128, 512]) for i in range(2)]
    gi = 0
    for c0 in range(0, B_T, 512):
        w_ = min(512, B_T - c0)
        p = pj[gi % 2]; gi += 1
        kb.mm(p[0:64, 0:w_], wq[:], xcb[:, c0:c0 + w_], True, True, r=[wq, xcb], w=[p])
        kb.op("act", lambda e: e.mul(qT[:, c0:c0 + w_], p[0:64, 0:w_], 0.125), r=[p], w=[qT])
        p = pj[gi % 2]; gi += 1
        kb.mm(p[0:64, 0:w_], wk[:], xcb[:, c0:c0 + w_], True, True, r=[wk, xcb], w=[p])
        kb.copy("dve", kT[:, c0:c0 + w_], p[0:64, 0:w_], r=[p], w=[kT])
    if DBG < 10.3:
        return
    for n0 in range(0, NCH, 8):
        nn = min(8, NCH - n0)
        p = pj[gi % 2]; gi += 1
        for i in range(nn):
            n = n0 + i
            kb.mm(p[:, i * 64:(i + 1) * 64], xcb[:, n * 128:(n + 1) * 128], wk[:], True, True, r=[xcb, wk], w=[p])
        kb.copy("dve", ktok[:, n0:n0 + nn, :].rearrange("p n d -> p (n d)"), p[:, 0:nn * 64], r=[p], w=[ktok])
        p = pj[gi % 2]; gi += 1
        for i in range(nn):
            n = n0 + i
            kb.tr(p[:, i * 64:(i + 1) * 64], acc[:, n * 128:(n + 1) * 128], ident[0:64, 0:64], r=[acc, ident], w=[p])
        kb.copy("act", xtok[:, n0:n0 + nn, :].rearrange("p n d -> p (n d)"), p[:, 0:nn * 64], r=[p], w=[xtok])
    kb.pop()
    if DBG < 10.4:
        return
    mg = kb.sb("mg", [128, NCH, 4])
    kb.dma("sp", mg[:], io["mg"], r=[], w=mg)
    gb = bcast_load(kb, "gb", io["gb"], 4)
    gp = kb.sb("gp", [128, 4, NCH])
    for g in range(4):
        kb.ts(gp[:, g, :], mg[:, :, g], gb[:, g:g + 1], None, ALU.add, r=[mg, gb], w=[gp])
    if DBG < 10.41:
        return
    lf = kb.sb("lf", [128, 2, NCH])
    for d in range(2):
        kb.act(lf[:, d, :], gp[:, 1 + 2 * d, :], AF.Sigmoid, r=[gp], w=[lf])
    kb.act(lf[:], lf[:], AF.Ln, r=[lf], w=[lf])
    if DBG < 10.42:
        return
    ones = kb.sb("ones", [128, 64], BF16)
    onesf = kb.sb("onesf", [128, 64])
    kb.op("dve", lambda e: e.memset(onesf[:], 1.0), r=[], w=[onesf])
    kb.copy("dve", ones[:], onesf[:], r=[onesf], w=[ones])
    lfh = kb.sb("lfh", [128, 2, NCH], BF16)
    lfl = kb.sb("lfl", [128, 2, NCH], BF16)
    kb.copy("dve", lfh[:], lf[:], r=[lf], w=[lfh])
    if DBG < 10.421:
        return
    kb.tt(lfl[:], lf[:], lfh[:], ALU.subtract, r=[lf, lfh], w=[lfl])
    if DBG < 10.422:
        return
    pgt = kb.ps("mpgate", [128, 4, 128])
    rr = kb.sb("mr", [128, 2, NCH])
    uu = kb.sb("mu", [128, 2, NCH])
    aa = [kb.sb("ma%d" % d, [64, NCH]) for d in range(2)]
    for d in range(2):
        tri = cst["triu_b"] if d == 0 else cst["tril_b"]
        kb.mm(pgt[:, d, 0:NCH], tri[:], lfh[:, d, :], True, False, r=[tri, lfh], w=[pgt])
        kb.mm(pgt[:, d, 0:NCH], tri[:], lfl[:, d, :], False, True, r=[tri, lfl], w=[pgt])
        if DBG < 10.423:
            continue
        kb.mm(pgt[0:64, 2 + d, 0:NCH], ones[:], lfh[:, d, :], True, False, r=[ones, lfh], w=[pgt])
        kb.mm(pgt[0:64, 2 + d, 0:NCH], ones[:], lfl[:, d, :], False, True, r=[ones, lfl], w=[pgt])
    if DBG < 10.43:
        return
    for d in range(2):
        kb.act(rr[:, d, :], pgt[:, d, 0:NCH], AF.Exp, r=[pgt], w=[rr])
        if DBG < 10.432:
            continue
        kb.stt(uu[:, d, :], pgt[:, d, 0:NCH], -1.0, gp[:, 2 * d, :], ALU.mult, ALU.add, r=[gp, pgt], w=[uu])
        if DBG < 10.433:
            continue
        kb.act(aa[d][:], pgt[0:64, 2 + d, 0:NCH], AF.Exp, r=[pgt], w=[aa[d]])
    if DBG < 10.44:
        return
    kb.act(uu[:], uu[:], AF.Exp, r=[uu], w=[uu])
    if DBG < 10.5:
        return
    vaug = kb.sb("vaug", [128, NCH, 66])
    kb.op("dve", lambda e: e.memset(vaug[:], 1.0), r=[], w=[vaug])
    if DBG < 10.501:
        return
    hsum = kb.sb("hsum", [128, NCH, 64])
    kb.dma("sp", hsum[:], io["mv"], r=[], w=hsum)
    if DBG < 10.502:
        return
    kb.copy("dve", vaug[:, :, 0:64], hsum[:], r=[hsum], w=[vaug])
    if DBG < 10.51:
        return
    vp = kb.sb("vp", [128, NCH, 66], BF16)
    vtmp = kb.sb("vtmp", [128, NCH, 66])
    dt_ = kb.sb("mdt", [128, 4, 8])
    htmp = kb.sb("htmp", [128, 7, 64])
    for d in range(2):
        kb.tt(vtmp[:], vaug[:], uu[:, d, :].unsqueeze(2).broadcast_to([128, NCH, 66]), ALU.mult, r=[vaug, uu], w=[vtmp])
        kb.copy("dve", vp[:], vtmp[:], r=[vtmp], w=[vp])

        def emit(pot, ns, d=d):
            g = len(ns)
            sl = nsl(ns)
            pv = pot[:, 0:g * 66].rearrange("p (g v) -> p g v", v=66)
            kb.tt(dt_[:, 0, 0:g], pv[:, :, 64], rr[:, d, sl], ALU.mult, r=[pot, rr], w=[dt_])
            kb.ts(dt_[:, 1, 0:g], dt_[:, 0, 0:g], -1.0, None, ALU.mult, r=[dt_], w=[dt_])
            kb.tt(dt_[:, 1, 0:g], dt_[:, 1, 0:g], dt_[:, 0, 0:g], ALU.max, r=[dt_], w=[dt_])
            kb.ts(dt_[:, 1, 0:g], dt_[:, 1, 0:g], 1.0, None, ALU.max, r=[dt_], w=[dt_])
            kb.op("dve", lambda e: e.reciprocal(out=dt_[:, 2, 0:g], in_=dt_[:, 1, 0:g]), r=[dt_], w=[dt_])
            kb.tt(dt_[:, 3, 0:g], dt_[:, 2, 0:g], rr[:, d, sl], ALU.mult, r=[dt_, rr], w=[dt_])
            if d == 0:
                kb.tt(hsum[:, sl, :], pv[:, :, 0:64], dt_[:, 3, 0:g].unsqueeze(2).broadcast_to([128, g, 64]), ALU.mult, r=[pot, dt_], w=[hsum])
            else:
                kb.tt(htmp[:, 0:g, :], pv[:, :, 0:64], dt_[:, 3, 0:g].unsqueeze(2).broadcast_to([128, g, 64]), ALU.mult, r=[pot, dt_], w=[htmp])
                kb.tt(hsum[:, sl, :], hsum[:, sl, :], htmp[:, 0:g, :], ALU.add, r=[hsum, htmp], w=[hsum], eng="pool")
        if DBG < 10.52:
            continue
        lin_attn(kb, cst, 64, 66, qT, kT, ktok, vp, aa[d], True, d, emit)
    if DBG < 10.7:
        return
    sk = bcast_load(kb, "msk", io["msk"], 64)
    kb.tt(xtok[:], xtok[:], sk[:].unsqueeze(1).broadcast_to([128, NCH, 64]), ALU.mult, r=[xtok, sk], w=[xtok])
    out_norm_gate(kb, cst, hsum, io["mnw"], io["mo"], AF.Sigmoid, xtok, 128, io, "mo")
    kb.pop()


def gla_B(kb, io, cst):
    kb.push()
    ident = cst["ident"]
    gvb = kb.sb("gvb", [128, NCH, 64], BF16)
    osum = kb.sb("osum", [128, NCH, 64])
    kb.dma("pool", gvb[:], io["gv"], r=[], w=gvb)
    ba = kb.sb("ba", [32, 2])
    kb.dma("sp", ba[:], io["ba"], r=[], w=ba)
    NP = 22
    rst = kb.sb("rst", [32, NP, 128])
    kb.op("pool", lambda e: e.memset(rst[:], 1.0), r=[], w=[rst])
    kb.op("pool", lambda e: e.memset(rst[:, :, 0:1], 0.0), r=[], w=[rst])
    qTt = kb.sb("gqT", [32, B_T], BF16)
    kTt = kb.sb("gkT", [32, B_T], BF16)
    ktok = kb.sb("gktok", [128, NCH, 32], BF16)
    a = kb.sb("ga", [32, NCH])
    for d in range(2):
        wa = kb.sb("wa%d" % d, [16, 32], BF16)
        kb.dma("pool", wa[:], io["wa"][d], r=[], w=wa)
        for n0 in range(0, NCH, NP):
            kb.push()
            c0 = n0 * 128
            cw_ = NP * 128
            ga = kb.sb("gain", [16, cw_], BF16)
            gq = kb.sb("gq", [32, cw_])
            gk = kb.sb("gk", [32, cw_])
            kb.dma("pool", ga[:], io["gaT"][d][:, c0:c0 + cw_], r=[], w=ga)
            kb.dma("sp", gq[:], io["gqT"][:, c0:c0 + cw_], r=[], w=gq)
            kb.dma("sp", gk[:], io["gkT"][:, c0:c0 + cw_], r=[], w=gk)
            la = kb.sb("la", [32, NP, 128])
            P = kb.sb("P", [32, NP, 128])
            Dm = kb.sb("Dm", [32, NP, 128])
            Ex = kb.sb("Ex", [32, NP, 128])
            khat = kb.sb("khat", [32, NP, 128])
            laf = la[:].rearrange("p n t -> p (n t)")
            Exf = Ex[:].rearrange("p n t -> p (n t)")
            pp = [kb.ps("gpp%d" % i, [128, 512]) for i in range(2)]
            for gi, x0 in enumerate(range(0, cw_, 512)):
                w_ = min(512, cw_ - x0)
                p = pp[gi % 2]
                kb.mm(p[0:32, 0:w_], wa[:], ga[:, x0:x0 + w_], True, True, r=[wa, ga], w=[p])
                kb.act(laf[:, x0:x0 + w_], p[0:32, 0:w_], AF.Sigmoid, r=[p, ba], w=[la], bias=ba[:, d:d + 1])
            kb.act(la[:], la[:], AF.Ln, r=[la], w=[la])
            kb.op("dve", lambda e: e.tensor_tensor_scan(out=P[:].rearrange("p n t -> p (n t)"), data0=rst[:].rearrange("p n t -> p (n t)"),
                                                         data1=laf, initial=0.0, op0=ALU.mult, op1=ALU.add), r=[rst, la], w=[P])
            kb.act(a[:, n0:n0 + NP], P[:, :, 127], AF.Exp, r=[P], w=[a], scale=1.0 / 16)
            kb.tt(Dm[:], P[:, :, 127:128].broadcast_to([32, NP, 128]), P[:], ALU.subtract, r=[P], w=[Dm])
            if d == 0:
                Bq = P
                Bke = Dm
            else:
                kb.tt(Dm[:], Dm[:], la[:], ALU.add, r=[Dm, la], w=[Dm])
                kb.tt(P[:], P[:], la[:], ALU.subtract, r=[P, la], w=[P])
                Bq = Dm
                Bke = P
            kb.act(Ex[:], Bq[:], AF.Exp, r=[Bq], w=[Ex], scale=1.0 / 16)
            kb.stt(qTt[:, c0:c0 + cw_], gq[:], float(32 ** -0.5), Exf, ALU.mult, ALU.mult, r=[gq, Ex], w=[qTt])
            kb.act(Ex[:], Bq[:], AF.Exp, r=[Bq], w=[Ex], scale=-1.0 / 16)
            kb.tt(kTt[:, c0:c0 + cw_], gk[:], Exf, ALU.mult, r=[gk, Ex], w=[kTt])
            kb.act(Ex[:], Bke[:], AF.Exp, r=[Bke], w=[Ex], scale=1.0 / 16)
            kb.tt(khat[:].rearrange("p n t -> p (n t)"), gk[:], Exf, ALU.mult, r=[gk, Ex], w=[khat])
            for gi, m0 in enumerate(range(0, NP, 16)):
                nn = min(16, NP - m0)
                p = pp[gi % 2]
                for i in range(nn):
                    kb.tr(p[:, i * 32:(i + 1) * 32], khat[:, m0 + i, :], ident[0:32, 0:32], r=[khat, ident], w=[p])
                kb.copy("dve", ktok[:, n0 + m0:n0 + m0 + nn, :].rearrange("p n d -> p (n d)"), p[:, 0:nn * 32], r=[p], w=[ktok])
            kb.pop()

        def emit(pot, ns, d=d):
            g = len(ns)
            sl = nsl(ns)
            pv = pot[:, 0:g * 64].rearrange("p (g v) -> p g v", v=64)
            if d == 0:
                kb.copy("dve", osum[:, sl, :], pv, r=[pot], w=[osum])
            else:
                kb.tt(osum[:, sl, :], pv, osum[:, sl, :], ALU.add, r=[osum, pot], w=[osum])
        lin_attn(kb, cst, 32, 64, qTt, kTt, ktok, gvb, a, False, d, emit)
    out_norm_gate(kb, cst, osum, io["gnw"], io["gr"], AF.Silu, None, 192, io, "go")
    kb.pop()


B_INPUTS = [("QT", [2, 96, SEQ], BF16), ("QcT", [2, 96, NCTX], BF16), ("KT", [2, 96, B_T], BF16), ("V", [128, NCH, 128], BF16),
            ("mxT", [64, B_T], F32), ("gqT", [32, B_T], F32), ("gkT", [32, B_T], F32), ("gaT", [2, 16, B_T], F32),
            ("mg", [128, NCH, 4], F32), ("mv", [128, NCH, 64], F32), ("mo", [128, NCH, 64], F32), ("gv", [128, NCH, 64], F32), ("gr", [128, NCH, 64], F32),
            ("cw", [64, 5], F32), ("cb", [64, 1], F32), ("wq", [64, 64], F32), ("wk", [64, 64], F32), ("gb", [4], F32),
            ("mnw", [64], F32), ("msk", [64], F32), ("wa", [2, 16, 32], F32), ("ba", [32, 2], F32), ("gnw", [64], F32)]


def build_B(need_ctx=True, parts=("attn", "mlstm", "gla")):
    nc = bass.Bass("TRN2", target_bir_lowering=False)
    io = IO()
    for nm, shp, dt in B_INPUTS:
        declare(nc, io, nm, shp, dt, "ExternalInput")
    cd = {}
    for nm in ("ident", "triu", "tril"):
        th = nc.dram_tensor("c_" + nm, CONST_SHAPES[nm], F32, kind="ExternalInput")
        cd[nm] = T(th, "c_" + nm)
    declare(nc, io, "mixT", [256, B_T], BF16, "ExternalOutput")
    with ExitStack() as st:
        kb = KB(nc, st)
        cst = load_consts(kb, cd, ["ident", "triu", "tril"])
        if "mlstm" in parts:
            mlstm_B(kb, io, cst)
        if "gla" in parts:
            gla_B(kb, io, cst)
        if "attn" in parts:
            attention_B(kb, io, need_ctx)
        while len(kb.stacks) > 1:
            kb.stacks.pop().close()
        kb.finish([io["mixT_res"]])
        print("phase B: n_inst", kb.n_inst, "n_wait", kb.n_wait, "dsems", kb.ndsem)
    return nc


def b_inmaps(inp, l, aout):
    cst, _ = consts()
    maps = []
    for b in range(NB):
        cs = [aout[4 * b + j] for j in range(4)]

        def cat_t(name, axis):
            parts = [np.take(c[name], np.arange(0, TL), axis=axis) for c in cs]
            parts.append(np.take(cs[0][name], np.arange(TL, NTOK), axis=axis))
            return np.concatenate(parts, axis=axis)
        qT = cat_t("qT", 2)
        kT = cat_t("kT", 2)
        v = cat_t("v", 0)
        fm = cat_t("fm", 1)
        tm = cat_t("tm", 0)
        for j in range(4):
            def pm(a):
                return np.ascontiguousarray(a.reshape(NCH, 128, a.shape[-1]).transpose(1, 0, 2))
            m = {"QT": np.ascontiguousarray(qT[2 * j:2 * j + 2, :, 0:SEQ]), "QcT": np.ascontiguousarray(qT[2 * j:2 * j + 2, :, SEQ:]),
                 "KT": np.ascontiguousarray(kT[2 * j:2 * j + 2]), "V": pm(v[:, 128 * j:128 * j + 128]),
                 "mxT": f32(fm[64 * j:64 * j + 64]), "gqT": f32(fm[256 + 32 * j:256 + 32 * j + 32]),
                 "gkT": f32(fm[384 + 32 * j:384 + 32 * j + 32]), "gaT": f32(fm[512:544].reshape(2, 16, B_T)),
                 "mg": pm(f32(tm[:, [j, 4 + j, 8 + j, 12 + j]])), "mv": pm(f32(tm[:, 16 + 64 * j:16 + 64 * j + 64])),
                 "mo": pm(f32(tm[:, 272 + 64 * j:272 + 64 * j + 64])), "gv": pm(f32(tm[:, 528 + 64 * j:528 + 64 * j + 64])),
                 "gr": pm(f32(tm[:, 784 + 64 * j:784 + 64 * j + 64])),
                 "cw": f32(inp["ml_conv_w"][l][:, 64 * j:64 * j + 64].T), "cb": f32(inp["ml_conv_b"][l][64 * j:64 * j + 64][:, None]),
                 "wq": f32(inp["ml_wq"][l][j]), "wk": f32(inp["ml_wk"][l][j]),
                 "gb": f32(inp["ml_gate_b"][l][[j, 4 + j, 8 + j, 12 + j]]),
                 "mnw": f32(inp["ml_norm_w"][l][64 * j:64 * j + 64]), "msk": f32(inp["ml_skip"][l][64 * j:64 * j + 64]),
                 "wa": f32(inp["gla_wa"][l][:, :, 32 * j:32 * j + 32]), "ba": f32(inp["gla_ba"][l][:, 32 * j:32 * j + 32].T),
                 "gnw": f32(inp["gla_norm_w"][l][64 * j:64 * j + 64]),
                 "c_ident": cst["ident"], "c_triu": cst["triu"], "c_tril": cst["tril"]}
            maps.append(m)
    return maps


def phase_C(kb, io, cst):
    ident = cst["ident"]
    NE = 32
    x1 = kb.sb("x1_all", [128, NT, 1024])
    h2T = kb.sb("h2T_all", [128, 8, NTOK], BF16)
    gTh = kb.sb("gTh", [32, NTOK], BF16)
    gTl = kb.sb("gTl", [32, NTOK], BF16)
    selb = kb.sb("selb", [32, NE * 128], BF16)
    kb.dma("pool", selb[:], cst["sel_d"], r=[], w=selb)
    gate2 = [kb.sb("gate2_%d" % i, [128, 1024]) for i in range(2)]
    kb.push()
    PS = [kb.ps("psC%d" % i, [128, 2, 512]) for i in range(2)]
    plg = kb.ps("plg", [128, 512])
    pgt = kb.ps("pgtC", [128, 512])
    mods = compute_mod(kb, io["cc"], io["w_mod"], io["b_mod"], [2, 3, 4, 5], PS, pre={5: (gate2[0], gate2[1])})
    n2 = bcast_load(kb, "n2", io["norm2_w"], 1024)
    G2 = []
    for i in range(2):
        g = mods[4][i]
        kb.stt(g[:], g[:], 1.0, n2[:], ALU.add, ALU.mult, r=[g, n2], w=[g])
        G2.append(g)
    gate1 = mods[2]
    S2 = mods[3]
    w_out = kb.sb("w_out", [128, 8, 1024], BF16)
    for kc in range(8):
        kb.dma("pool", w_out[:, kc, :], io["w_out"][kc * 128:(kc + 1) * 128, :], r=[], w=w_out)
    wr = kb.sb("wr", [128, 8, 36])
    kb.dma("sp", wr[:, :, 0:4], io["w_grp"].rearrange("(k p) n -> p k n", p=128), r=[], w=wr)
    kb.dma("sp", wr[:, :, 4:36], io["w_erouter"].rearrange("(k p) n -> p k n", p=128), r=[], w=wr)
    wrh = kb.sb("wrh", [128, 8, 36], BF16)
    wrl = kb.sb("wrl", [128, 8, 36], BF16)
    kb.copy("dve", wrh[:], wr[:], r=[wr], w=[wrh])
    kb.tt(wrl[:], wr[:], wrh[:], ALU.subtract, r=[wr, wrh], w=[wrl])
    rb = kb.sb("rb", [128, 36])
    kb.dma("sp", rb[:, 0:4], io["b_grp"].partition_broadcast(128), r=[], w=rb)
    kb.dma("sp", rb[:, 4:36], io["b_erouter"].partition_broadcast(128), r=[], w=rb)
    NB_ = 2
    xt = [kb.sb("xtC%d" % i, [128, 1024]) for i in range(NB_)]
    mT = [kb.sb("mTC%d" % i, [128, 8, 128], BF16) for i in range(NB_)]
    tmp = [kb.sb("tmpC", [128, 1024])] * NB_
    h2 = [kb.sb("h2C", [128, 1024])] * NB_
    h2l = [kb.sb("h2l", [128, 8, 128], BF16)] * NB_
    st = [kb.sb("stC%d" % i, [128, 16]) for i in range(NB_)]
    lg = [kb.sb("lg%d" % i, [128, 36]) for i in range(NB_)]
    rw = [kb.sb("rw%d" % i, [128, 6, 32]) for i in range(NB_)]
    m8 = [kb.sb("m8_%d" % i, [128, 8]) for i in range(NB_)]
    for ti in range(NT):
        b_ = ti % NB_
        mi = 1 if ti >= 16 else 0
        t0 = ti * 128
        X, MT, TMP, H2, H2L, ST, LG, RW, M8 = xt[b_], mT[b_], tmp[b_], h2[b_], h2l[b_], st[b_], lg[b_], rw[b_], m8[b_]
        kb.dma("sp", X[:], io["x"][t0:t0 + 128, :], r=[], w=X)
        kb.dma("sp", MT[:], io["mixT"][:, t0:t0 + 128].rearrange("(k p) t -> p k t", p=128), r=[], w=MT)
        for hf in range(2):
            for kc in range(8):
                kb.mm(PS[0][:, hf, :], MT[:, kc, :], w_out[:, kc, hf * 512:(hf + 1) * 512], kc == 0, kc == 7, r=[MT, w_out], w=[PS[0]])
        X1 = x1[:, ti, :]
        kb.tt(TMP[:], PS[0][:].rearrange("p a b -> p (a b)"), gate1[mi][:], ALU.mult, r=[PS[0], gate1[mi]], w=[TMP])
        kb.tt(X1, TMP[:], X[:], ALU.add, r=[TMP, X], w=[x1], eng="pool")
        kb.act(TMP[:], X1, AF.Square, r=[x1], w=[TMP, ST], accum_out=ST[:, 0:1])
        rstd_of(kb, ST[:, 0:1], 1, 1024, ST[:, 1:2], ST[:, 2:3], r=[ST], w=[ST])
        kb.stt(H2[:], X1, ST[:, 2:3], G2[mi][:], ALU.mult, ALU.mult, r=[x1, ST, G2[mi]], w=[H2])
        kb.tt(H2[:], H2[:], S2[mi][:], ALU.add, r=[H2, S2[mi]], w=[H2], eng="pool")
        for kc in range(8):
            kb.tr(PS[1][:, kc // 4, (kc % 4) * 128:(kc % 4 + 1) * 128], H2[:, kc * 128:(kc + 1) * 128], ident[:], r=[H2, ident], w=[PS[1]])
        hi = h2T[:, :, t0:t0 + 128]
        for a in range(2):
            kb.copy("act", h2T[:, 4 * a:4 * a + 4, t0:t0 + 128], PS[1][:, a, :].rearrange("p (b t) -> p b t", t=128), r=[PS[1]], w=[h2T])
        for a in range(2):
            kb.tt(H2L[:, 4 * a:4 * a + 4, :], PS[1][:, a, :].rearrange("p (b t) -> p b t", t=128), h2T[:, 4 * a:4 * a + 4, t0:t0 + 128],
                  ALU.subtract, r=[PS[1], h2T], w=[H2L])
        n = 0
        for kc in range(8):
            for (l_, r_, lr, rr_) in ((hi[:, kc, :], wrh[:, kc, :], h2T, wrh), (H2L[:, kc, :], wrh[:, kc, :], H2L, wrh), (hi[:, kc, :], wrl[:, kc, :], h2T, wrl)):
                kb.mm(plg[:, 0:36], l_, r_, n == 0, n == 23, r=[lr, rr_], w=[plg])
                n += 1
        kb.tt(LG[:], plg[:, 0:36], rb[:], ALU.add, r=[plg, rb], w=[LG])
        kb.op("dve", lambda e: e.tensor_reduce(out=ST[:, 4:5], in_=LG[:, 0:4], axis=AX.X, op=ALU.max), r=[LG], w=[ST])
        kb.tt(RW[:, 0, 0:4], LG[:, 0:4], ST[:, 4:5].broadcast_to([128, 4]), ALU.is_equal, r=[LG, ST], w=[RW])
        kb.ts(ST[:, 5:6], ST[:, 4:5], -1.0, None, ALU.mult, r=[ST], w=[ST])
        kb.act(RW[:, 1, 0:4], LG[:, 0:4], AF.Exp, r=[LG, ST], w=[RW, ST], bias=ST[:, 5:6], accum_out=ST[:, 6:7])
        kb.op("dve", lambda e: e.reciprocal(out=ST[:, 7:8], in_=ST[:, 6:7]), r=[ST], w=[ST])
        kb.ts(RW[:, 2, 0:4], RW[:, 0, 0:4], 1e9, -1e9, ALU.mult, ALU.add, r=[RW], w=[RW])
        EM = RW[:, 3, :]
        kb.tt(RW[:, 3, :].rearrange("p (g e) -> p g e", e=8), LG[:, 4:36].rearrange("p (g e) -> p g e", e=8),
              RW[:, 2, 0:4].unsqueeze(2).broadcast_to([128, 4, 8]), ALU.add, r=[LG, RW], w=[RW])
        kb.op("dve", lambda e: e.max(out=M8[:], in_=EM), r=[RW], w=[M8])
        kb.tt(ST[:, 8:9], M8[:, 1:2], M8[:, 0:1], ALU.subtract, r=[M8], w=[ST])
        kb.act(ST[:, 8:9], ST[:, 8:9], AF.Exp, r=[ST], w=[ST])
        kb.ts(ST[:, 8:9], ST[:, 8:9], 1.0, None, ALU.add, r=[ST], w=[ST])
        kb.op("dve", lambda e: e.reciprocal(out=ST[:, 9:10], in_=ST[:, 8:9]), r=[ST], w=[ST])
        kb.ts(ST[:, 10:11], ST[:, 9:10], -1.0, 1.0, ALU.mult, ALU.add, r=[ST], w=[ST])
        kb.tt(ST[:, 11:12], ST[:, 9:10], ST[:, 7:8], ALU.mult, r=[ST], w=[ST])
        kb.tt(ST[:, 12:13], ST[:, 10:11], ST[:, 7:8], ALU.mult, r=[ST], w=[ST])
        kb.tt(RW[:, 4, :], EM, M8[:, 0:1].broadcast_to([128, 32]), ALU.is_equal, r=[RW, M8], w=[RW])
        kb.ts(RW[:, 4, :], RW[:, 4, :], ST[:, 11:12], None, ALU.mult, r=[RW, ST], w=[RW])
        kb.tt(RW[:, 5, :], EM, M8[:, 1:2].broadcast_to([128, 32]), ALU.is_equal, r=[RW, M8], w=[RW])
        kb.ts(RW[:, 5, :], RW[:, 5, :], ST[:, 12:13], None, ALU.mult, r=[RW, ST], w=[RW])
        kb.tt(RW[:, 4, :], RW[:, 4, :], RW[:, 5, :], ALU.add, r=[RW], w=[RW])
        kb.tr(pgt[0:32, 0:128], RW[:, 4, :], ident[:], r=[RW, ident], w=[pgt])
        kb.copy("dve", gTh[:, t0:t0 + 128], pgt[0:32, 0:128], r=[pgt], w=[gTh])
        kb.tt(gTl[:, t0:t0 + 128], pgt[0:32, 0:128], gTh[:, t0:t0 + 128], ALU.subtract, r=[pgt, gTh], w=[gTl])
    kb.pop()
    kb.push()
    NSLOT = 4
    EG = 2
    wg = [kb.sb("wg%d" % i, [128, 8, 256], BF16) for i in range(NSLOT)]
    wu = [kb.sb("wu%d" % i, [128, 8, 256], BF16) for i in range(NSLOT)]
    wd = [kb.sb("wd%d" % i, [128, 2, 1024], BF16) for i in range(NSLOT)]
    pgu = [kb.ps("pgu%d" % i, [128, 2, 256]) for i in range(2)]
    pbc = kb.ps("pbcC", [128, 512])
    pacc = [kb.ps("pacc%d" % i, [128, 512]) for i in range(4)]
    sg = [kb.sb("sg%d" % i, [128, 256], BF16) for i in range(2)]
    tu = [kb.sb("tu%d" % i, [128, 256]) for i in range(2)]
    aT = [kb.sb("aT%d" % i, [128, 256], BF16) for i in range(4)]
    fl = [kb.sb("fl%d" % i, [128, 512]) for i in range(2)]
    TG = 256
    it = {"f": 0, "a": 0, "fl": 0}

    def load_expert(e):
        s_ = e % NSLOT
        kb.dma("pool", wg[s_][:], io["w_gate"][e].rearrange("(k p) f -> p k f", p=128), r=[], w=wg[s_])
        kb.dma("pool", wu[s_][:], io["w_up"][e].rearrange("(k p) f -> p k f", p=128), r=[], w=wu[s_])
        kb.dma("pool", wd[s_][:], io["w_down"][e].rearrange("(c p) d -> p c d", p=128), r=[], w=wd[s_])

    for e in range(min(NSLOT, NE)):
        load_expert(e)
    for g0 in range(0, NE, EG):
        for tg in range(NTOK // TG):
            c0 = tg * TG
            mi = 1 if tg >= 8 else 0
            first = True
            for e in range(g0, g0 + EG):
                s_ = e % NSLOT
                kb.mm(pbc[:, 0:TG], selb[:, e * 128:(e + 1) * 128], gTh[:, c0:c0 + TG], True, False, r=[selb, gTh], w=[pbc])
                kb.mm(pbc[:, 0:TG], selb[:, e * 128:(e + 1) * 128], gTl[:, c0:c0 + TG], False, True, r=[selb, gTl], w=[pbc])
                ats = []
                for fc in range(2):
                    p = pgu[it["f"] % 2]
                    sgt = sg[it["f"] % 2]
                    tut = tu[it["f"] % 2]
                    it["f"] += 1
                    at = aT[it["a"] % 4]
                    it["a"] += 1
                    for kc in range(8):
                        kb.mm(p[:, 0, :], wg[s_][:, kc, fc * 128:(fc + 1) * 128], h2T[:, kc, c0:c0 + TG], kc == 0, kc == 7, r=[wg[s_], h2T], w=[p])
                    for kc in range(8):
                        kb.mm(p[:, 1, :], wu[s_][:, kc, fc * 128:(fc + 1) * 128], h2T[:, kc, c0:c0 + TG], kc == 0, kc == 7, r=[wu[s_], h2T], w=[p])
                    kb.act(sgt[:], p[:, 0, :], AF.Silu, r=[p], w=[sgt])
                    kb.tt(tut[:], p[:, 1, :], sgt[:], ALU.mult, r=[p, sgt], w=[tut])
                    kb.tt(at[:], pbc[:, 0:TG], tut[:], ALU.mult, r=[pbc, tut], w=[at])
                    ats.append(at)
                for fc in range(2):
                    last = (e == g0 + EG - 1) and fc == 1
                    for sub in range(2):
                        for hf in range(2):
                            kb.mm(pacc[sub * 2 + hf][:, :], ats[fc][:, sub * 128:(sub + 1) * 128], wd[s_][:, fc, hf * 512:(hf + 1) * 512],
                                  first and fc == 0, last, r=[ats[fc], wd[s_]], w=[pacc[sub * 2 + hf]])
                first = False
            for sub in range(2):
                ti = tg * 2 + sub
                for hf in range(2):
                    f = fl[it["fl"] % 2]
                    it["fl"] += 1
                    kb.tt(f[:], pacc[sub * 2 + hf][:, :], gate2[mi][:, hf * 512:(hf + 1) * 512], ALU.mult, r=[pacc[sub * 2 + hf], gate2[mi]], w=[f])
                    kb.tt(x1[:, ti, hf * 512:(hf + 1) * 512], x1[:, ti, hf * 512:(hf + 1) * 512], f[:], ALU.add, r=[x1, f], w=[x1], eng="pool")
        for e in range(g0 + NSLOT, min(g0 + NSLOT + EG, NE)):
            load_expert(e)
    for ti in range(NT):
        kb.dma("sp", io["xo"][ti * 128:(ti + 1) * 128, :], x1[:, ti, :], r=[x1], w=io["xo_res"])
    kb.pop()


C_INPUTS = [("x", [NTOK, D], F32), ("mixT", [D, NTOK], BF16), ("cc", [128, 8, 2], F32), ("w_mod", [D, 6 * D], F32), ("b_mod", [6 * D], F32),
            ("w_out", [D, D], F32), ("norm2_w", [D], F32), ("w_grp", [D, 4], F32), ("b_grp", [4], F32),
            ("w_erouter", [D, 32], F32), ("b_erouter", [32], F32), ("w_gate", [32, D, 256], F32), ("w_up", [32, D, 256], F32),
            ("w_down", [32, 256, D], F32)]


def build_C():
    nc = bass.Bass("TRN2", target_bir_lowering=False)
    io = IO()
    for nm, shp, dt in C_INPUTS:
        declare(nc, io, nm, shp, dt, "ExternalInput")
    cd = {}
    for nm in ("ident", "sel"):
        th = nc.dram_tensor("c_" + nm, CONST_SHAPES[nm], F32, kind="ExternalInput")
        cd[nm] = T(th, "c_" + nm)
    declare(nc, io, "xo", [NTOK, D], F32, "ExternalOutput")
    with ExitStack() as st:
        kb = KB(nc, st)
        cst = load_consts(kb, cd, ["ident"])
        cst["sel_d"] = cd["sel"][:]
        phase_C(kb, io, cst)
        while len(kb.stacks) > 1:
            kb.stacks.pop().close()
        kb.finish([io["xo_res"]])
        print("phase C: n_inst", kb.n_inst, "n_wait", kb.n_wait, "dsems", kb.ndsem)
    return nc


def c_inmaps(inp, l, xl, xc, bout):
    cst, _ = consts()
    maps = []
    for b in range(NB):
        full = np.zeros((D, B_T), dtype=bout[0].dtype)
        for j in range(4):
            m = bout[4 * b + j]
            full[128 * j:128 * j + 128] = m[0:128]
            full[512 + 64 * j:512 + 64 * j + 64] = m[128:192]
            full[768 + 64 * j:768 + 64 * j + 64] = m[192:256]
        for j in range(4):
            core = 4 * b + j
            cols = np.concatenate([np.arange(j * TL, (j + 1) * TL), np.arange(SEQ, B_T)])
            mm_ = {"x": f32(core_tokens(xl, xc, core)), "mixT": np.ascontiguousarray(full[:, cols]),
                   "cc": cc_layout(inp["c"][b], inp["c_ctx"]), "c_ident": cst["ident"], "c_sel": cst["sel"]}
            for nm in ("w_mod", "b_mod", "w_out", "norm2_w", "w_grp", "b_grp", "w_erouter", "b_erouter", "w_gate", "w_up", "w_down"):
                mm_[nm] = f32(inp[nm][l])
            maps.append(mm_)
    return maps


def _run(nc, maps):
    return run_bass_kernel_spmd(nc, maps, core_ids=list(range(NCORE))).results


def kernel(**inputs):
    inp = {k: np.asarray(v) for k, v in inputs.items()}
    xl = f32(inp["x"])
    xc = f32(inp["ctx"])
    for l in range(DEPTH):
        ra = _run(build_A(), a_inmaps(inp, l, xl, xc))
        aout = [{k: np.asarray(r[k]) for k in ("qT", "kT", "v", "fm", "tm")} for r in ra]
        del ra
        rb = _run(build_B(True), b_inmaps(inp, l, aout))
        bout = [np.asarray(r["mixT"]) for r in rb]
        del rb, aout
        rc = _run(build_C(), c_inmaps(inp, l, xl, xc, bout))
        xl_n = np.empty_like(xl)
        xc_n = np.empty_like(xc)
        for core in range(NCORE):
            b, j = core // 4, core % 4
            xo = np.asarray(rc[core]["xo"], dtype=np.float32)
            xl_n[b, j * TL:(j + 1) * TL] = xo[:TL]
            if j == 0:
                xc_n[b] = xo[TL:]
        xl, xc = xl_n, xc_n
        del rc, bout
    return xl
```

```python
import os
import numpy as np
import ml_dtypes
from contextlib import ExitStack
import concourse.bass as bass
import concourse.mybir as mybir
from concourse.bass_utils import run_bass_kernel_spmd

F32 = mybir.dt.float32
BF16 = mybir.dt.bfloat16
AF = mybir.ActivationFunctionType
ALU = mybir.AluOpType
AX = mybir.AxisListType

D = 1024
NB = 2
SEQ = 8192
DEPTH = 4
NCTX = 256
EPS = 1e-6
NCORE = 8
TL = 2048
NT = 18
NTOK = NT * 128
D_IN = 2000
O_CQ, O_CKV, O_KR, O_MX, O_MV, O_MO, O_MG, O_GQ, O_GK, O_GV, O_GR, O_GA = (
    0, 256, 384, 416, 672, 928, 1184, 1200, 1328, 1456, 1712, 1968)

SAME_ENG_SYNC = bool(int(os.environ.get("KSES", "1")))

DBG = float(os.environ.get('KDBG', '99'))


class Res:
    __slots__ = ("name", "w", "r", "dsem", "dcnt", "dkey", "excl")

    def __init__(self, name):
        self.name = name
        self.excl = False
        self.w = None
        self.r = {}
        self.dsem = None
        self.dcnt = 0
        self.dkey = None


class T:
    def __init__(self, th, name):
        self.t = th
        self.res = Res(name)
        self.name = name

    def __getitem__(self, idx):
        return self.t[idx]


def _res(x):
    return x.res if isinstance(x, T) else x


class KB:
    def __init__(self, nc, stack):
        self.nc = nc
        self.st = stack
        self.eng = {"pe": nc.tensor, "dve": nc.vector, "act": nc.scalar,
                    "pool": nc.gpsimd, "sp": nc.sync}
        self.semh = {}
        self.cnt = {}
        for k in self.eng:
            self.semh[k] = stack.enter_context(nc.semaphore("s_" + k))
            self.cnt[k] = 0
        self.waited = {k: {} for k in self.eng}
        self.ndsem = 0
        self.n_inst = 0
        self.n_wait = 0
        self.uid = 0
        self.stacks = [stack]
        self.dres = []

    def sb(self, name, shape, dt=F32):
        self.uid += 1
        nm = "%s_%d" % (name, self.uid)
        return T(self.stacks[-1].enter_context(self.nc.sbuf_tensor(nm, list(shape), dt)), nm)

    def ps(self, name, shape, dt=F32):
        self.uid += 1
        nm = "%s_%d" % (name, self.uid)
        t = T(self.stacks[-1].enter_context(self.nc.psum_tensor(nm, list(shape), dt)), nm)
        t.res.excl = True
        return t

    def push(self):
        self.stacks.append(ExitStack())

    def pop(self):
        self.barrier()
        self.stacks.pop().close()

    def barrier(self):
        for e in self.eng:
            deps = {k: self.cnt[k] for k in self.eng if k != e and self.cnt[k] > 0}
            for res in self.dres:
                deps[res.dkey] = res.dcnt
            self._wait(e, deps)

    def _wait(self, e, deps):
        for key, val in deps.items():
            if self.waited[e].get(key, 0) >= val:
                continue
            if key == e and (e == "pe" or not SAME_ENG_SYNC):
                continue
            self.eng[e].wait_ge(self.semh[key], val)
            self.waited[e][key] = val
            self.n_wait += 1

    def _collect(self, reads, writes, dma_write=None):
        deps = {}

        def add(ev):
            if ev is None:
                return
            k, v = ev
            if deps.get(k, 0) < v:
                deps[k] = v

        def cur(res):
            if res.w is None:
                return None
            if res.w[0] == res.dkey:
                return (res.dkey, res.dcnt)
            return res.w

        for r in reads:
            add(cur(r))
        for w in writes:
            if dma_write is not None and w is dma_write and w.w is not None \
                    and w.w[0] == w.dkey and not w.r:
                continue
            add(cur(w))
            for k, v in w.r.items():
                add((k, v))
        return deps

    def op(self, e, fn, r=(), w=()):
        reads = [_res(x) for x in r]
        writes = [_res(x) for x in w]
        ex = [x for x in reads if x.excl and x not in writes]
        if ex:
            reads = [x for x in reads if not x.excl]
            writes = writes + ex
        self._wait(e, self._collect(reads, writes))
        ins = fn(self.eng[e])
        self.cnt[e] += 1
        ins.then_inc(self.semh[e], 1)
        self.n_inst += 1
        ev = (e, self.cnt[e])
        for rr in reads:
            if rr.r.get(e, 0) < ev[1]:
                rr.r[e] = ev[1]
        for ww in writes:
            ww.w = ev
            ww.r = {}
        return ins

    def dma(self, q, out, in_, r=(), w=None, **kw):
        reads = [_res(x) for x in r]
        wres = _res(w)
        if wres.dsem is None:
            wres.dkey = "d%d" % self.ndsem
            self.ndsem += 1
            wres.dsem = self.st.enter_context(self.nc.semaphore(wres.dkey))
            self.semh[wres.dkey] = wres.dsem
            self.dres.append(wres)
        self._wait(q, self._collect(reads, [wres], dma_write=wres))
        ins = self.eng[q].dma_start(out=out, in_=in_, **kw)
        wres.dcnt += 16
        ins.then_inc(wres.dsem, 16)
        self.n_inst += 1
        ev = (wres.dkey, wres.dcnt)
        for rr in reads:
            if rr.r.get(ev[0], 0) < ev[1]:
                rr.r[ev[0]] = ev[1]
        wres.w = ev
        wres.r = {}
        return ins

    def finish(self, outs, e="sp"):
        self._wait(e, self._collect([_res(x) for x in outs], []))

    def mm(self, out, lhsT, rhs, start, stop, r, w):
        return self.op("pe", lambda e: e.matmul(out, lhsT=lhsT, rhs=rhs, start=start, stop=stop), r=r, w=w)

    def tr(self, out, in_, ident, r, w):
        return self.op("pe", lambda e: e.transpose(out=out, in_=in_, identity=ident), r=r, w=w)

    def copy(self, eng, out, in_, r, w):
        if eng == "act":
            return self.op("act", lambda e: e.copy(out=out, in_=in_), r=r, w=w)
        return self.op(eng, lambda e: e.tensor_copy(out=out, in_=in_), r=r, w=w)

    def act(self, out, in_, func, r, w, **kw):
        return self.op("act", lambda e: e.activation(out=out, in_=in_, func=func, **kw), r=r, w=w)

    def tt(self, out, in0, in1, op, r, w, eng="dve"):
        return self.op(eng, lambda e: e.tensor_tensor(out=out, in0=in0, in1=in1, op=op), r=r, w=w)

    def ts(self, out, in0, s1, s2, op0, op1=None, r=(), w=(), eng="dve"):
        if op1 is None:
            return self.op(eng, lambda e: e.tensor_scalar(out=out, in0=in0, scalar1=s1, scalar2=None, op0=op0), r=r, w=w)
        return self.op(eng, lambda e: e.tensor_scalar(out=out, in0=in0, scalar1=s1, scalar2=s2, op0=op0, op1=op1), r=r, w=w)

    def stt(self, out, in0, scalar, in1, op0, op1, r, w, accum_out=None):
        return self.op("dve", lambda e: e.scalar_tensor_tensor(out=out, in0=in0, scalar=scalar, in1=in1, op0=op0, op1=op1, accum_out=accum_out), r=r, w=w)


def rstd_of(kb, ss, n, nfeat, tmp, out, r, w):
    if os.environ.get("KRSTD", "1") == "1":
        kb.act(tmp, ss, AF.Ln, r=r, w=w, scale=1.0 / nfeat, bias=EPS)
        kb.act(out, tmp, AF.Exp, r=w, w=w, scale=-0.5)
        return
    kb.ts(tmp, ss, 1.0 / nfeat, EPS, ALU.mult, ALU.add, r=r, w=w)
    kb.act(tmp, tmp, AF.Sqrt, r=w, w=w)
    kb.op("dve", lambda e: e.reciprocal(out=out, in_=tmp), r=w, w=w)


def rope_tables():
    rows = SEQ // 64
    row = np.broadcast_to(np.arange(rows, dtype=np.float32)[:, None], (rows, 64)).reshape(-1)
    col = np.broadcast_to(np.arange(64, dtype=np.float32)[None, :], (rows, 64)).reshape(-1)
    inv = (np.float32(10000.0) ** (-np.arange(8, dtype=np.float32) / np.float32(8))).astype(np.float32)
    ang = np.concatenate([row[:, None] * inv, col[:, None] * inv], axis=-1).astype(np.float32)
    return np.concatenate([np.cos(ang), np.sin(ang)], axis=-1).astype(np.float32)


def host_consts():
    c = {}
    c["ident"] = np.eye(128, dtype=np.float32)
    j = np.arange(128)
    c["triu"] = (j[:, None] <= j[None, :]).astype(np.float32)
    c["tril"] = (j[:, None] >= j[None, :]).astype(np.float32)
    sel = np.zeros((32, 32, 128), np.float32)
    for e in range(32):
        sel[e, e, :] = 1.0
    c["sel"] = sel.reshape(32, 32 * 128)
    return c


def load_consts(kb, cd, names):
    out = {}
    for nm in names:
        shp = {"ident": [128, 128], "triu": [128, 128], "tril": [128, 128], "sel": [32, 32 * 128]}[nm]
        t = kb.sb("c_" + nm, shp)
        kb.dma("sp", t[:], cd[nm][:], r=[cd[nm]], w=t)
        out[nm] = t
        if nm in ("triu", "tril"):
            tb = kb.sb("c_" + nm + "_b", shp, BF16)
            kb.copy("dve", tb[:], t[:], r=[t], w=[tb])
            out[nm + "_b"] = tb
    return out


def bcast_load(kb, name, ap1d, n, q="sp"):
    t = kb.sb(name, [128, n])
    kb.dma(q, t[:], ap1d.partition_broadcast(128), r=[], w=t)
    return t


def compute_mod(kb, cc_d, w_mod_d, b_mod_d, chunks, pool_ps, pre=None):
    res = {}
    for j in chunks:
        if pre is not None and j in pre:
            res[j] = pre[j]
        else:
            res[j] = (kb.sb("mod_l%d" % j, [128, 1024]), kb.sb("mod_c%d" % j, [128, 1024]))
    kb.push()
    cc = kb.sb("cc", [128, 8, 2])
    kb.dma("sp", cc[:], cc_d, r=[], w=cc)
    sc = kb.sb("sc", [128, 8, 2])
    kb.act(sc[:], cc[:], AF.Silu, r=[cc], w=[sc])
    SC = [kb.sb("SC%d" % i, [128, 8, 128], BF16) for i in range(2)]
    for i in range(2):
        kb.copy("dve", SC[i][:], sc[:, :, i:i + 1].broadcast_to([128, 8, 128]), r=[sc], w=[SC[i]])
    wm = [kb.sb("wm%d" % i, [128, 8, 512], BF16) for i in range(2)]
    bm = [kb.sb("bm%d" % i, [128, 512]) for i in range(2)]
    si = 0
    for j in chunks:
        tl, tc_ = res[j]
        for hf in range(2):
            c0 = j * 1024 + hf * 512
            wmt = wm[si % 2]
            bmt = bm[si % 2]
            si += 1
            kb.dma("pool", wmt[:], w_mod_d[:, c0:c0 + 512].rearrange("(k p) n -> p k n", p=128), r=[], w=wmt)
            kb.dma("sp", bmt[:], b_mod_d[c0:c0 + 512].partition_broadcast(128), r=[], w=bmt)
            for i, dst in enumerate((tl, tc_)):
                ps = pool_ps[i]
                for kc in range(8):
                    kb.mm(ps[:, 0, :], SC[i][:, kc, :], wmt[:, kc, :], kc == 0, kc == 7, r=[SC[i], wmt], w=[ps])
                kb.tt(dst[:, hf * 512:(hf + 1) * 512], ps[:, 0, :], bmt[:], ALU.add, r=[ps, bmt], w=[dst])
    kb.pop()
    return res


def phase_A(kb, io, cst):
    ident = cst["ident"]
    PS = [kb.ps("psA%d" % i, [128, 2, 512]) for i in range(4)]
    mods = compute_mod(kb, io["cc"], io["w_mod"], io["b_mod"], [0, 1], PS)
    n1 = bcast_load(kb, "n1", io["norm1_w"], 1024)
    G1 = []
    S1 = []
    for i in range(2):
        g = kb.sb("G1_%d" % i, [128, 1024])
        kb.stt(g[:], mods[1][i][:], 1.0, n1[:], ALU.add, ALU.mult, r=[mods[1][i], n1], w=[g])
        G1.append(g)
        S1.append(mods[0][i])
    w_in = kb.sb("w_in", [128, 8, D_IN], BF16)
    for kc in range(8):
        kb.dma("pool", w_in[:, kc, :], io["w_in"][kc * 128:(kc + 1) * 128, :], r=[], w=w_in)
    w_uq = kb.sb("w_uq", [128, 2, 768], BF16)
    kb.dma("pool", w_uq[:], io["w_uq"].rearrange("(k p) n -> p k n", p=128), r=[], w=w_uq)
    w_ukv = kb.sb("w_ukv", [128, 1024], BF16)
    kb.dma("pool", w_ukv[:], io["w_ukv"], r=[], w=w_ukv)
    qan = bcast_load(kb, "qan", io["q_a_norm"], 256)
    kvan = bcast_load(kb, "kvan", io["kv_a_norm"], 128)
    qnw1 = bcast_load(kb, "qnw1", io["q_norm_w"], 96)
    knw1 = bcast_load(kb, "knw1", io["k_norm_w"], 96)
    qnw = kb.sb("qnw", [128, 8, 96])
    knw = kb.sb("knw", [128, 8, 96])
    kb.ts(qnw[:], qnw1[:].unsqueeze(1).broadcast_to([128, 8, 96]), float(96 ** -0.5), None, ALU.mult, r=[qnw1], w=[qnw])
    kb.copy("dve", knw[:], knw1[:].unsqueeze(1).broadcast_to([128, 8, 96]), r=[knw1], w=[knw])
    rope = kb.sb("rope", [128, 16, 32])
    kb.dma("sp", rope[:], io["rope"].rearrange("(n p) c -> p n c", p=128), r=[], w=rope)

    if DBG < 1:
        return
    TM_SLABS = [[(O_CQ, 416), (O_MG, 16)], [(O_MV, 512)], [(O_GV, 512)]]
    FM_GROUPS = [(O_MX, 128), (O_MX + 128, 128), (O_GQ, 128), (O_GK, 128), (O_GA, 32)]
    NB_ = 2
    xt = [kb.sb("xt%d" % i, [128, 1024]) for i in range(NB_)]
    junk = [kb.sb("junk%d" % i, [128, 1024]) for i in range(NB_)]
    xm = [kb.sb("xm%d" % i, [128, 1024]) for i in range(NB_)]
    xmT = [kb.sb("xmT%d" % i, [128, 8, 128], BF16) for i in range(NB_)]
    htm = [kb.sb("htm%d" % i, [128, 1456]) for i in range(NB_)]
    hfm = [kb.sb("hfm%d" % i, [128, 5, 128]) for i in range(NB_)]
    st1 = [kb.sb("st1_%d" % i, [128, 32]) for i in range(NB_)]
    cqn = [kb.sb("cqn%d" % i, [128, 384]) for i in range(NB_)]
    cT = [kb.sb("cT%d" % i, [128, 3, 128], BF16) for i in range(NB_)]
    qf = [kb.sb("qf%d" % i, [128, 8, 96]) for i in range(NB_)]
    kf = [kb.sb("kf%d" % i, [128, 8, 96]) for i in range(NB_)]
    sq = [kb.sb("sq%d" % i, [128, 8, 96]) for i in range(NB_)]
    rt = [kb.sb("rt%d" % i, [128, 4, 8, 16]) for i in range(NB_)]
    qTs = [kb.sb("qTs%d" % i, [96, 8, 128], BF16) for i in range(NB_)]
    kTs = [kb.sb("kTs%d" % i, [96, 8, 128], BF16) for i in range(NB_)]
    vs = [kb.sb("vs%d" % i, [128, 8, 64], BF16) for i in range(NB_)]

    def head_norm_rope(src, dst_f, sqt, stt_, wbc, ropeidx, rtt, r_extra):
        kb.act(sqt[:], src, AF.Square, r=r_extra, w=[sqt])
        kb.op("dve", lambda e: e.tensor_reduce(out=stt_[:, 0:8], in_=sqt[:], axis=AX.X, op=ALU.add), r=[sqt], w=[stt_])
        rstd_of(kb, stt_[:, 0:8], 8, 96, stt_[:, 8:16], stt_[:, 16:24], r=[stt_], w=[stt_])
        kb.tt(dst_f[:], src, stt_[:, 16:24].unsqueeze(2).broadcast_to([128, 8, 96]), ALU.mult, r=r_extra + [stt_], w=[dst_f])
        kb.tt(dst_f[:], dst_f[:], wbc[:], ALU.mult, r=[dst_f, wbc], w=[dst_f])
        if ropeidx is not None:
            cos = rope[:, ropeidx, 0:16].unsqueeze(1).broadcast_to([128, 8, 16])
            sin = rope[:, ropeidx, 16:32].unsqueeze(1).broadcast_to([128, 8, 16])
            x1 = dst_f[:, :, 64:80]
            x2 = dst_f[:, :, 80:96]
            kb.tt(rtt[:, 0], x1, cos, ALU.mult, r=[dst_f, rope], w=[rtt])
            kb.tt(rtt[:, 1], x2, sin, ALU.mult, r=[dst_f, rope], w=[rtt])
            kb.tt(rtt[:, 2], x1, sin, ALU.mult, r=[dst_f, rope], w=[rtt])
            kb.tt(rtt[:, 3], x2, cos, ALU.mult, r=[dst_f, rope], w=[rtt])
            kb.tt(x1, rtt[:, 0], rtt[:, 1], ALU.subtract, r=[rtt], w=[dst_f])
            kb.tt(x2, rtt[:, 2], rtt[:, 3], ALU.add, r=[rtt], w=[dst_f])

    for ti in range(NT):
        b_ = ti % NB_
        is_ctx = ti >= 16
        mi = 1 if is_ctx else 0
        t0 = ti * 128
        X, XM, XMT, H, HF, ST = xt[b_], xm[b_], xmT[b_], htm[b_], hfm[b_], st1[b_]
        kb.dma("sp", X[:], io["x"][t0:t0 + 128, :], r=[io["x_res"]], w=X)
        kb.act(junk[b_][:], X[:], AF.Square, r=[X], w=[junk[b_], ST], accum_out=ST[:, 0:1])
        rstd_of(kb, ST[:, 0:1], 1, 1024, ST[:, 1:2], ST[:, 2:3], r=[ST], w=[ST])
        kb.stt(XM[:], X[:], ST[:, 2:3], G1[mi][:], ALU.mult, ALU.mult, r=[X, ST, G1[mi]], w=[XM])
        kb.tt(XM[:], XM[:], S1[mi][:], ALU.add, r=[XM, S1[mi]], w=[XM], eng="pool")
        if DBG < 2:
            continue
        for kc in range(8):
            kb.tr(PS[0][:, kc // 4, (kc % 4) * 128:(kc % 4 + 1) * 128], XM[:, kc * 128:(kc + 1) * 128], ident[:], r=[XM, ident], w=[PS[0]])
        kb.copy("act", XMT[:].rearrange("p (a b) t -> p a (b t)", a=2), PS[0][:], r=[PS[0]], w=[XMT])
        pcol = 0
        for si, slab in enumerate(TM_SLABS):
            pst = PS[1] if si < 2 else PS[2]
            bank = si if si < 2 else 0
            off = 0
            for (c0, wd) in slab:
                for kc in range(8):
                    kb.mm(pst[:, bank, off:off + wd], XMT[:, kc, :], w_in[:, kc, c0:c0 + wd], kc == 0, kc == 7, r=[XMT, w_in], w=[pst])
                off += wd
            kb.copy("act" if si != 1 else "dve", H[:, pcol:pcol + off], pst[:, bank, 0:off], r=[pst], w=[H])
            pcol += off
        for gi, (c0, wd) in enumerate(FM_GROUPS):
            for kc in range(8):
                kb.mm(PS[3][0:wd, gi // 4, (gi % 4) * 128:(gi % 4 + 1) * 128], w_in[:, kc, c0:c0 + wd], XMT[:, kc, :], kc == 0, kc == 7, r=[XMT, w_in], w=[PS[3]])
        kb.copy("dve", HF[:, 0:4, :].rearrange("p a t -> p (a t)"), PS[3][:, 0, :], r=[PS[3]], w=[HF])
        kb.copy("dve", HF[0:32, 4, :], PS[3][0:32, 1, 0:128], r=[PS[3]], w=[HF])
        if DBG < 3:
            continue
        kb.dma("sp", io["fm"][0:512, t0:t0 + 128].rearrange("(a p) t -> p a t", p=128), HF[:, 0:4, :], r=[HF], w=io["fm_res"])
        kb.dma("sp", io["fm"][512:544, t0:t0 + 128], HF[0:32, 4, :], r=[HF], w=io["fm_res"])
        kb.dma("sp", io["tm"][t0:t0 + 128, :], H[:, 416:1456], r=[H], w=io["tm_res"])
        if DBG < 4:
            continue
        CQ = cqn[b_]
        kb.act(junk[b_][:, 0:256], H[:, 0:256], AF.Square, r=[H], w=[junk[b_], ST], accum_out=ST[:, 4:5])
        kb.act(junk[b_][:, 256:384], H[:, 256:384], AF.Square, r=[H], w=[junk[b_], ST], accum_out=ST[:, 5:6])
        rstd_of(kb, ST[:, 4:5], 1, 256, ST[:, 6:7], ST[:, 8:9], r=[ST], w=[ST])
        rstd_of(kb, ST[:, 5:6], 1, 128, ST[:, 7:8], ST[:, 9:10], r=[ST], w=[ST])
        kb.stt(CQ[:, 0:256], H[:, 0:256], ST[:, 8:9], qan[:], ALU.mult, ALU.mult, r=[H, ST, qan], w=[CQ])
        kb.stt(CQ[:, 256:384], H[:, 256:384], ST[:, 9:10], kvan[:], ALU.mult, ALU.mult, r=[H, ST, kvan], w=[CQ])
        for kc in range(3):
            kb.tr(PS[2][:, 1, kc * 128:(kc + 1) * 128], CQ[:, kc * 128:(kc + 1) * 128], ident[:], r=[CQ, ident], w=[PS[2]])
        kb.copy("act", cT[b_][:].rearrange("p a t -> p (a t)"), PS[2][:, 1, 0:384], r=[PS[2]], w=[cT[b_]])
        if DBG < 4.1:
            continue
        for s in range(2):
            for kc in range(2):
                kb.mm(PS[0][:, s, 0:384], cT[b_][:, kc, :], w_uq[:, kc, s * 384:(s + 1) * 384], kc == 0, kc == 1, r=[cT[b_], w_uq], w=[PS[0]])
        QF, KF = qf[b_], kf[b_]
        kb.copy("act", QF[:].rearrange("p (s a) d -> p s (a d)", s=2), PS[0][:, :, 0:384], r=[PS[0]], w=[QF])
        if DBG < 4.2:
            continue
        for s in range(2):
            kb.mm(PS[1][:, s, :], cT[b_][:, 2, :], w_ukv[:, s * 512:(s + 1) * 512], True, True, r=[cT[b_], w_ukv], w=[PS[1]])
        kvv = PS[1][:].rearrange("p s (a d) -> p (s a) d", d=128)
        if DBG < 4.21:
            continue
        kb.copy("dve", KF[:, :, 0:64], kvv[:, :, 0:64], r=[PS[1]], w=[KF])
        if DBG < 4.22:
            continue
        for s in range(2):
            kb.copy("dve", vs[b_][:, 4 * s:4 * s + 4, :], PS[1][:, s, :].rearrange("p (a d) -> p a d", d=128)[:, :, 64:128], r=[PS[1]], w=[vs[b_]])
        if DBG < 4.23:
            continue
        kb.copy("dve", KF[:, :, 64:96], H[:, 384:416].unsqueeze(1).broadcast_to([128, 8, 32]), r=[H], w=[KF])
        if DBG < 4.3:
            continue
        ridx = None if is_ctx else ti
        if DBG < 4.4:
            ridx = None
        head_norm_rope(QF[:], QF, sq[b_], ST, qnw, ridx, rt[b_], [QF])
        head_norm_rope(KF[:], KF, sq[b_], ST, knw, ridx, rt[b_], [KF])
        if DBG < 5:
            continue
        for (SRC, DST, dname) in ((QF, qTs[b_], "qT"), (KF, kTs[b_], "kT")):
            for h in range(8):
                kb.tr(PS[3][0:96, h // 4, (h % 4) * 128:(h % 4 + 1) * 128], SRC[:, h, :], ident[:], r=[SRC, ident], w=[PS[3]])
            kb.copy("act", DST[:].rearrange("p (a b) t -> p a (b t)", a=2), PS[3][0:96, :, :], r=[PS[3]], w=[DST])
            kb.dma("sp", io[dname][:, :, t0:t0 + 128].rearrange("h d t -> d h t"), DST[:], r=[DST], w=io[dname + "_res"])
        kb.dma("sp", io["v"][t0:t0 + 128, :], vs[b_][:].rearrange("p h d -> p (h d)"), r=[vs[b_]], w=io["v_res"])


class IO(dict):
    pass


def declare(nc, io, name, shape, dt, kind):
    th = nc.dram_tensor(name, list(shape), dt, kind=kind)
    io[name] = th[:] if len(shape) > 0 else th
    io[name + "_res"] = Res(name)
    return th


CONST_SHAPES = {"ident": [128, 128], "triu": [128, 128], "tril": [128, 128], "sel": [32, 32 * 128]}

A_INPUTS = [("x", [NTOK, D]), ("cc", [128, 8, 2]), ("w_mod", [D, 6 * D]), ("b_mod", [6 * D]), ("norm1_w", [D]),
            ("w_in", [D, D_IN]), ("q_a_norm", [256]), ("w_uq", [256, 768]), ("kv_a_norm", [128]),
            ("w_ukv", [128, 1024]), ("q_norm_w", [96]), ("k_norm_w", [96]), ("rope", [TL, 32])]
A_OUTPUTS = [("qT", [8, 96, NTOK], BF16), ("kT", [8, 96, NTOK], BF16), ("v", [NTOK, 512], BF16),
             ("fm", [544, NTOK], F32), ("tm", [NTOK, 1040], F32)]


def build_A():
    nc = bass.Bass("TRN2", target_bir_lowering=False)
    io = IO()
    for nm, shp in A_INPUTS:
        declare(nc, io, nm, shp, F32, "ExternalInput")
    cd = {}
    for nm in ("ident",):
        th = nc.dram_tensor("c_" + nm, CONST_SHAPES[nm], F32, kind="ExternalInput")
        cd[nm] = T(th, "c_" + nm)
    for nm, shp, dt in A_OUTPUTS:
        declare(nc, io, nm, shp, dt, "ExternalOutput")
    with ExitStack() as st:
        kb = KB(nc, st)
        cst = load_consts(kb, cd, ["ident"])
        phase_A(kb, io, cst)
        kb.finish([io[nm + "_res"] for nm, _, _ in A_OUTPUTS])
        print("phase A: n_inst", kb.n_inst, "n_wait", kb.n_wait, "dsems", kb.ndsem)
    return nc


_ROPE = None
_CONSTS = None


def consts():
    global _ROPE, _CONSTS
    if _CONSTS is None:
        _CONSTS = host_consts()
        _ROPE = rope_tables()
    return _CONSTS, _ROPE


def f32(a):
    return np.ascontiguousarray(a, dtype=np.float32)


def cc_layout(c_b, c_ctx):
    cc = np.stack([c_b, c_ctx], axis=-1)
    return f32(cc.reshape(8, 128, 2).transpose(1, 0, 2))


def core_tokens(xl, xc, core):
    b, j = core // 4, core % 4
    return np.concatenate([xl[b, j * TL:(j + 1) * TL], xc[b]], axis=0)


def a_inmaps(inp, l, xl, xc):
    cst, rope = consts()
    maps = []
    for core in range(NCORE):
        b, j = core // 4, core % 4
        m = {"x": f32(core_tokens(xl, xc, core)), "cc": cc_layout(inp["c"][b], inp["c_ctx"]),
             "rope": f32(rope[j * TL:(j + 1) * TL]), "c_ident": cst["ident"]}
        for nm in ("w_mod", "b_mod", "norm1_w", "w_in", "q_a_norm", "w_uq", "kv_a_norm", "w_ukv", "q_norm_w", "k_norm_w"):
            m[nm] = f32(inp[nm][l])
        maps.append(m)
    return maps


B_T = SEQ + NCTX
NCH = B_T // 128
SEQ_F = [64, 65] + list(range(64))
SEQ_B = list(range(65, -1, -1))


def nsl(ns):
    if len(ns) == 1:
        return slice(ns[0], ns[0] + 1)
    if ns[1] == ns[0] + 1:
        return slice(ns[0], ns[-1] + 1)
    stop = ns[-1] - 1
    return slice(ns[0], stop if stop >= 0 else None, -1)


def seq_groups(seq, g):
    groups, cur = [], []
    for n in seq:
        if cur and (len(cur) == g or abs(n - cur[-1]) != 1 or (len(cur) >= 2 and (n - cur[-1]) != (cur[-1] - cur[-2]))):
            groups.append(cur)
            cur = []
        cur.append(n)
    groups.append(cur)
    return groups


def attention_B(kb, io, need_ctx):
    kb.push()
    KT = kb.sb("KT", [96, 2, B_T], BF16)
    QT = kb.sb("QT", [96, 2, SEQ], BF16)
    QC = kb.sb("QC", [96, 2, 256], BF16)
    Vst = kb.sb("Vst", [128, NCH, 128], BF16)
    V = kb.sb("V", [128, NCH, 2, 66], BF16)
    E = kb.sb("E", [65, 64], BF16)
    for hh in range(2):
        kb.dma("sp", KT[:, hh, :], io["KT"][hh], r=[], w=KT)
        kb.dma("sp", QT[:, hh, :], io["QT"][hh], r=[], w=QT)
        kb.dma("sp", QC[:, hh, :], io["QcT"][hh], r=[], w=QC)
    vsrc = io["V"]
    for i in range(0, NCH, 11):
        kb.dma("sp", Vst[:, i:i + 11, :], vsrc[:, i:i + 11, :], r=[], w=Vst)
    cf = kb.sb("cf", [128, 512])
    kb.op("dve", lambda e: e.memset(cf[:], 1.0), r=[], w=[cf])
    kb.copy("dve", V[:, :, :, 64:65], cf[:, 0:NCH * 2].rearrange("p (n h o) -> p n h o", h=2, o=1), r=[cf], w=[V])
    kb.copy("dve", V[:, :, :, 0:64], Vst[:].rearrange("p n (h d) -> p n h d", h=2), r=[Vst], w=[V])
    Ef = kb.sb("Ef", [65, 64])
    kb.op("dve", lambda e: e.memset(Ef[:], 0.0), r=[], w=[Ef])
    kb.op("dve", lambda e: e.memset(Ef[64:65, :], 1.0), r=[], w=[Ef])
    kb.copy("dve", E[:], Ef[:], r=[Ef], w=[E])
    zf = kb.sb("zf", [65, 512])
    kb.op("dve", lambda e: e.memset(zf[:], 0.0), r=[], w=[zf])
    pst = [kb.ps("pst%d" % i, [128, 2, 512]) for i in range(3)]
    pot = [kb.ps("pot0", [128, 512])] * 2
    pbc = kb.ps("pbc", [128, 512])
    PT = [kb.sb("PT%d" % i, [128, 2, 512], BF16) for i in range(4)]
    osb = [kb.sb("osb%d" % i, [65, 512]) for i in range(2)]
    rd = [kb.sb("rd%d" % i, [65, 512]) for i in range(2)]
    rh = [kb.sb("rh%d" % i, [65, 512], BF16) for i in range(2)]
    rl = [kb.sb("rl%d" % i, [65, 512], BF16) for i in range(2)]
    for t in rh + rl:
        kb.copy("dve", t[:], zf[:], r=[zf], w=[t])
    oT = [kb.sb("oT%d" % i, [64, 512], BF16) for i in range(2)]
    for t in rd:
        kb.op("dve", lambda e: e.memset(t[:], 0.0), r=[], w=[t])
    cnt = {"it": 0, "blk": 0}

    pending = []

    def finalize(po, ob, rdt, rht, rlt, ot, nq, out_ap):
        kb.copy("act", ob[:, 0:nq], po[0:65, 0:nq], r=[po], w=[ob])
        kb.op("dve", lambda e: e.reciprocal(out=rdt[64:65, 0:nq], in_=ob[64:65, 0:nq]), r=[ob], w=[rdt])
        kb.copy("dve", rht[64:65, 0:nq], rdt[64:65, 0:nq], r=[rdt], w=[rht])
        kb.tt(rlt[64:65, 0:nq], rdt[64:65, 0:nq], rht[64:65, 0:nq], ALU.subtract, r=[rdt, rht], w=[rlt])
        kb.mm(pbc[0:64, 0:nq], E[:, :], rht[:, 0:nq], True, False, r=[E, rht], w=[pbc])
        kb.mm(pbc[0:64, 0:nq], E[:, :], rlt[:, 0:nq], False, True, r=[E, rlt], w=[pbc])
        kb.tt(ot[:, 0:nq], pbc[0:64, 0:nq], ob[0:64, 0:nq], ALU.mult, r=[ob, pbc], w=[ot])
        kb.dma("sp", out_ap, ot[:, 0:nq], r=[ot], w=io["mixT_res"])

    def block(q_ap, qres, nq, key_tiles, hh, out_ap):
        k_ = cnt["blk"] % 2
        po, ob, rdt, rht, rlt, ot = pot[k_], osb[k_], rd[k_], rh[k_], rl[k_], oT[k_]
        cnt["blk"] += 1
        nk = len(key_tiles)
        assert nk % 2 == 0
        pairs = [key_tiles[i:i + 2] for i in range(0, nk, 2)]
        prev = None

        def pv(pi, pr, ppt, last):
            for a, pkt in enumerate(pr):
                kb.mm(po[0:65, 0:nq], V[:, pkt, hh, 0:65], ppt[:, a, 0:nq], pi == 0 and a == 0, last and a == 1, r=[V, ppt], w=[po])

        queue = []
        for i, pr in enumerate(pairs):
            ps_ = pst[cnt["it"] % 3]
            pt = PT[cnt["it"] % 4]
            cnt["it"] += 1
            for a, kt in enumerate(pr):
                kb.mm(ps_[:, a, 0:nq], KT[:, hh, kt * 128:(kt + 1) * 128], q_ap, True, True, r=[KT, qres], w=[ps_])
            kb.act(pt[:, :, 0:nq], ps_[:, :, 0:nq], AF.Exp, r=[ps_], w=[pt])
            if i == 1 and pending:
                finalize(*pending.pop())
            queue.append((i, pr, pt))
            if len(queue) > 2:
                pv(*queue.pop(0), False)
        if pending:
            finalize(*pending.pop())
        while queue:
            q_ = queue.pop(0)
            pv(*q_, len(queue) == 0)
        if pending:
            finalize(*pending.pop())
        pending.append((po, ob, rdt, rht, rlt, ot, nq, out_ap))

    for hh in range(2):
        for qb in range(SEQ // 512):
            block(QT[:, hh, qb * 512:(qb + 1) * 512], QT, 512, list(range(NCH)), hh,
                  io["mixT"][hh * 64:(hh + 1) * 64, qb * 512:(qb + 1) * 512])
        if need_ctx:
            block(QC[:, hh, :], QC, 256, [64, 65], hh, io["mixT"][hh * 64:(hh + 1) * 64, SEQ:B_T])
    while pending:
        finalize(*pending.pop())
    kb.pop()


def lin_attn(kb, cst, dk, nv, qT, kT, ktok, vp, a, a_on_G, dirn, emit):
    kb.push()
    seq = SEQ_F if dirn == 0 else SEQ_B
    mask = cst["triu"] if dirn == 0 else cst["tril"]
    Gs = kb.sb("Gs", [dk, nv, NCH])
    Ar = kb.sb("Ar", [dk, nv, NCH])
    Cs = kb.sb("Cs", [dk, nv, NCH], BF16)
    pg = [kb.ps("pg%d" % i, [128, 512]) for i in range(2)]
    pp = [kb.ps("pp%d" % i, [128, 512]) for i in range(2)]
    po = [kb.ps("po%d" % i, [128, 512]) for i in range(2)]
    PTm = [kb.sb("PTm%d" % i, [128, 128], BF16) for i in range(3)]
    gsz = 512 // nv
    for gi, s0 in enumerate(range(0, NCH, gsz)):
        pgt = pg[gi % 2]
        ss = list(range(s0, min(NCH, s0 + gsz)))
        for i, s in enumerate(ss):
            n = seq[s]
            kb.mm(pgt[0:dk, i * nv:(i + 1) * nv], ktok[:, n, :], vp[:, n, :], True, True, r=[ktok, vp], w=[pgt])
        if DBG < 10.6:
            continue
        for i, s in enumerate(ss):
            n = seq[s]
            if a_on_G:
                kb.tt(Gs[:, :, s], pgt[0:dk, i * nv:(i + 1) * nv], a[:, n:n + 1].broadcast_to([dk, nv]), ALU.mult, r=[pgt, a], w=[Gs])
            else:
                kb.copy("dve", Gs[:, :, s], pgt[0:dk, i * nv:(i + 1) * nv], r=[pgt], w=[Gs])
    if DBG < 10.61:
        kb.pop()
        return
    if dirn == 0:
        kb.copy("pool", Ar[:, :, 2:NCH], a[:, 0:64].unsqueeze(1).broadcast_to([dk, nv, 64]), r=[a], w=[Ar])
        kb.copy("pool", Ar[:, :, 0:2], a[:, 64:66].unsqueeze(1).broadcast_to([dk, nv, 2]), r=[a], w=[Ar])
    else:
        kb.copy("pool", Ar[:], a[:, ::-1].unsqueeze(1).broadcast_to([dk, nv, NCH]), r=[a], w=[Ar])
    kb.op("pool", lambda e: e.memset(Ar[:, :, 0:1], 0.0), r=[], w=[Ar])
    kb.op("dve", lambda e: e.tensor_tensor_scan(out=Cs[:].rearrange("p v s -> p (v s)"), data0=Ar[:].rearrange("p v s -> p (v s)"),
                                                 data1=Gs[:].rearrange("p v s -> p (v s)"), initial=0.0, op0=ALU.mult, op1=ALU.add),
          r=[Ar, Gs], w=[Cs])
    if DBG < 10.62:
        kb.pop()
        return
    it = 0

    def issue_pv(pot, i, n, ptm):
        s = seq.index(n)
        t0 = n * 128
        kb.mm(pot[:, i * nv:(i + 1) * nv], ptm[:], vp[:, n, :], True, s == 0, r=[ptm, vp], w=[pot])
        if s > 0:
            kb.mm(pot[:, i * nv:(i + 1) * nv], qT[:, t0:t0 + 128], Cs[:, :, s - 1], False, True, r=[qT, Cs], w=[pot])

    for gi, ns in enumerate(seq_groups(seq, gsz)):
        pot = po[gi % 2]
        prev = None
        for i, n in enumerate(ns):
            t0 = n * 128
            ppt = pp[it % 2]
            ptm = PTm[it % 3]
            it += 1
            kb.mm(ppt[:, 0:128], kT[:, t0:t0 + 128], qT[:, t0:t0 + 128], True, True, r=[kT, qT], w=[ppt])
            kb.tt(ptm[:], ppt[:, 0:128], mask[:], ALU.mult, r=[ppt, mask], w=[ptm])
            if prev is not None:
                issue_pv(pot, *prev)
            prev = (i, n, ptm)
        issue_pv(pot, *prev)
        emit(pot, ns)
    kb.pop()


def out_norm_gate(kb, cst, hsum, nw_d, gate_d, gate_func, extra, mix_rows, io, name):
    kb.push()
    nw = bcast_load(kb, name + "nw", nw_d, 64)
    gt = kb.sb(name + "gt", [128, NCH, 64])
    kb.dma("sp", gt[:], gate_d, r=[], w=gt)
    sq = kb.sb(name + "sq", [128, NCH, 64])
    st = kb.sb(name + "st", [128, 3, NCH])
    kb.act(sq[:], hsum[:], AF.Square, r=[hsum], w=[sq])
    kb.op("dve", lambda e: e.tensor_reduce(out=st[:, 0, :], in_=sq[:], axis=AX.X, op=ALU.add), r=[sq], w=[st])
    rstd_of(kb, st[:, 0, :], NCH, 64, st[:, 1, :], st[:, 2, :], r=[st], w=[st])
    if DBG < 10.71:
        kb.pop()
        return
    kb.tt(hsum[:], hsum[:], st[:, 2, :].unsqueeze(2).broadcast_to([128, NCH, 64]), ALU.mult, r=[hsum, st], w=[hsum])
    kb.tt(hsum[:], hsum[:], nw[:].unsqueeze(1).broadcast_to([128, NCH, 64]), ALU.mult, r=[hsum, nw], w=[hsum])
    if extra is not None:
        kb.tt(hsum[:], hsum[:], extra[:], ALU.add, r=[hsum, extra], w=[hsum])
    kb.act(gt[:], gt[:], gate_func, r=[gt], w=[gt])
    kb.tt(hsum[:], hsum[:], gt[:], ALU.mult, r=[hsum, gt], w=[hsum])
    if DBG < 10.72:
        kb.pop()
        return
    oT = kb.sb(name + "oT", [64, B_T], BF16)
    ptr = [kb.ps(name + "ptr%d" % i, [128, 512]) for i in range(2)]
    for gi, n0 in enumerate(range(0, NCH, 4)):
        nn = min(4, NCH - n0)
        p = ptr[gi % 2]
        for i in range(nn):
            kb.tr(p[0:64, i * 128:(i + 1) * 128], hsum[:, n0 + i, :], cst["ident"][:], r=[hsum, cst["ident"]], w=[p])
        kb.copy("act", oT[:, n0 * 128:(n0 + nn) * 128], p[0:64, 0:nn * 128], r=[p], w=[oT])
    if DBG < 10.73:
        kb.pop()
        return
    for i in range(4):
        kb.dma("sp", io["mixT"][mix_rows:mix_rows + 64, i * 2112:(i + 1) * 2112], oT[:, i * 2112:(i + 1) * 2112], r=[oT], w=io["mixT_res"])
    kb.pop()


def mlstm_B(kb, io, cst):
    kb.push()
    ident = cst["ident"]
    qT = kb.sb("mqT", [64, B_T], BF16)
    kT = kb.sb("mkT", [64, B_T], BF16)
    ktok = kb.sb("mktok", [128, NCH, 64], BF16)
    xtok = kb.sb("mxtok", [128, NCH, 64])
    kb.push()
    mx = kb.sb("mx", [64, B_T])
    acc = kb.sb("macc", [64, B_T])
    xcb = kb.sb("mxcb", [64, B_T], BF16)
    cw = kb.sb("cw", [64, 5])
    cb = kb.sb("cb", [64, 1])
    wq = kb.sb("wq", [64, 64], BF16)
    wk = kb.sb("wk", [64, 64], BF16)
    kb.dma("sp", cw[:], io["cw"], r=[], w=cw)
    kb.dma("sp", cb[:], io["cb"], r=[], w=cb)
    kb.dma("pool", wq[:], io["wq"], r=[], w=wq)
    kb.dma("pool", wk[:], io["wk"], r=[], w=wk)
    for i in range(4):
        kb.dma("sp", mx[:, i * 2112:(i + 1) * 2112], io["mxT"][:, i * 2112:(i + 1) * 2112], r=[], w=mx)
    for (s0, ln) in ((0, SEQ), (SEQ, NCTX)):
        kb.ts(acc[:, s0:s0 + ln], mx[:, s0:s0 + ln], cw[:, 2:3], None, ALU.mult, r=[mx, cw], w=[acc])
        for k in (0, 1, 3, 4):
            sh = k - 2
            a0 = max(0, -sh)
            a1 = ln - max(0, sh)
            kb.stt(acc[:, s0 + a0:s0 + a1], mx[:, s0 + a0 + sh:s0 + a1 + sh], cw[:, k:k + 1], acc[:, s0 + a0:s0 + a1],
                   ALU.mult, ALU.add, r=[mx, cw, acc], w=[acc])
    if DBG < 10.1:
        return
    kb.act(acc[:], acc[:], AF.Silu, r=[acc, cb], w=[acc], bias=cb[:, 0:1])
    kb.copy("dve", xcb[:], acc[:], r=[acc], w=[xcb])
    if DBG < 10.2:
        return
    pj = [kb.ps("mpj%d" % i, [128, 512]) for i in range(2)]
    gi = 0
    for c0 in range(0, B_T, 512):
        w_ = min(512, B_T - c0)
        p = pj[gi % 2]; gi += 1
        kb.mm(p[0:64, 0:w_], wq[:], xcb[:, c0:c0 + w_], True, True, r=[wq, xcb], w=[p])
        kb.op("act", lambda e: e.mul(qT[:, c0:c0 + w_], p[0:64, 0:w_], 0.125), r=[p], w=[qT])
        p = pj[gi % 2]; gi += 1
        kb.mm(p[0:64, 0:w_], wk[:], xcb[:, c0:c0 + w_], True, True, r=[wk, xcb], w=[p])
        kb.copy("dve", kT[:, c0:c0 + w_], p[0:64, 0:w_], r=[p], w=[kT])
    if DBG < 10.3:
        return
    for n0 in range(0, NCH, 8):
        nn = min(8, NCH - n0)
        p = pj[gi % 2]; gi += 1
        for i in range(nn):
            n = n0 + i
            kb.mm(p[:, i * 64:(i + 1) * 64], xcb[:, n * 128:(n + 1) * 128], wk[:], True, True, r=[xcb, wk], w=[p])
        kb.copy("dve", ktok[:, n0:n0 + nn, :].rearrange("p n d -> p (n d)"), p[:, 0:nn * 64], r=[p], w=[ktok])
        p = pj[gi % 2]; gi += 1
        for i in range(nn):
            n = n0 + i
            kb.tr(p[:, i * 64:(i + 1) * 64], acc[:, n * 128:(n + 1) * 128], ident[0:64, 0:64], r=[acc, ident], w=[p])
        kb.copy("act", xtok[:, n0:n0 + nn, :].rearrange("p n d -> p (n d)"), p[:, 0:nn * 64], r=[p], w=[xtok])
    kb.pop()
    if DBG < 10.4:
        return
    mg = kb.sb("mg", [128, NCH, 4])
    kb.dma("sp", mg[:], io["mg"], r=[], w=mg)
    gb = bcast_load(kb, "gb", io["gb"], 4)
    gp = kb.sb("gp", [128, 4, NCH])
    for g in range(4):
        kb.ts(gp[:, g, :], mg[:, :, g], gb[:, g:g + 1], None, ALU.add, r=[mg, gb], w=[gp])
    if DBG < 10.41:
        return
    lf = kb.sb("lf", [128, 2, NCH])
    for d in range(2):
        kb.act(lf[:, d, :], gp[:, 1 + 2 * d, :], AF.Sigmoid, r=[gp], w=[lf])
    kb.act(lf[:], lf[:], AF.Ln, r=[lf], w=[lf])
    if DBG < 10.42:
        return
    ones = kb.sb("ones", [128, 64], BF16)
    onesf = kb.sb("onesf", [128, 64])
    kb.op("dve", lambda e: e.memset(onesf[:], 1.0), r=[], w=[onesf])
    kb.copy("dve", ones[:], onesf[:], r=[onesf], w=[ones])
    lfh = kb.sb("lfh", [128, 2, NCH], BF16)
    lfl = kb.sb("lfl", [128, 2, NCH], BF16)
    kb.copy("dve", lfh[:], lf[:], r=[lf], w=[lfh])
    if DBG < 10.421:
        return
    kb.tt(lfl[:], lf[:], lfh[:], ALU.subtract, r=[lf, lfh], w=[lfl])
    if DBG < 10.422:
        return
    pgt = kb.ps("mpgate", [128, 4, 128])
    rr = kb.sb("mr", [128, 2, NCH])
    uu = kb.sb("mu", [128, 2, NCH])
    aa = [kb.sb("ma%d" % d, [64, NCH]) for d in range(2)]
    for d in range(2):
        tri = cst["triu_b"] if d == 0 else cst["tril_b"]
        kb.mm(pgt[:, d, 0:NCH], tri[:], lfh[:, d, :], True, False, r=[tri, lfh], w=[pgt])
        kb.mm(pgt[:, d, 0:NCH], tri[:], lfl[:, d, :], False, True, r=[tri, lfl], w=[pgt])
        if DBG < 10.423:
            continue
        kb.mm(pgt[0:64, 2 + d, 0:NCH], ones[:], lfh[:, d, :], True, False, r=[ones, lfh], w=[pgt])
        kb.mm(pgt[0:64, 2 + d, 0:NCH], ones[:], lfl[:, d, :], False, True, r=[ones, lfl], w=[pgt])
    if DBG < 10.43:
        return
    for d in range(2):
        kb.act(rr[:, d, :], pgt[:, d, 0:NCH], AF.Exp, r=[pgt], w=[rr])
        if DBG < 10.432:
            continue
        kb.stt(uu[:, d, :], pgt[:, d, 0:NCH], -1.0, gp[:, 2 * d, :], ALU.mult, ALU.add, r=[gp, pgt], w=[uu])
        if DBG < 10.433:
            continue
        kb.act(aa[d][:], pgt[0:64, 2 + d, 0:NCH], AF.Exp, r=[pgt], w=[aa[d]])
    if DBG < 10.44:
        return
    kb.act(uu[:], uu[:], AF.Exp, r=[uu], w=[uu])
    if DBG < 10.5:
        return
    vaug = kb.sb("vaug", [128, NCH, 66])
    kb.op("dve", lambda e: e.memset(vaug[:], 1.0), r=[], w=[vaug])
    if DBG < 10.501:
        return
    hsum = kb.sb("hsum", [128, NCH, 64])
    kb.dma("sp", hsum[:], io["mv"], r=[], w=hsum)
    if DBG < 10.502:
        return
    kb.copy("dve", vaug[:, :, 0:64], hsum[:], r=[hsum], w=[vaug])
    if DBG < 10.51:
        return
    vp = kb.sb("vp", [128, NCH, 66], BF16)
    vtmp = kb.sb("vtmp", [128, NCH, 66])
    dt_ = kb.sb("mdt", [128, 4, 8])
    htmp = kb.sb("htmp", [128, 7, 64])
    for d in range(2):
        kb.tt(vtmp[:], vaug[:], uu[:, d, :].unsqueeze(2).broadcast_to([128, NCH, 66]), ALU.mult, r=[vaug, uu], w=[vtmp])
        kb.copy("dve", vp[:], vtmp[:], r=[vtmp], w=[vp])

        def emit(pot, ns, d=d):
            g = len(ns)
            sl = nsl(ns)
            pv = pot[:, 0:g * 66].rearrange("p (g v) -> p g v", v=66)
            kb.tt(dt_[:, 0, 0:g], pv[:, :, 64], rr[:, d, sl], ALU.mult, r=[pot, rr], w=[dt_])
            kb.ts(dt_[:, 1, 0:g], dt_[:, 0, 0:g], -1.0, None, ALU.mult, r=[dt_], w=[dt_])
            kb.tt(dt_[:, 1, 0:g], dt_[:, 1, 0:g], dt_[:, 0, 0:g], ALU.max, r=[dt_], w=[dt_])
            kb.ts(dt_[:, 1, 0:g], dt_[:, 1, 0:g], 1.0, None, ALU.max, r=[dt_], w=[dt_])
            kb.op("dve", lambda e: e.reciprocal(out=dt_[:, 2, 0:g], in_=dt_[:, 1, 0:g]), r=[dt_], w=[dt_])
            kb.tt(dt_[:, 3, 0:g], dt_[:, 2, 0:g], rr[:, d, sl], ALU.mult, r=[dt_, rr], w=[dt_])
            if d == 0:
                kb.tt(hsum[:, sl, :], pv[:, :, 0:64], dt_[:, 3, 0:g].unsqueeze(2).broadcast_to([128, g, 64]), ALU.mult, r=[pot, dt_], w=[hsum])
            else:
                kb.tt(htmp[:, 0:g, :], pv[:, :, 0:64], dt_[:, 3, 0:g].unsqueeze(2).broadcast_to([128, g, 64]), ALU.mult, r=[pot, dt_], w=[htmp])
                kb.tt(hsum[:, sl, :], hsum[:, sl, :], htmp[:, 0:g, :], ALU.add, r=[hsum, htmp], w=[hsum], eng="pool")
        if DBG < 10.52:
            continue
        lin_attn(kb, cst, 64, 66, qT, kT, ktok, vp, aa[d], True, d, emit)
    if DBG < 10.7:
        return
    sk = bcast_load(kb, "msk", io["msk"], 64)
    kb.tt(xtok[:], xtok[:], sk[:].unsqueeze(1).broadcast_to([128, NCH, 64]), ALU.mult, r=[xtok, sk], w=[xtok])
    out_norm_gate(kb, cst, hsum, io["mnw"], io["mo"], AF.Sigmoid, xtok, 128, io, "mo")
    kb.pop()


def gla_B(kb, io, cst):
    kb.push()
    ident = cst["ident"]
    gvb = kb.sb("gvb", [128, NCH, 64], BF16)
    osum = kb.sb("osum", [128, NCH, 64])
    kb.dma("pool", gvb[:], io["gv"], r=[], w=gvb)
    ba = kb.sb("ba", [32, 2])
    kb.dma("sp", ba[:], io["ba"], r=[], w=ba)
    NP = 22
    rst = kb.sb("rst", [32, NP, 128])
    kb.op("pool", lambda e: e.memset(rst[:], 1.0), r=[], w=[rst])
    kb.op("pool", lambda e: e.memset(rst[:, :, 0:1], 0.0), r=[], w=[rst])
    qTt = kb.sb("gqT", [32, B_T], BF16)
    kTt = kb.sb("gkT", [32, B_T], BF16)
    ktok = kb.sb("gktok", [128, NCH, 32], BF16)
    a = kb.sb("ga", [32, NCH])
    for d in range(2):
        wa = kb.sb("wa%d" % d, [16, 32], BF16)
        kb.dma("pool", wa[:], io["wa"][d], r=[], w=wa)
        for n0 in range(0, NCH, NP):
            kb.push()
            c0 = n0 * 128
            cw_ = NP * 128
            ga = kb.sb("gain", [16, cw_], BF16)
            gq = kb.sb("gq", [32, cw_])
            gk = kb.sb("gk", [32, cw_])
            kb.dma("pool", ga[:], io["gaT"][d][:, c0:c0 + cw_], r=[], w=ga)
            kb.dma("sp", gq[:], io["gqT"][:, c0:c0 + cw_], r=[], w=gq)
            kb.dma("sp", gk[:], io["gkT"][:, c0:c0 + cw_], r=[], w=gk)
            la = kb.sb("la", [32, NP, 128])
            P = kb.sb("P", [32, NP, 128])
            Dm = kb.sb("Dm", [32, NP, 128])
            Ex = kb.sb("Ex", [32, NP, 128])
            khat = kb.sb("khat", [32, NP, 128])
            laf = la[:].rearrange("p n t -> p (n t)")
            Exf = Ex[:].rearrange("p n t -> p (n t)")
            pp = [kb.ps("gpp%d" % i, [128, 512]) for i in range(2)]
            for gi, x0 in enumerate(range(0, cw_, 512)):
                w_ = min(512, cw_ - x0)
                p = pp[gi % 2]
                kb.mm(p[0:32, 0:w_], wa[:], ga[:, x0:x0 + w_], True, True, r=[wa, ga], w=[p])
                kb.act(laf[:, x0:x0 + w_], p[0:32, 0:w_], AF.Sigmoid, r=[p, ba], w=[la], bias=ba[:, d:d + 1])
            kb.act(la[:], la[:], AF.Ln, r=[la], w=[la])
            kb.op("dve", lambda e: e.tensor_tensor_scan(out=P[:].rearrange("p n t -> p (n t)"), data0=rst[:].rearrange("p n t -> p (n t)"),
                                                         data1=laf, initial=0.0, op0=ALU.mult, op1=ALU.add), r=[rst, la], w=[P])
            kb.act(a[:, n0:n0 + NP], P[:, :, 127], AF.Exp, r=[P], w=[a], scale=1.0 / 16)
            kb.tt(Dm[:], P[:, :, 127:128].broadcast_to([32, NP, 128]), P[:], ALU.subtract, r=[P], w=[Dm])
            if d == 0:
                Bq = P
                Bke = Dm
            else:
                kb.tt(Dm[:], Dm[:], la[:], ALU.add, r=[Dm, la], w=[Dm])
                kb.tt(P[:], P[:], la[:], ALU.subtract, r=[P, la], w=[P])
                Bq = Dm
                Bke = P
            kb.act(Ex[:], Bq[:], AF.Exp, r=[Bq], w=[Ex], scale=1.0 / 16)
            kb.stt(qTt[:, c0:c0 + cw_], gq[:], float(32 ** -0.5), Exf, ALU.mult, ALU.mult, r=[gq, Ex], w=[qTt])
            kb.act(Ex[:], Bq[:], AF.Exp, r=[Bq], w=[Ex], scale=-1.0 / 16)
            kb.tt(kTt[:, c0:c0 + cw_], gk[:], Exf, ALU.mult, r=[gk, Ex], w=[kTt])
            kb.act(Ex[:], Bke[:], AF.Exp, r=[Bke], w=[Ex], scale=1.0 / 16)
            kb.tt(khat[:].rearrange("p n t -> p (n t)"), gk[:], Exf, ALU.mult, r=[gk, Ex], w=[khat])
            for gi, m0 in enumerate(range(0, NP, 16)):
                nn = min(16, NP - m0)
                p = pp[gi % 2]
                for i in range(nn):
                    kb.tr(p[:, i * 32:(i + 1) * 32], khat[:, m0 + i, :], ident[0:32, 0:32], r=[khat, ident], w=[p])
                kb.copy("dve", ktok[:, n0 + m0:n0 + m0 + nn, :].rearrange("p n d -> p (n d)"), p[:, 0:nn * 32], r=[p], w=[ktok])
            kb.pop()

        def emit(pot, ns, d=d):
            g = len(ns)
            sl = nsl(ns)
            pv = pot[:, 0:g * 64].rearrange("p (g v) -> p g v", v=64)
            if d == 0:
                kb.copy("dve", osum[:, sl, :], pv, r=[pot], w=[osum])
            else:
                kb.tt(osum[:, sl, :], pv, osum[:, sl, :], ALU.add, r=[osum, pot], w=[osum])
        lin_attn(kb, cst, 32, 64, qTt, kTt, ktok, gvb, a, False, d, emit)
    out_norm_gate(kb, cst, osum, io["gnw"], io["gr"], AF.Silu, None, 192, io, "go")
    kb.pop()


B_INPUTS = [("QT", [2, 96, SEQ], BF16), ("QcT", [2, 96, NCTX], BF16), ("KT", [2, 96, B_T], BF16), ("V", [128, NCH, 128], BF16),
            ("mxT", [64, B_T], F32), ("gqT", [32, B_T], F32), ("gkT", [32, B_T], F32), ("gaT", [2, 16, B_T], F32),
            ("mg", [128, NCH, 4], F32), ("mv", [128, NCH, 64], F32), ("mo", [128, NCH, 64], F32), ("gv", [128, NCH, 64], F32), ("gr", [128, NCH, 64], F32),
            ("cw", [64, 5], F32), ("cb", [64, 1], F32), ("wq", [64, 64], F32), ("wk", [64, 64], F32), ("gb", [4], F32),
            ("mnw", [64], F32), ("msk", [64], F32), ("wa", [2, 16, 32], F32), ("ba", [32, 2], F32), ("gnw", [64], F32)]


def build_B(need_ctx=True, parts=("attn", "mlstm", "gla")):
    nc = bass.Bass("TRN2", target_bir_lowering=False)
    io = IO()
    for nm, shp, dt in B_INPUTS:
        declare(nc, io, nm, shp, dt, "ExternalInput")
    cd = {}
    for nm in ("ident", "triu", "tril"):
        th = nc.dram_tensor("c_" + nm, CONST_SHAPES[nm], F32, kind="ExternalInput")
        cd[nm] = T(th, "c_" + nm)
    declare(nc, io, "mixT", [256, B_T], BF16, "ExternalOutput")
    with ExitStack() as st:
        kb = KB(nc, st)
        cst = load_consts(kb, cd, ["ident", "triu", "tril"])
        if "mlstm" in parts:
            mlstm_B(kb, io, cst)
        if "gla" in parts:
            gla_B(kb, io, cst)
        if "attn" in parts:
            attention_B(kb, io, need_ctx)
        while len(kb.stacks) > 1:
            kb.stacks.pop().close()
        kb.finish([io["mixT_res"]])
        print("phase B: n_inst", kb.n_inst, "n_wait", kb.n_wait, "dsems", kb.ndsem)
    return nc


def b_inmaps(inp, l, aout):
    cst, _ = consts()
    maps = []
    for b in range(NB):
        cs = [aout[4 * b + j] for j in range(4)]

        def cat_t(name, axis):
            parts = [np.take(c[name], np.arange(0, TL), axis=axis) for c in cs]
            parts.append(np.take(cs[0][name], np.arange(TL, NTOK), axis=axis))
            return np.concatenate(parts, axis=axis)
        qT = cat_t("qT", 2)
        kT = cat_t("kT", 2)
        v = cat_t("v", 0)
        fm = cat_t("fm", 1)
        tm = cat_t("tm", 0)
        for j in range(4):
            def pm(a):
                return np.ascontiguousarray(a.reshape(NCH, 128, a.shape[-1]).transpose(1, 0, 2))
            m = {"QT": np.ascontiguousarray(qT[2 * j:2 * j + 2, :, 0:SEQ]), "QcT": np.ascontiguousarray(qT[2 * j:2 * j + 2, :, SEQ:]),
                 "KT": np.ascontiguousarray(kT[2 * j:2 * j + 2]), "V": pm(v[:, 128 * j:128 * j + 128]),
                 "mxT": f32(fm[64 * j:64 * j + 64]), "gqT": f32(fm[256 + 32 * j:256 + 32 * j + 32]),
                 "gkT": f32(fm[384 + 32 * j:384 + 32 * j + 32]), "gaT": f32(fm[512:544].reshape(2, 16, B_T)),
                 "mg": pm(f32(tm[:, [j, 4 + j, 8 + j, 12 + j]])), "mv": pm(f32(tm[:, 16 + 64 * j:16 + 64 * j + 64])),
                 "mo": pm(f32(tm[:, 272 + 64 * j:272 + 64 * j + 64])), "gv": pm(f32(tm[:, 528 + 64 * j:528 + 64 * j + 64])),
                 "gr": pm(f32(tm[:, 784 + 64 * j:784 + 64 * j + 64])),
                 "cw": f32(inp["ml_conv_w"][l][:, 64 * j:64 * j + 64].T), "cb": f32(inp["ml_conv_b"][l][64 * j:64 * j + 64][:, None]),
                 "wq": f32(inp["ml_wq"][l][j]), "wk": f32(inp["ml_wk"][l][j]),
                 "gb": f32(inp["ml_gate_b"][l][[j, 4 + j, 8 + j, 12 + j]]),
                 "mnw": f32(inp["ml_norm_w"][l][64 * j:64 * j + 64]), "msk": f32(inp["ml_skip"][l][64 * j:64 * j + 64]),
                 "wa": f32(inp["gla_wa"][l][:, :, 32 * j:32 * j + 32]), "ba": f32(inp["gla_ba"][l][:, 32 * j:32 * j + 32].T),
                 "gnw": f32(inp["gla_norm_w"][l][64 * j:64 * j + 64]),
                 "c_ident": cst["ident"], "c_triu": cst["triu"], "c_tril": cst["tril"]}
            maps.append(m)
    return maps


def phase_C(kb, io, cst):
    ident = cst["ident"]
    NE = 32
    x1 = kb.sb("x1_all", [128, NT, 1024])
    h2T = kb.sb("h2T_all", [128, 8, NTOK], BF16)
    gTh = kb.sb("gTh", [32, NTOK], BF16)
    gTl = kb.sb("gTl", [32, NTOK], BF16)
    selb = kb.sb("selb", [32, NE * 128], BF16)
    kb.dma("pool", selb[:], cst["sel_d"], r=[], w=selb)
    gate2 = [kb.sb("gate2_%d" % i, [128, 1024]) for i in range(2)]
    kb.push()
    PS = [kb.ps("psC%d" % i, [128, 2, 512]) for i in range(2)]
    plg = kb.ps("plg", [128, 512])
    pgt = kb.ps("pgtC", [128, 512])
    mods = compute_mod(kb, io["cc"], io["w_mod"], io["b_mod"], [2, 3, 4, 5], PS, pre={5: (gate2[0], gate2[1])})
    n2 = bcast_load(kb, "n2", io["norm2_w"], 1024)
    G2 = []
    for i in range(2):
        g = mods[4][i]
        kb.stt(g[:], g[:], 1.0, n2[:], ALU.add, ALU.mult, r=[g, n2], w=[g])
        G2.append(g)
    gate1 = mods[2]
    S2 = mods[3]
    w_out = kb.sb("w_out", [128, 8, 1024], BF16)
    for kc in range(8):
        kb.dma("pool", w_out[:, kc, :], io["w_out"][kc * 128:(kc + 1) * 128, :], r=[], w=w_out)
    wr = kb.sb("wr", [128, 8, 36])
    kb.dma("sp", wr[:, :, 0:4], io["w_grp"].rearrange("(k p) n -> p k n", p=128), r=[], w=wr)
    kb.dma("sp", wr[:, :, 4:36], io["w_erouter"].rearrange("(k p) n -> p k n", p=128), r=[], w=wr)
    wrh = kb.sb("wrh", [128, 8, 36], BF16)
    wrl = kb.sb("wrl", [128, 8, 36], BF16)
    kb.copy("dve", wrh[:], wr[:], r=[wr], w=[wrh])
    kb.tt(wrl[:], wr[:], wrh[:], ALU.subtract, r=[wr, wrh], w=[wrl])
    rb = kb.sb("rb", [128, 36])
    kb.dma("sp", rb[:, 0:4], io["b_grp"].partition_broadcast(128), r=[], w=rb)
    kb.dma("sp", rb[:, 4:36], io["b_erouter"].partition_broadcast(128), r=[], w=rb)
    NB_ = 2
    xt = [kb.sb("xtC%d" % i, [128, 1024]) for i in range(NB_)]
    mT = [kb.sb("mTC%d" % i, [128, 8, 128], BF16) for i in range(NB_)]
    tmp = [kb.sb("tmpC", [128, 1024])] * NB_
    h2 = [kb.sb("h2C", [128, 1024])] * NB_
    h2l = [kb.sb("h2l", [128, 8, 128], BF16)] * NB_
    st = [kb.sb("stC%d" % i, [128, 16]) for i in range(NB_)]
    lg = [kb.sb("lg%d" % i, [128, 36]) for i in range(NB_)]
    rw = [kb.sb("rw%d" % i, [128, 6, 32]) for i in range(NB_)]
    m8 = [kb.sb("m8_%d" % i, [128, 8]) for i in range(NB_)]
    for ti in range(NT):
        b_ = ti % NB_
        mi = 1 if ti >= 16 else 0
        t0 = ti * 128
        X, MT, TMP, H2, H2L, ST, LG, RW, M8 = xt[b_], mT[b_], tmp[b_], h2[b_], h2l[b_], st[b_], lg[b_], rw[b_], m8[b_]
        kb.dma("sp", X[:], io["x"][t0:t0 + 128, :], r=[], w=X)
        kb.dma("sp", MT[:], io["mixT"][:, t0:t0 + 128].rearrange("(k p) t -> p k t", p=128), r=[], w=MT)
        for hf in range(2):
            for kc in range(8):
                kb.mm(PS[0][:, hf, :], MT[:, kc, :], w_out[:, kc, hf * 512:(hf + 1) * 512], kc == 0, kc == 7, r=[MT, w_out], w=[PS[0]])
        X1 = x1[:, ti, :]
        kb.tt(TMP[:], PS[0][:].rearrange("p a b -> p (a b)"), gate1[mi][:], ALU.mult, r=[PS[0], gate1[mi]], w=[TMP])
        kb.tt(X1, TMP[:], X[:], ALU.add, r=[TMP, X], w=[x1], eng="pool")
        kb.act(TMP[:], X1, AF.Square, r=[x1], w=[TMP, ST], accum_out=ST[:, 0:1])
        rstd_of(kb, ST[:, 0:1], 1, 1024, ST[:, 1:2], ST[:, 2:3], r=[ST], w=[ST])
        kb.stt(H2[:], X1, ST[:, 2:3], G2[mi][:], ALU.mult, ALU.mult, r=[x1, ST, G2[mi]], w=[H2])
        kb.tt(H2[:], H2[:], S2[mi][:], ALU.add, r=[H2, S2[mi]], w=[H2], eng="pool")
        for kc in range(8):
            kb.tr(PS[1][:, kc // 4, (kc % 4) * 128:(kc % 4 + 1) * 128], H2[:, kc * 128:(kc + 1) * 128], ident[:], r=[H2, ident], w=[PS[1]])
        hi = h2T[:, :, t0:t0 + 128]
        for a in range(2):
            kb.copy("act", h2T[:, 4 * a:4 * a + 4, t0:t0 + 128], PS[1][:, a, :].rearrange("p (b t) -> p b t", t=128), r=[PS[1]], w=[h2T])
        for a in range(2):
            kb.tt(H2L[:, 4 * a:4 * a + 4, :], PS[1][:, a, :].rearrange("p (b t) -> p b t", t=128), h2T[:, 4 * a:4 * a + 4, t0:t0 + 128],
                  ALU.subtract, r=[PS[1], h2T], w=[H2L])
        n = 0
        for kc in range(8):
            for (l_, r_, lr, rr_) in ((hi[:, kc, :], wrh[:, kc, :], h2T, wrh), (H2L[:, kc, :], wrh[:, kc, :], H2L, wrh), (hi[:, kc, :], wrl[:, kc, :], h2T, wrl)):
                kb.mm(plg[:, 0:36], l_, r_, n == 0, n == 23, r=[lr, rr_], w=[plg])
                n += 1
        kb.tt(LG[:], plg[:, 0:36], rb[:], ALU.add, r=[plg, rb], w=[LG])
        kb.op("dve", lambda e: e.tensor_reduce(out=ST[:, 4:5], in_=LG[:, 0:4], axis=AX.X, op=ALU.max), r=[LG], w=[ST])
        kb.tt(RW[:, 0, 0:4], LG[:, 0:4], ST[:, 4:5].broadcast_to([128, 4]), ALU.is_equal, r=[LG, ST], w=[RW])
        kb.ts(ST[:, 5:6], ST[:, 4:5], -1.0, None, ALU.mult, r=[ST], w=[ST])
        kb.act(RW[:, 1, 0:4], LG[:, 0:4], AF.Exp, r=[LG, ST], w=[RW, ST], bias=ST[:, 5:6], accum_out=ST[:, 6:7])
        kb.op("dve", lambda e: e.reciprocal(out=ST[:, 7:8], in_=ST[:, 6:7]), r=[ST], w=[ST])
        kb.ts(RW[:, 2, 0:4], RW[:, 0, 0:4], 1e9, -1e9, ALU.mult, ALU.add, r=[RW], w=[RW])
        EM = RW[:, 3, :]
        kb.tt(RW[:, 3, :].rearrange("p (g e) -> p g e", e=8), LG[:, 4:36].rearrange("p (g e) -> p g e", e=8),
              RW[:, 2, 0:4].unsqueeze(2).broadcast_to([128, 4, 8]), ALU.add, r=[LG, RW], w=[RW])
        kb.op("dve", lambda e: e.max(out=M8[:], in_=EM), r=[RW], w=[M8])
        kb.tt(ST[:, 8:9], M8[:, 1:2], M8[:, 0:1], ALU.subtract, r=[M8], w=[ST])
        kb.act(ST[:, 8:9], ST[:, 8:9], AF.Exp, r=[ST], w=[ST])
        kb.ts(ST[:, 8:9], ST[:, 8:9], 1.0, None, ALU.add, r=[ST], w=[ST])
        kb.op("dve", lambda e: e.reciprocal(out=ST[:, 9:10], in_=ST[:, 8:9]), r=[ST], w=[ST])
        kb.ts(ST[:, 10:11], ST[:, 9:10], -1.0, 1.0, ALU.mult, ALU.add, r=[ST], w=[ST])
        kb.tt(ST[:, 11:12], ST[:, 9:10], ST[:, 7:8], ALU.mult, r=[ST], w=[ST])
        kb.tt(ST[:, 12:13], ST[:, 10:11], ST[:, 7:8], ALU.mult, r=[ST], w=[ST])
        kb.tt(RW[:, 4, :], EM, M8[:, 0:1].broadcast_to([128, 32]), ALU.is_equal, r=[RW, M8], w=[RW])
        kb.ts(RW[:, 4, :], RW[:, 4, :], ST[:, 11:12], None, ALU.mult, r=[RW, ST], w=[RW])
        kb.tt(RW[:, 5, :], EM, M8[:, 1:2].broadcast_to([128, 32]), ALU.is_equal, r=[RW, M8], w=[RW])
        kb.ts(RW[:, 5, :], RW[:, 5, :], ST[:, 12:13], None, ALU.mult, r=[RW, ST], w=[RW])
        kb.tt(RW[:, 4, :], RW[:, 4, :], RW[:, 5, :], ALU.add, r=[RW], w=[RW])
        kb.tr(pgt[0:32, 0:128], RW[:, 4, :], ident[:], r=[RW, ident], w=[pgt])
        kb.copy("dve", gTh[:, t0:t0 + 128], pgt[0:32, 0:128], r=[pgt], w=[gTh])
        kb.tt(gTl[:, t0:t0 + 128], pgt[0:32, 0:128], gTh[:, t0:t0 + 128], ALU.subtract, r=[pgt, gTh], w=[gTl])
    kb.pop()
    kb.push()
    NSLOT = 4
    EG = 2
    wg = [kb.sb("wg%d" % i, [128, 8, 256], BF16) for i in range(NSLOT)]
    wu = [kb.sb("wu%d" % i, [128, 8, 256], BF16) for i in range(NSLOT)]
    wd = [kb.sb("wd%d" % i, [128, 2, 1024], BF16) for i in range(NSLOT)]
    pgu = [kb.ps("pgu%d" % i, [128, 2, 256]) for i in range(2)]
    pbc = kb.ps("pbcC", [128, 512])
    pacc = [kb.ps("pacc%d" % i, [128, 512]) for i in range(4)]
    sg = [kb.sb("sg%d" % i, [128, 256], BF16) for i in range(2)]
    tu = [kb.sb("tu%d" % i, [128, 256]) for i in range(2)]
    aT = [kb.sb("aT%d" % i, [128, 256], BF16) for i in range(4)]
    fl = [kb.sb("fl%d" % i, [128, 512]) for i in range(2)]
    TG = 256
    it = {"f": 0, "a": 0, "fl": 0}
    pend = []

    def load_expert(e):
        s_ = e % NSLOT
        kb.dma("pool", wg[s_][:], io["w_gate"][e].rearrange("(k p) f -> p k f", p=128), r=[], w=wg[s_])
        kb.dma("pool", wu[s_][:], io["w_up"][e].rearrange("(k p) f -> p k f", p=128), r=[], w=wu[s_])
        kb.dma("pool", wd[s_][:], io["w_down"][e].rearrange("(c p) d -> p c d", p=128), r=[], w=wd[s_])

    for e in range(min(NSLOT, NE)):
        load_expert(e)
    for g0 in range(0, NE, EG):
        for tg in range(NTOK // TG):
            c0 = tg * TG
            mi = 1 if tg >= 8 else 0
            first = True
            for e in range(g0, g0 + EG):
                s_ = e % NSLOT
                kb.mm(pbc[:, 0:TG], selb[:, e * 128:(e + 1) * 128], gTh[:, c0:c0 + TG], True, False, r=[selb, gTh], w=[pbc])
                kb.mm(pbc[:, 0:TG], selb[:, e * 128:(e + 1) * 128], gTl[:, c0:c0 + TG], False, True, r=[selb, gTl], w=[pbc])
                for fc in range(2):
                    p = pgu[it["f"] % 2]
                    sgt = sg[it["f"] % 2]
                    tut = tu[it["f"] % 2]
                    it["f"] += 1
                    at = aT[it["a"] % 4]
                    it["a"] += 1
                    for kc in range(8):
                        kb.mm(p[:, 0, :], wg[s_][:, kc, fc * 128:(fc + 1) * 128], h2T[:, kc, c0:c0 + TG], kc == 0, kc == 7, r=[wg[s_], h2T], w=[p])
                    for kc in range(8):
                        kb.mm(p[:, 1, :], wu[s_][:, kc, fc * 128:(fc + 1) * 128], h2T[:, kc, c0:c0 + TG], kc == 0, kc == 7, r=[wu[s_], h2T], w=[p])
                    kb.act(sgt[:], p[:, 0, :], AF.Silu, r=[p], w=[sgt])
                    kb.tt(tut[:], p[:, 1, :], sgt[:], ALU.mult, r=[p, sgt], w=[tut])
                    kb.tt(at[:], pbc[:, 0:TG], tut[:], ALU.mult, r=[pbc, tut], w=[at])
                    while pend:
                        pend.pop(0)()

                    def down(at=at, s_=s_, fc=fc, st_=(first and fc == 0), sp_=((e == g0 + EG - 1) and fc == 1)):
                        for sub in range(2):
                            for hf in range(2):
                                kb.mm(pacc[sub * 2 + hf][:, :], at[:, sub * 128:(sub + 1) * 128], wd[s_][:, fc, hf * 512:(hf + 1) * 512],
                                      st_, sp_, r=[at, wd[s_]], w=[pacc[sub * 2 + hf]])
                    pend.append(down)
                first = False
            while pend:
                pend.pop(0)()
            for sub in range(2):
                ti = tg * 2 + sub
                for hf in range(2):
                    f = fl[it["fl"] % 2]
                    it["fl"] += 1
                    kb.tt(f[:], pacc[sub * 2 + hf][:, :], gate2[mi][:, hf * 512:(hf + 1) * 512], ALU.mult, r=[pacc[sub * 2 + hf], gate2[mi]], w=[f])
                    kb.tt(x1[:, ti, hf * 512:(hf + 1) * 512], x1[:, ti, hf * 512:(hf + 1) * 512], f[:], ALU.add, r=[x1, f], w=[x1], eng="pool")
        for e in range(g0 + NSLOT, min(g0 + NSLOT + EG, NE)):
            if not os.environ.get("KNOLOAD"):
                load_expert(e)
    for ti in range(NT):
        kb.dma("sp", io["xo"][ti * 128:(ti + 1) * 128, :], x1[:, ti, :], r=[x1], w=io["xo_res"])
    kb.pop()


C_INPUTS = [("x", [NTOK, D], F32), ("mixT", [D, NTOK], BF16), ("cc", [128, 8, 2], F32), ("w_mod", [D, 6 * D], F32), ("b_mod", [6 * D], F32),
            ("w_out", [D, D], F32), ("norm2_w", [D], F32), ("w_grp", [D, 4], F32), ("b_grp", [4], F32),
            ("w_erouter", [D, 32], F32), ("b_erouter", [32], F32), ("w_gate", [32, D, 256], F32), ("w_up", [32, D, 256], F32),
            ("w_down", [32, 256, D], F32)]


def build_C():
    nc = bass.Bass("TRN2", target_bir_lowering=False)
    io = IO()
    for nm, shp, dt in C_INPUTS:
        declare(nc, io, nm, shp, dt, "ExternalInput")
    cd = {}
    for nm in ("ident", "sel"):
        th = nc.dram_tensor("c_" + nm, CONST_SHAPES[nm], F32, kind="ExternalInput")
        cd[nm] = T(th, "c_" + nm)
    declare(nc, io, "xo", [NTOK, D], F32, "ExternalOutput")
    with ExitStack() as st:
        kb = KB(nc, st)
        cst = load_consts(kb, cd, ["ident"])
        cst["sel_d"] = cd["sel"][:]
        phase_C(kb, io, cst)
        while len(kb.stacks) > 1:
            kb.stacks.pop().close()
        kb.finish([io["xo_res"]])
        print("phase C: n_inst", kb.n_inst, "n_wait", kb.n_wait, "dsems", kb.ndsem)
    return nc


def c_inmaps(inp, l, xl, xc, bout):
    cst, _ = consts()
    maps = []
    for b in range(NB):
        full = np.zeros((D, B_T), dtype=bout[0].dtype)
        for j in range(4):
            m = bout[4 * b + j]
            full[128 * j:128 * j + 128] = m[0:128]
            full[512 + 64 * j:512 + 64 * j + 64] = m[128:192]
            full[768 + 64 * j:768 + 64 * j + 64] = m[192:256]
        for j in range(4):
            core = 4 * b + j
            cols = np.concatenate([np.arange(j * TL, (j + 1) * TL), np.arange(SEQ, B_T)])
            mm_ = {"x": f32(core_tokens(xl, xc, core)), "mixT": np.ascontiguousarray(full[:, cols]),
                   "cc": cc_layout(inp["c"][b], inp["c_ctx"]), "c_ident": cst["ident"], "c_sel": cst["sel"]}
            for nm in ("w_mod", "b_mod", "w_out", "norm2_w", "w_grp", "b_grp", "w_erouter", "b_erouter", "w_gate", "w_up", "w_down"):
                mm_[nm] = f32(inp[nm][l])
            maps.append(mm_)
    return maps


def _run(nc, maps):
    return run_bass_kernel_spmd(nc, maps, core_ids=list(range(NCORE))).results


def kernel(**inputs):
    inp = {k: np.asarray(v) for k, v in inputs.items()}
    xl = f32(inp["x"])
    xc = f32(inp["ctx"])
    for l in range(DEPTH):
        ra = _run(build_A(), a_inmaps(inp, l, xl, xc))
        aout = [{k: np.asarray(r[k]) for k in ("qT", "kT", "v", "fm", "tm")} for r in ra]
        del ra
        rb = _run(build_B(True), b_inmaps(inp, l, aout))
        bout = [np.asarray(r["mixT"]) for r in rb]
        del rb, aout
        rc = _run(build_C(), c_inmaps(inp, l, xl, xc, bout))
        xl_n = np.empty_like(xl)
        xc_n = np.empty_like(xc)
        for core in range(NCORE):
            b, j = core // 4, core % 4
            xo = np.asarray(rc[core]["xo"], dtype=np.float32)
            xl_n[b, j * TL:(j + 1) * TL] = xo[:TL]
            if j == 0:
                xc_n[b] = xo[TL:]
        xl, xc = xl_n, xc_n
        del rc, bout
    return xl
```

```python
import os
import numpy as np
import ml_dtypes
from contextlib import ExitStack
import concourse.bass as bass
import concourse.mybir as mybir
from concourse.bass_utils import run_bass_kernel_spmd

F32 = mybir.dt.float32
BF16 = mybir.dt.bfloat16
AF = mybir.ActivationFunctionType
ALU = mybir.AluOpType
AX = mybir.AxisListType

D = 1024
NB = 2
SEQ = 8192
DEPTH = 4
NCTX = 256
EPS = 1e-6
NCORE = 8
TL = 2048
NT = 18
NTOK = NT * 128
D_IN = 2000
O_CQ, O_CKV, O_KR, O_MX, O_MV, O_MO, O_MG, O_GQ, O_GK, O_GV, O_GR, O_GA = (
    0, 256, 384, 416, 672, 928, 1184, 1200, 1328, 1456, 1712, 1968)

SAME_ENG_SYNC = bool(int(os.environ.get("KSES", "1")))

DBG = float(os.environ.get('KDBG', '99'))


class Res:
    __slots__ = ("name", "w", "r", "dsems", "excl")

    def __init__(self, name):
        self.name = name
        self.excl = False
        self.w = None
        self.r = {}
        self.dsems = {}


class T:
    def __init__(self, th, name):
        self.t = th
        self.res = Res(name)
        self.name = name

    def __getitem__(self, idx):
        return self.t[idx]


def _res(x):
    return x.res if isinstance(x, T) else x


class KB:
    def __init__(self, nc, stack):
        self.nc = nc
        self.st = stack
        self.eng = {"pe": nc.tensor, "dve": nc.vector, "act": nc.scalar,
                    "pool": nc.gpsimd, "sp": nc.sync}
        self.semh = {}
        self.cnt = {}
        for k in self.eng:
            self.semh[k] = stack.enter_context(nc.semaphore("s_" + k))
            self.cnt[k] = 0
        self.waited = {k: {} for k in self.eng}
        self.ndsem = 0
        self.n_inst = 0
        self.n_wait = 0
        self.uid = 0
        self.stacks = [stack]
        self.dres = []

    def sb(self, name, shape, dt=F32):
        self.uid += 1
        nm = "%s_%d" % (name, self.uid)
        return T(self.stacks[-1].enter_context(self.nc.sbuf_tensor(nm, list(shape), dt)), nm)

    def ps(self, name, shape, dt=F32):
        self.uid += 1
        nm = "%s_%d" % (name, self.uid)
        t = T(self.stacks[-1].enter_context(self.nc.psum_tensor(nm, list(shape), dt)), nm)
        t.res.excl = True
        return t

    def push(self):
        self.stacks.append(ExitStack())

    def pop(self):
        self.barrier()
        self.stacks.pop().close()

    def barrier(self):
        for e in self.eng:
            deps = {k: self.cnt[k] for k in self.eng if k != e and self.cnt[k] > 0}
            for res in self.dres:
                for dk, c in res.dsems.values():
                    deps[dk] = c
            self._wait(e, deps)

    def _wait(self, e, deps):
        for key, val in deps.items():
            if self.waited[e].get(key, 0) >= val:
                continue
            if key == e and (e == "pe" or not SAME_ENG_SYNC):
                continue
            self.eng[e].wait_ge(self.semh[key], val)
            self.waited[e][key] = val
            self.n_wait += 1

    def _collect(self, reads, writes, dma_write=None):
        deps = {}

        def add(ev):
            if ev is None:
                return
            k, v = ev
            if deps.get(k, 0) < v:
                deps[k] = v

        def cur(res):
            if res.w is None:
                return []
            if res.w[0] == "dma":
                return [(dk, c) for dk, c in res.dsems.values()]
            return [res.w]

        for r in reads:
            for ev in cur(r):
                add(ev)
        for w in writes:
            if dma_write is not None and w is dma_write and w.w is not None \
                    and w.w[0] == "dma" and not w.r:
                continue
            for ev in cur(w):
                add(ev)
            for k, v in w.r.items():
                add((k, v))
        return deps

    def op(self, e, fn, r=(), w=()):
        reads = [_res(x) for x in r]
        writes = [_res(x) for x in w]
        ex = [x for x in reads if x.excl and x not in writes]
        if ex:
            reads = [x for x in reads if not x.excl]
            writes = writes + ex
        self._wait(e, self._collect(reads, writes))
        ins = fn(self.eng[e])
        self.cnt[e] += 1
        ins.then_inc(self.semh[e], 1)
        self.n_inst += 1
        ev = (e, self.cnt[e])
        for rr in reads:
            if rr.r.get(e, 0) < ev[1]:
                rr.r[e] = ev[1]
        for ww in writes:
            ww.w = ev
            ww.r = {}
        return ins

    def dma(self, q, out, in_, r=(), w=None, **kw):
        reads = [_res(x) for x in r]
        wres = _res(w)
        skey = reads[0].name if (isinstance(w, Res) and reads) else None
        if skey not in wres.dsems:
            dkey = "d%d" % self.ndsem
            self.ndsem += 1
            self.semh[dkey] = self.st.enter_context(self.nc.semaphore(dkey))
            wres.dsems[skey] = [dkey, 0]
            if wres not in self.dres:
                self.dres.append(wres)
        ent = wres.dsems[skey]
        self._wait(q, self._collect(reads, [wres], dma_write=wres))
        ins = self.eng[q].dma_start(out=out, in_=in_, **kw)
        ent[1] += 16
        ins.then_inc(self.semh[ent[0]], 16)
        self.n_inst += 1
        ev = (ent[0], ent[1])
        for rr in reads:
            if rr.r.get(ev[0], 0) < ev[1]:
                rr.r[ev[0]] = ev[1]
        wres.w = ("dma",)
        wres.r = {}
        return ins

    def finish(self, outs, e="sp"):
        self._wait(e, self._collect([_res(x) for x in outs], []))

    def mm(self, out, lhsT, rhs, start, stop, r, w):
        return self.op("pe", lambda e: e.matmul(out, lhsT=lhsT, rhs=rhs, start=start, stop=stop), r=r, w=w)

    def tr(self, out, in_, ident, r, w):
        return self.op("pe", lambda e: e.transpose(out=out, in_=in_, identity=ident), r=r, w=w)

    def copy(self, eng, out, in_, r, w):
        if eng == "act":
            return self.op("act", lambda e: e.copy(out=out, in_=in_), r=r, w=w)
        return self.op(eng, lambda e: e.tensor_copy(out=out, in_=in_), r=r, w=w)

    def act(self, out, in_, func, r, w, **kw):
        return self.op("act", lambda e: e.activation(out=out, in_=in_, func=func, **kw), r=r, w=w)

    def tt(self, out, in0, in1, op, r, w, eng="dve"):
        return self.op(eng, lambda e: e.tensor_tensor(out=out, in0=in0, in1=in1, op=op), r=r, w=w)

    def ts(self, out, in0, s1, s2, op0, op1=None, r=(), w=(), eng="dve"):
        if op1 is None:
            return self.op(eng, lambda e: e.tensor_scalar(out=out, in0=in0, scalar1=s1, scalar2=None, op0=op0), r=r, w=w)
        return self.op(eng, lambda e: e.tensor_scalar(out=out, in0=in0, scalar1=s1, scalar2=s2, op0=op0, op1=op1), r=r, w=w)

    def stt(self, out, in0, scalar, in1, op0, op1, r, w, accum_out=None):
        return self.op("dve", lambda e: e.scalar_tensor_tensor(out=out, in0=in0, scalar=scalar, in1=in1, op0=op0, op1=op1, accum_out=accum_out), r=r, w=w)


def rstd_of(kb, ss, n, nfeat, tmp, out, r, w):
    if os.environ.get("KRSTD", "1") == "1":
        kb.act(tmp, ss, AF.Ln, r=r, w=w, scale=1.0 / nfeat, bias=EPS)
        kb.act(out, tmp, AF.Exp, r=w, w=w, scale=-0.5)
        return
    kb.ts(tmp, ss, 1.0 / nfeat, EPS, ALU.mult, ALU.add, r=r, w=w)
    kb.act(tmp, tmp, AF.Sqrt, r=w, w=w)
    kb.op("dve", lambda e: e.reciprocal(out=out, in_=tmp), r=w, w=w)


def rope_tables():
    rows = SEQ // 64
    row = np.broadcast_to(np.arange(rows, dtype=np.float32)[:, None], (rows, 64)).reshape(-1)
    col = np.broadcast_to(np.arange(64, dtype=np.float32)[None, :], (rows, 64)).reshape(-1)
    inv = (np.float32(10000.0) ** (-np.arange(8, dtype=np.float32) / np.float32(8))).astype(np.float32)
    ang = np.concatenate([row[:, None] * inv, col[:, None] * inv], axis=-1).astype(np.float32)
    return np.concatenate([np.cos(ang), np.sin(ang)], axis=-1).astype(np.float32)


def host_consts():
    c = {}
    c["ident"] = np.eye(128, dtype=np.float32)
    j = np.arange(128)
    c["triu"] = (j[:, None] <= j[None, :]).astype(np.float32)
    c["tril"] = (j[:, None] >= j[None, :]).astype(np.float32)
    sel = np.zeros((32, 32, 128), np.float32)
    for e in range(32):
        sel[e, e, :] = 1.0
    c["sel"] = sel.reshape(32, 32 * 128)
    return c


def load_consts(kb, cd, names):
    out = {}
    for nm in names:
        shp = {"ident": [128, 128], "triu": [128, 128], "tril": [128, 128], "sel": [32, 32 * 128]}[nm]
        t = kb.sb("c_" + nm, shp)
        kb.dma("sp", t[:], cd[nm][:], r=[cd[nm]], w=t)
        out[nm] = t
        if nm in ("triu", "tril"):
            tb = kb.sb("c_" + nm + "_b", shp, BF16)
            kb.copy("dve", tb[:], t[:], r=[t], w=[tb])
            out[nm + "_b"] = tb
    return out


def bcast_load(kb, name, ap1d, n, q="sp"):
    t = kb.sb(name, [128, n])
    kb.dma(q, t[:], ap1d.partition_broadcast(128), r=[], w=t)
    return t


def compute_mod(kb, cc_d, w_mod_d, b_mod_d, chunks, pool_ps, pre=None):
    res = {}
    for j in chunks:
        if pre is not None and j in pre:
            res[j] = pre[j]
        else:
            res[j] = (kb.sb("mod_l%d" % j, [128, 1024]), kb.sb("mod_c%d" % j, [128, 1024]))
    kb.push()
    cc = kb.sb("cc", [128, 8, 2])
    kb.dma("sp", cc[:], cc_d, r=[], w=cc)
    sc = kb.sb("sc", [128, 8, 2])
    kb.act(sc[:], cc[:], AF.Silu, r=[cc], w=[sc])
    SC = [kb.sb("SC%d" % i, [128, 8, 128], BF16) for i in range(2)]
    for i in range(2):
        kb.copy("dve", SC[i][:], sc[:, :, i:i + 1].broadcast_to([128, 8, 128]), r=[sc], w=[SC[i]])
    wm = [kb.sb("wm%d" % i, [128, 8, 512], BF16) for i in range(2)]
    bm = [kb.sb("bm%d" % i, [128, 512]) for i in range(2)]
    si = 0
    for j in chunks:
        tl, tc_ = res[j]
        for hf in range(2):
            c0 = j * 1024 + hf * 512
            wmt = wm[si % 2]
            bmt = bm[si % 2]
            si += 1
            kb.dma("pool", wmt[:], w_mod_d[:, c0:c0 + 512].rearrange("(k p) n -> p k n", p=128), r=[], w=wmt)
            kb.dma("sp", bmt[:], b_mod_d[c0:c0 + 512].partition_broadcast(128), r=[], w=bmt)
            for i, dst in enumerate((tl, tc_)):
                ps = pool_ps[i]
                for kc in range(8):
                    kb.mm(ps[:, 0, :], SC[i][:, kc, :], wmt[:, kc, :], kc == 0, kc == 7, r=[SC[i], wmt], w=[ps])
                kb.tt(dst[:, hf * 512:(hf + 1) * 512], ps[:, 0, :], bmt[:], ALU.add, r=[ps, bmt], w=[dst])
    kb.pop()
    return res


def phase_A(kb, io, cst):
    ident = cst["ident"]
    PS = [kb.ps("psA%d" % i, [128, 2, 512]) for i in range(4)]
    mods = compute_mod(kb, io["cc"], io["w_mod"], io["b_mod"], [0, 1], PS)
    n1 = bcast_load(kb, "n1", io["norm1_w"], 1024)
    G1 = []
    S1 = []
    for i in range(2):
        g = kb.sb("G1_%d" % i, [128, 1024])
        kb.stt(g[:], mods[1][i][:], 1.0, n1[:], ALU.add, ALU.mult, r=[mods[1][i], n1], w=[g])
        G1.append(g)
        S1.append(mods[0][i])
    w_in = kb.sb("w_in", [128, 8, D_IN], BF16)
    for kc in range(8):
        kb.dma("pool", w_in[:, kc, :], io["w_in"][kc * 128:(kc + 1) * 128, :], r=[], w=w_in)
    w_uq = kb.sb("w_uq", [128, 2, 768], BF16)
    kb.dma("pool", w_uq[:], io["w_uq"].rearrange("(k p) n -> p k n", p=128), r=[], w=w_uq)
    w_ukv = kb.sb("w_ukv", [128, 1024], BF16)
    kb.dma("pool", w_ukv[:], io["w_ukv"], r=[], w=w_ukv)
    qan = bcast_load(kb, "qan", io["q_a_norm"], 256)
    kvan = bcast_load(kb, "kvan", io["kv_a_norm"], 128)
    qnw1 = bcast_load(kb, "qnw1", io["q_norm_w"], 96)
    knw1 = bcast_load(kb, "knw1", io["k_norm_w"], 96)
    qnw = kb.sb("qnw", [128, 8, 96])
    knw = kb.sb("knw", [128, 8, 96])
    kb.ts(qnw[:], qnw1[:].unsqueeze(1).broadcast_to([128, 8, 96]), float(96 ** -0.5), None, ALU.mult, r=[qnw1], w=[qnw])
    kb.copy("dve", knw[:], knw1[:].unsqueeze(1).broadcast_to([128, 8, 96]), r=[knw1], w=[knw])
    rope = kb.sb("rope", [128, 16, 32])
    kb.dma("sp", rope[:], io["rope"].rearrange("(n p) c -> p n c", p=128), r=[], w=rope)

    if DBG < 1:
        return
    TM_SLABS = [[(O_CQ, 416), (O_MG, 16)], [(O_MV, 512)], [(O_GV, 512)]]
    FM_GROUPS = [(O_MX, 128), (O_MX + 128, 128), (O_GQ, 128), (O_GK, 128), (O_GA, 32)]
    NB_ = 2
    xt = [kb.sb("xt%d" % i, [128, 1024]) for i in range(NB_)]
    junk = [kb.sb("junk%d" % i, [128, 1024]) for i in range(NB_)]
    xm = [kb.sb("xm%d" % i, [128, 1024]) for i in range(NB_)]
    xmT = [kb.sb("xmT%d" % i, [128, 8, 128], BF16) for i in range(NB_)]
    htm = [kb.sb("htm%d" % i, [128, 1456]) for i in range(NB_)]
    hfm = [kb.sb("hfm%d" % i, [128, 5, 128]) for i in range(NB_)]
    st1 = [kb.sb("st1_%d" % i, [128, 32]) for i in range(NB_)]
    cqn = [kb.sb("cqn%d" % i, [128, 384]) for i in range(NB_)]
    cT = [kb.sb("cT%d" % i, [128, 3, 128], BF16) for i in range(NB_)]
    qf = [kb.sb("qf%d" % i, [128, 8, 96]) for i in range(NB_)]
    kf = [kb.sb("kf%d" % i, [128, 8, 96]) for i in range(NB_)]
    sq = [kb.sb("sq%d" % i, [128, 8, 96]) for i in range(NB_)]
    rt = [kb.sb("rt%d" % i, [128, 4, 8, 16]) for i in range(NB_)]
    qTs = [kb.sb("qTs%d" % i, [96, 8, 128], BF16) for i in range(NB_)]
    kTs = [kb.sb("kTs%d" % i, [96, 8, 128], BF16) for i in range(NB_)]
    vs = [kb.sb("vs%d" % i, [128, 8, 64], BF16) for i in range(NB_)]

    def head_norm_rope(src, dst_f, sqt, stt_, wbc, ropeidx, rtt, r_extra):
        kb.act(sqt[:], src, AF.Square, r=r_extra, w=[sqt])
        kb.op("dve", lambda e: e.tensor_reduce(out=stt_[:, 0:8], in_=sqt[:], axis=AX.X, op=ALU.add), r=[sqt], w=[stt_])
        rstd_of(kb, stt_[:, 0:8], 8, 96, stt_[:, 8:16], stt_[:, 16:24], r=[stt_], w=[stt_])
        kb.tt(dst_f[:], src, stt_[:, 16:24].unsqueeze(2).broadcast_to([128, 8, 96]), ALU.mult, r=r_extra + [stt_], w=[dst_f])
        kb.tt(dst_f[:], dst_f[:], wbc[:], ALU.mult, r=[dst_f, wbc], w=[dst_f])
        if ropeidx is not None:
            cos = rope[:, ropeidx, 0:16].unsqueeze(1).broadcast_to([128, 8, 16])
            sin = rope[:, ropeidx, 16:32].unsqueeze(1).broadcast_to([128, 8, 16])
            x1 = dst_f[:, :, 64:80]
            x2 = dst_f[:, :, 80:96]
            kb.tt(rtt[:, 0], x1, cos, ALU.mult, r=[dst_f, rope], w=[rtt])
            kb.tt(rtt[:, 1], x2, sin, ALU.mult, r=[dst_f, rope], w=[rtt])
            kb.tt(rtt[:, 2], x1, sin, ALU.mult, r=[dst_f, rope], w=[rtt])
            kb.tt(rtt[:, 3], x2, cos, ALU.mult, r=[dst_f, rope], w=[rtt])
            kb.tt(x1, rtt[:, 0], rtt[:, 1], ALU.subtract, r=[rtt], w=[dst_f])
            kb.tt(x2, rtt[:, 2], rtt[:, 3], ALU.add, r=[rtt], w=[dst_f])

    for ti in range(NT):
        b_ = ti % NB_
        is_ctx = ti >= 16
        mi = 1 if is_ctx else 0
        t0 = ti * 128
        X, XM, XMT, H, HF, ST = xt[b_], xm[b_], xmT[b_], htm[b_], hfm[b_], st1[b_]
        kb.dma("sp", X[:], io["x"][t0:t0 + 128, :], r=[io["x_res"]], w=X)
        kb.act(junk[b_][:], X[:], AF.Square, r=[X], w=[junk[b_], ST], accum_out=ST[:, 0:1])
        rstd_of(kb, ST[:, 0:1], 1, 1024, ST[:, 1:2], ST[:, 2:3], r=[ST], w=[ST])
        kb.stt(XM[:], X[:], ST[:, 2:3], G1[mi][:], ALU.mult, ALU.mult, r=[X, ST, G1[mi]], w=[XM])
        kb.tt(XM[:], XM[:], S1[mi][:], ALU.add, r=[XM, S1[mi]], w=[XM], eng="pool")
        if DBG < 2:
            continue
        for kc in range(8):
            kb.tr(PS[0][:, kc // 4, (kc % 4) * 128:(kc % 4 + 1) * 128], XM[:, kc * 128:(kc + 1) * 128], ident[:], r=[XM, ident], w=[PS[0]])
        kb.copy("act", XMT[:].rearrange("p (a b) t -> p a (b t)", a=2), PS[0][:], r=[PS[0]], w=[XMT])
        pcol = 0
        for si, slab in enumerate(TM_SLABS):
            pst = PS[1] if si < 2 else PS[2]
            bank = si if si < 2 else 0
            off = 0
            for (c0, wd) in slab:
                for kc in range(8):
                    kb.mm(pst[:, bank, off:off + wd], XMT[:, kc, :], w_in[:, kc, c0:c0 + wd], kc == 0, kc == 7, r=[XMT, w_in], w=[pst])
                off += wd
            kb.copy("act" if si != 1 else "dve", H[:, pcol:pcol + off], pst[:, bank, 0:off], r=[pst], w=[H])
            pcol += off
        for gi, (c0, wd) in enumerate(FM_GROUPS):
            for kc in range(8):
                kb.mm(PS[3][0:wd, gi // 4, (gi % 4) * 128:(gi % 4 + 1) * 128], w_in[:, kc, c0:c0 + wd], XMT[:, kc, :], kc == 0, kc == 7, r=[XMT, w_in], w=[PS[3]])
        kb.copy("dve", HF[:, 0:4, :].rearrange("p a t -> p (a t)"), PS[3][:, 0, :], r=[PS[3]], w=[HF])
        kb.copy("dve", HF[0:32, 4, :], PS[3][0:32, 1, 0:128], r=[PS[3]], w=[HF])
        if DBG < 3:
            continue
        kb.dma("sp", io["fm"][0:512, t0:t0 + 128].rearrange("(a p) t -> p a t", p=128), HF[:, 0:4, :], r=[HF], w=io["fm_res"])
        kb.dma("sp", io["fm"][512:544, t0:t0 + 128], HF[0:32, 4, :], r=[HF], w=io["fm_res"])
        kb.dma("sp", io["tm"][t0:t0 + 128, :], H[:, 416:1456], r=[H], w=io["tm_res"])
        if DBG < 4:
            continue
        CQ = cqn[b_]
        kb.act(junk[b_][:, 0:256], H[:, 0:256], AF.Square, r=[H], w=[junk[b_], ST], accum_out=ST[:, 4:5])
        kb.act(junk[b_][:, 256:384], H[:, 256:384], AF.Square, r=[H], w=[junk[b_], ST], accum_out=ST[:, 5:6])
        rstd_of(kb, ST[:, 4:5], 1, 256, ST[:, 6:7], ST[:, 8:9], r=[ST], w=[ST])
        rstd_of(kb, ST[:, 5:6], 1, 128, ST[:, 7:8], ST[:, 9:10], r=[ST], w=[ST])
        kb.stt(CQ[:, 0:256], H[:, 0:256], ST[:, 8:9], qan[:], ALU.mult, ALU.mult, r=[H, ST, qan], w=[CQ])
        kb.stt(CQ[:, 256:384], H[:, 256:384], ST[:, 9:10], kvan[:], ALU.mult, ALU.mult, r=[H, ST, kvan], w=[CQ])
        for kc in range(3):
            kb.tr(PS[2][:, 1, kc * 128:(kc + 1) * 128], CQ[:, kc * 128:(kc + 1) * 128], ident[:], r=[CQ, ident], w=[PS[2]])
        kb.copy("act", cT[b_][:].rearrange("p a t -> p (a t)"), PS[2][:, 1, 0:384], r=[PS[2]], w=[cT[b_]])
        if DBG < 4.1:
            continue
        for s in range(2):
            for kc in range(2):
                kb.mm(PS[0][:, s, 0:384], cT[b_][:, kc, :], w_uq[:, kc, s * 384:(s + 1) * 384], kc == 0, kc == 1, r=[cT[b_], w_uq], w=[PS[0]])
        QF, KF = qf[b_], kf[b_]
        kb.copy("act", QF[:].rearrange("p (s a) d -> p s (a d)", s=2), PS[0][:, :, 0:384], r=[PS[0]], w=[QF])
        if DBG < 4.2:
            continue
        for s in range(2):
            kb.mm(PS[1][:, s, :], cT[b_][:, 2, :], w_ukv[:, s * 512:(s + 1) * 512], True, True, r=[cT[b_], w_ukv], w=[PS[1]])
        kvv = PS[1][:].rearrange("p s (a d) -> p (s a) d", d=128)
        if DBG < 4.21:
            continue
        kb.copy("dve", KF[:, :, 0:64], kvv[:, :, 0:64], r=[PS[1]], w=[KF])
        if DBG < 4.22:
            continue
        for s in range(2):
            kb.copy("dve", vs[b_][:, 4 * s:4 * s + 4, :], PS[1][:, s, :].rearrange("p (a d) -> p a d", d=128)[:, :, 64:128], r=[PS[1]], w=[vs[b_]])
        if DBG < 4.23:
            continue
        kb.copy("dve", KF[:, :, 64:96], H[:, 384:416].unsqueeze(1).broadcast_to([128, 8, 32]), r=[H], w=[KF])
        if DBG < 4.3:
            continue
        ridx = None if is_ctx else ti
        if DBG < 4.4:
            ridx = None
        head_norm_rope(QF[:], QF, sq[b_], ST, qnw, ridx, rt[b_], [QF])
        head_norm_rope(KF[:], KF, sq[b_], ST, knw, ridx, rt[b_], [KF])
        if DBG < 5:
            continue
        for (SRC, DST, dname) in ((QF, qTs[b_], "qT"), (KF, kTs[b_], "kT")):
            for h in range(8):
                kb.tr(PS[3][0:96, h // 4, (h % 4) * 128:(h % 4 + 1) * 128], SRC[:, h, :], ident[:], r=[SRC, ident], w=[PS[3]])
            kb.copy("act", DST[:].rearrange("p (a b) t -> p a (b t)", a=2), PS[3][0:96, :, :], r=[PS[3]], w=[DST])
            kb.dma("sp", io[dname][:, :, t0:t0 + 128].rearrange("h d t -> d h t"), DST[:], r=[DST], w=io[dname + "_res"])
        kb.dma("sp", io["v"][t0:t0 + 128, :], vs[b_][:].rearrange("p h d -> p (h d)"), r=[vs[b_]], w=io["v_res"])


class IO(dict):
    pass


def declare(nc, io, name, shape, dt, kind):
    th = nc.dram_tensor(name, list(shape), dt, kind=kind)
    io[name] = th[:] if len(shape) > 0 else th
    io[name + "_res"] = Res(name)
    return th


CONST_SHAPES = {"ident": [128, 128], "triu": [128, 128], "tril": [128, 128], "sel": [32, 32 * 128]}

A_INPUTS = [("x", [NTOK, D]), ("cc", [128, 8, 2]), ("w_mod", [D, 6 * D]), ("b_mod", [6 * D]), ("norm1_w", [D]),
            ("w_in", [D, D_IN]), ("q_a_norm", [256]), ("w_uq", [256, 768]), ("kv_a_norm", [128]),
            ("w_ukv", [128, 1024]), ("q_norm_w", [96]), ("k_norm_w", [96]), ("rope", [TL, 32])]
A_OUTPUTS = [("qT", [8, 96, NTOK], BF16), ("kT", [8, 96, NTOK], BF16), ("v", [NTOK, 512], BF16),
             ("fm", [544, NTOK], F32), ("tm", [NTOK, 1040], F32)]


def build_A():
    nc = bass.Bass("TRN2", target_bir_lowering=False)
    io = IO()
    for nm, shp in A_INPUTS:
        declare(nc, io, nm, shp, F32, "ExternalInput")
    cd = {}
    for nm in ("ident",):
        th = nc.dram_tensor("c_" + nm, CONST_SHAPES[nm], F32, kind="ExternalInput")
        cd[nm] = T(th, "c_" + nm)
    for nm, shp, dt in A_OUTPUTS:
        declare(nc, io, nm, shp, dt, "ExternalOutput")
    with ExitStack() as st:
        kb = KB(nc, st)
        cst = load_consts(kb, cd, ["ident"])
        phase_A(kb, io, cst)
        kb.finish([io[nm + "_res"] for nm, _, _ in A_OUTPUTS])
        print("phase A: n_inst", kb.n_inst, "n_wait", kb.n_wait, "dsems", kb.ndsem)
    return nc


_ROPE = None
_CONSTS = None


def consts():
    global _ROPE, _CONSTS
    if _CONSTS is None:
        _CONSTS = host_consts()
        _ROPE = rope_tables()
    return _CONSTS, _ROPE


def f32(a):
    return np.ascontiguousarray(a, dtype=np.float32)


def cc_layout(c_b, c_ctx):
    cc = np.stack([c_b, c_ctx], axis=-1)
    return f32(cc.reshape(8, 128, 2).transpose(1, 0, 2))


def core_tokens(xl, xc, core):
    b, j = core // 4, core % 4
    return np.concatenate([xl[b, j * TL:(j + 1) * TL], xc[b]], axis=0)


def a_inmaps(inp, l, xl, xc):
    cst, rope = consts()
    maps = []
    for core in range(NCORE):
        b, j = core // 4, core % 4
        m = {"x": f32(core_tokens(xl, xc, core)), "cc": cc_layout(inp["c"][b], inp["c_ctx"]),
             "rope": f32(rope[j * TL:(j + 1) * TL]), "c_ident": cst["ident"]}
        for nm in ("w_mod", "b_mod", "norm1_w", "w_in", "q_a_norm", "w_uq", "kv_a_norm", "w_ukv", "q_norm_w", "k_norm_w"):
            m[nm] = f32(inp[nm][l])
        maps.append(m)
    return maps


B_T = SEQ + NCTX
NCH = B_T // 128
SEQ_F = [64, 65] + list(range(64))
SEQ_B = list(range(65, -1, -1))


def nsl(ns):
    if len(ns) == 1:
        return slice(ns[0], ns[0] + 1)
    if ns[1] == ns[0] + 1:
        return slice(ns[0], ns[-1] + 1)
    stop = ns[-1] - 1
    return slice(ns[0], stop if stop >= 0 else None, -1)


def seq_groups(seq, g):
    groups, cur = [], []
    for n in seq:
        if cur and (len(cur) == g or abs(n - cur[-1]) != 1 or (len(cur) >= 2 and (n - cur[-1]) != (cur[-1] - cur[-2]))):
            groups.append(cur)
            cur = []
        cur.append(n)
    groups.append(cur)
    return groups


def attention_B(kb, io, need_ctx):
    kb.push()
    KT = kb.sb("KT", [96, 2, B_T], BF16)
    QT = kb.sb("QT", [96, 2, SEQ], BF16)
    QC = kb.sb("QC", [96, 2, 256], BF16)
    Vst = kb.sb("Vst", [128, NCH, 128], BF16)
    V = kb.sb("V", [128, NCH, 2, 66], BF16)
    E = kb.sb("E", [65, 64], BF16)
    for hh in range(2):
        kb.dma("sp", KT[:, hh, :], io["KT"][hh], r=[], w=KT)
        kb.dma("sp", QT[:, hh, :], io["QT"][hh], r=[], w=QT)
        kb.dma("sp", QC[:, hh, :], io["QcT"][hh], r=[], w=QC)
    vsrc = io["V"]
    for i in range(0, NCH, 11):
        kb.dma("sp", Vst[:, i:i + 11, :], vsrc[:, i:i + 11, :], r=[], w=Vst)
    cf = kb.sb("cf", [128, 512])
    kb.op("dve", lambda e: e.memset(cf[:], 1.0), r=[], w=[cf])
    kb.copy("dve", V[:, :, :, 64:65], cf[:, 0:NCH * 2].rearrange("p (n h o) -> p n h o", h=2, o=1), r=[cf], w=[V])
    kb.copy("dve", V[:, :, :, 0:64], Vst[:].rearrange("p n (h d) -> p n h d", h=2), r=[Vst], w=[V])
    Ef = kb.sb("Ef", [65, 64])
    kb.op("dve", lambda e: e.memset(Ef[:], 0.0), r=[], w=[Ef])
    kb.op("dve", lambda e: e.memset(Ef[64:65, :], 1.0), r=[], w=[Ef])
    kb.copy("dve", E[:], Ef[:], r=[Ef], w=[E])
    zf = kb.sb("zf", [65, 512])
    kb.op("dve", lambda e: e.memset(zf[:], 0.0), r=[], w=[zf])
    pst = [kb.ps("pst%d" % i, [128, 2, 512]) for i in range(3)]
    pot = [kb.ps("pot0", [128, 512])] * 2
    pbc = kb.ps("pbc", [128, 512])
    PT = [kb.sb("PT%d" % i, [128, 2, 512], BF16) for i in range(4)]
    osb = [kb.sb("osb%d" % i, [65, 512]) for i in range(2)]
    rd = [kb.sb("rd%d" % i, [65, 512]) for i in range(2)]
    rh = [kb.sb("rh%d" % i, [65, 512], BF16) for i in range(2)]
    rl = [kb.sb("rl%d" % i, [65, 512], BF16) for i in range(2)]
    for t in rh + rl:
        kb.copy("dve", t[:], zf[:], r=[zf], w=[t])
    oT = [kb.sb("oT%d" % i, [64, 512], BF16) for i in range(2)]
    for t in rd:
        kb.op("dve", lambda e: e.memset(t[:], 0.0), r=[], w=[t])
    cnt = {"it": 0, "blk": 0}

    pending = []

    def finalize(po, ob, rdt, rht, rlt, ot, nq, out_ap):
        kb.copy("act", ob[:, 0:nq], po[0:65, 0:nq], r=[po], w=[ob])
        kb.op("dve", lambda e: e.reciprocal(out=rdt[64:65, 0:nq], in_=ob[64:65, 0:nq]), r=[ob], w=[rdt])
        kb.copy("dve", rht[64:65, 0:nq], rdt[64:65, 0:nq], r=[rdt], w=[rht])
        kb.tt(rlt[64:65, 0:nq], rdt[64:65, 0:nq], rht[64:65, 0:nq], ALU.subtract, r=[rdt, rht], w=[rlt])
        kb.mm(pbc[0:64, 0:nq], E[:, :], rht[:, 0:nq], True, False, r=[E, rht], w=[pbc])
        kb.mm(pbc[0:64, 0:nq], E[:, :], rlt[:, 0:nq], False, True, r=[E, rlt], w=[pbc])
        kb.tt(ot[:, 0:nq], pbc[0:64, 0:nq], ob[0:64, 0:nq], ALU.mult, r=[ob, pbc], w=[ot])
        kb.dma("sp", out_ap, ot[:, 0:nq], r=[ot], w=io["mixT_res"])

    def block(q_ap, qres, nq, key_tiles, hh, out_ap):
        k_ = cnt["blk"] % 2
        po, ob, rdt, rht, rlt, ot = pot[k_], osb[k_], rd[k_], rh[k_], rl[k_], oT[k_]
        cnt["blk"] += 1
        nk = len(key_tiles)
        assert nk % 2 == 0
        pairs = [key_tiles[i:i + 2] for i in range(0, nk, 2)]
        prev = None

        def pv(pi, pr, ppt, last):
            for a, pkt in enumerate(pr):
                kb.mm(po[0:65, 0:nq], V[:, pkt, hh, 0:65], ppt[:, a, 0:nq], pi == 0 and a == 0, last and a == 1, r=[V, ppt], w=[po])

        queue = []
        for i, pr in enumerate(pairs):
            ps_ = pst[cnt["it"] % 3]
            pt = PT[cnt["it"] % 4]
            cnt["it"] += 1
            for a, kt in enumerate(pr):
                kb.mm(ps_[:, a, 0:nq], KT[:, hh, kt * 128:(kt + 1) * 128], q_ap, True, True, r=[KT, qres], w=[ps_])
            kb.act(pt[:, :, 0:nq], ps_[:, :, 0:nq], AF.Exp, r=[ps_], w=[pt])
            if i == 1 and pending:
                finalize(*pending.pop())
            queue.append((i, pr, pt))
            if len(queue) > 2:
                pv(*queue.pop(0), False)
        if pending:
            finalize(*pending.pop())
        while queue:
            q_ = queue.pop(0)
            pv(*q_, len(queue) == 0)
        if pending:
            finalize(*pending.pop())
        pending.append((po, ob, rdt, rht, rlt, ot, nq, out_ap))

    for hh in range(2):
        for qb in range(SEQ // 512):
            block(QT[:, hh, qb * 512:(qb + 1) * 512], QT, 512, list(range(NCH)), hh,
                  io["mixT"][hh * 64:(hh + 1) * 64, qb * 512:(qb + 1) * 512])
        if need_ctx:
            block(QC[:, hh, :], QC, 256, [64, 65], hh, io["mixT"][hh * 64:(hh + 1) * 64, SEQ:B_T])
    while pending:
        finalize(*pending.pop())
    kb.pop()


def lin_attn(kb, cst, dk, nv, qT, kT, ktok, vp, a, a_on_G, dirn, emit):
    kb.push()
    seq = SEQ_F if dirn == 0 else SEQ_B
    mask = cst["triu"] if dirn == 0 else cst["tril"]
    Gs = kb.sb("Gs", [dk, nv, NCH])
    Ar = kb.sb("Ar", [dk, nv, NCH])
    Cs = kb.sb("Cs", [dk, nv, NCH], BF16)
    pg = [kb.ps("pg%d" % i, [128, 512]) for i in range(2)]
    pp = [kb.ps("pp%d" % i, [128, 512]) for i in range(2)]
    po = [kb.ps("po%d" % i, [128, 512]) for i in range(2)]
    PTm = [kb.sb("PTm%d" % i, [128, 128], BF16) for i in range(3)]
    gsz = 512 // nv
    for gi, s0 in enumerate(range(0, NCH, gsz)):
        pgt = pg[gi % 2]
        ss = list(range(s0, min(NCH, s0 + gsz)))
        for i, s in enumerate(ss):
            n = seq[s]
            kb.mm(pgt[0:dk, i * nv:(i + 1) * nv], ktok[:, n, :], vp[:, n, :], True, True, r=[ktok, vp], w=[pgt])
        if DBG < 10.6:
            continue
        for i, s in enumerate(ss):
            n = seq[s]
            if a_on_G:
                kb.tt(Gs[:, :, s], pgt[0:dk, i * nv:(i + 1) * nv], a[:, n:n + 1].broadcast_to([dk, nv]), ALU.mult, r=[pgt, a], w=[Gs])
            else:
                kb.copy("dve", Gs[:, :, s], pgt[0:dk, i * nv:(i + 1) * nv], r=[pgt], w=[Gs])
    if DBG < 10.61:
        kb.pop()
        return
    if dirn == 0:
        kb.copy("pool", Ar[:, :, 2:NCH], a[:, 0:64].unsqueeze(1).broadcast_to([dk, nv, 64]), r=[a], w=[Ar])
        kb.copy("pool", Ar[:, :, 0:2], a[:, 64:66].unsqueeze(1).broadcast_to([dk, nv, 2]), r=[a], w=[Ar])
    else:
        kb.copy("pool", Ar[:], a[:, ::-1].unsqueeze(1).broadcast_to([dk, nv, NCH]), r=[a], w=[Ar])
    kb.op("pool", lambda e: e.memset(Ar[:, :, 0:1], 0.0), r=[], w=[Ar])
    kb.op("dve", lambda e: e.tensor_tensor_scan(out=Cs[:].rearrange("p v s -> p (v s)"), data0=Ar[:].rearrange("p v s -> p (v s)"),
                                                 data1=Gs[:].rearrange("p v s -> p (v s)"), initial=0.0, op0=ALU.mult, op1=ALU.add),
          r=[Ar, Gs], w=[Cs])
    if DBG < 10.62:
        kb.pop()
        return
    it = 0

    def issue_pv(pot, i, n, ptm):
        s = seq.index(n)
        t0 = n * 128
        kb.mm(pot[:, i * nv:(i + 1) * nv], ptm[:], vp[:, n, :], True, s == 0, r=[ptm, vp], w=[pot])
        if s > 0:
            kb.mm(pot[:, i * nv:(i + 1) * nv], qT[:, t0:t0 + 128], Cs[:, :, s - 1], False, True, r=[qT, Cs], w=[pot])

    for gi, ns in enumerate(seq_groups(seq, gsz)):
        pot = po[gi % 2]
        prev = None
        for i, n in enumerate(ns):
            t0 = n * 128
            ppt = pp[it % 2]
            ptm = PTm[it % 3]
            it += 1
            kb.mm(ppt[:, 0:128], kT[:, t0:t0 + 128], qT[:, t0:t0 + 128], True, True, r=[kT, qT], w=[ppt])
            kb.tt(ptm[:], ppt[:, 0:128], mask[:], ALU.mult, r=[ppt, mask], w=[ptm])
            if prev is not None:
                issue_pv(pot, *prev)
            prev = (i, n, ptm)
        issue_pv(pot, *prev)
        emit(pot, ns)
    kb.pop()


def out_norm_gate(kb, cst, hsum, nw_d, gate_d, gate_func, extra, mix_rows, io, name):
    kb.push()
    nw = bcast_load(kb, name + "nw", nw_d, 64)
    gt = kb.sb(name + "gt", [128, NCH, 64])
    kb.dma("sp", gt[:], gate_d, r=[], w=gt)
    sq = kb.sb(name + "sq", [128, NCH, 64])
    st = kb.sb(name + "st", [128, 3, NCH])
    kb.act(sq[:], hsum[:], AF.Square, r=[hsum], w=[sq])
    kb.op("dve", lambda e: e.tensor_reduce(out=st[:, 0, :], in_=sq[:], axis=AX.X, op=ALU.add), r=[sq], w=[st])
    rstd_of(kb, st[:, 0, :], NCH, 64, st[:, 1, :], st[:, 2, :], r=[st], w=[st])
    if DBG < 10.71:
        kb.pop()
        return
    kb.tt(hsum[:], hsum[:], st[:, 2, :].unsqueeze(2).broadcast_to([128, NCH, 64]), ALU.mult, r=[hsum, st], w=[hsum])
    kb.tt(hsum[:], hsum[:], nw[:].unsqueeze(1).broadcast_to([128, NCH, 64]), ALU.mult, r=[hsum, nw], w=[hsum])
    if extra is not None:
        kb.tt(hsum[:], hsum[:], extra[:], ALU.add, r=[hsum, extra], w=[hsum])
    kb.act(gt[:], gt[:], gate_func, r=[gt], w=[gt])
    kb.tt(hsum[:], hsum[:], gt[:], ALU.mult, r=[hsum, gt], w=[hsum])
    if DBG < 10.72:
        kb.pop()
        return
    oT = kb.sb(name + "oT", [64, B_T], BF16)
    ptr = [kb.ps(name + "ptr%d" % i, [128, 512]) for i in range(2)]
    for gi, n0 in enumerate(range(0, NCH, 4)):
        nn = min(4, NCH - n0)
        p = ptr[gi % 2]
        for i in range(nn):
            kb.tr(p[0:64, i * 128:(i + 1) * 128], hsum[:, n0 + i, :], cst["ident"][:], r=[hsum, cst["ident"]], w=[p])
        kb.copy("act", oT[:, n0 * 128:(n0 + nn) * 128], p[0:64, 0:nn * 128], r=[p], w=[oT])
    if DBG < 10.73:
        kb.pop()
        return
    for i in range(4):
        kb.dma("sp", io["mixT"][mix_rows:mix_rows + 64, i * 2112:(i + 1) * 2112], oT[:, i * 2112:(i + 1) * 2112], r=[oT], w=io["mixT_res"])
    kb.pop()


def mlstm_B(kb, io, cst):
    kb.push()
    ident = cst["ident"]
    qT = kb.sb("mqT", [64, B_T], BF16)
    kT = kb.sb("mkT", [64, B_T], BF16)
    ktok = kb.sb("mktok", [128, NCH, 64], BF16)
    xtok = kb.sb("mxtok", [128, NCH, 64])
    kb.push()
    mx = kb.sb("mx", [64, B_T])
    acc = kb.sb("macc", [64, B_T])
    xcb = kb.sb("mxcb", [64, B_T], BF16)
    cw = kb.sb("cw", [64, 5])
    cb = kb.sb("cb", [64, 1])
    wq = kb.sb("wq", [64, 64], BF16)
    wk = kb.sb("wk", [64, 64], BF16)
    kb.dma("sp", cw[:], io["cw"], r=[], w=cw)
    kb.dma("sp", cb[:], io["cb"], r=[], w=cb)
    kb.dma("pool", wq[:], io["wq"], r=[], w=wq)
    kb.dma("pool", wk[:], io["wk"], r=[], w=wk)
    for i in range(4):
        kb.dma("sp", mx[:, i * 2112:(i + 1) * 2112], io["mxT"][:, i * 2112:(i + 1) * 2112], r=[], w=mx)
    for (s0, ln) in ((0, SEQ), (SEQ, NCTX)):
        kb.ts(acc[:, s0:s0 + ln], mx[:, s0:s0 + ln], cw[:, 2:3], None, ALU.mult, r=[mx, cw], w=[acc])
        for k in (0, 1, 3, 4):
            sh = k - 2
            a0 = max(0, -sh)
            a1 = ln - max(0, sh)
            kb.stt(acc[:, s0 + a0:s0 + a1], mx[:, s0 + a0 + sh:s0 + a1 + sh], cw[:, k:k + 1], acc[:, s0 + a0:s0 + a1],
                   ALU.mult, ALU.add, r=[mx, cw, acc], w=[acc])
    if DBG < 10.1:
        return
    kb.act(acc[:], acc[:], AF.Silu, r=[acc, cb], w=[acc], bias=cb[:, 0:1])
    kb.copy("dve", xcb[:], acc[:], r=[acc], w=[xcb])
    if DBG < 10.2:
        return
    pj = [kb.ps("mpj%d" % i, [128, 512]) for i in range(2)]
    gi = 0
    for c0 in range(0, B_T, 512):
        w_ = min(512, B_T - c0)
        p = pj[gi % 2]; gi += 1
        kb.mm(p[0:64, 0:w_], wq[:], xcb[:, c0:c0 + w_], True, True, r=[wq, xcb], w=[p])
        kb.op("act", lambda e: e.mul(qT[:, c0:c0 + w_], p[0:64, 0:w_], 0.125), r=[p], w=[qT])
        p = pj[gi % 2]; gi += 1
        kb.mm(p[0:64, 0:w_], wk[:], xcb[:, c0:c0 + w_], True, True, r=[wk, xcb], w=[p])
        kb.copy("dve", kT[:, c0:c0 + w_], p[0:64, 0:w_], r=[p], w=[kT])
    if DBG < 10.3:
        return
    for n0 in range(0, NCH, 8):
        nn = min(8, NCH - n0)
        p = pj[gi % 2]; gi += 1
        for i in range(nn):
            n = n0 + i
            kb.mm(p[:, i * 64:(i + 1) * 64], xcb[:, n * 128:(n + 1) * 128], wk[:], True, True, r=[xcb, wk], w=[p])
        kb.copy("dve", ktok[:, n0:n0 + nn, :].rearrange("p n d -> p (n d)"), p[:, 0:nn * 64], r=[p], w=[ktok])
        p = pj[gi % 2]; gi += 1
        for i in range(nn):
            n = n0 + i
            kb.tr(p[:, i * 64:(i + 1) * 64], acc[:, n * 128:(n + 1) * 128], ident[0:64, 0:64], r=[acc, ident], w=[p])
        kb.copy("act", xtok[:, n0:n0 + nn, :].rearrange("p n d -> p (n d)"), p[:, 0:nn * 64], r=[p], w=[xtok])
    kb.pop()
    if DBG < 10.4:
        return
    mg = kb.sb("mg", [128, NCH, 4])
    kb.dma("sp", mg[:], io["mg"], r=[], w=mg)
    gb = bcast_load(kb, "gb", io["gb"], 4)
    gp = kb.sb("gp", [128, 4, NCH])
    for g in range(4):
        kb.ts(gp[:, g, :], mg[:, :, g], gb[:, g:g + 1], None, ALU.add, r=[mg, gb], w=[gp])
    if DBG < 10.41:
        return
    lf = kb.sb("lf", [128, 2, NCH])
    for d in range(2):
        kb.act(lf[:, d, :], gp[:, 1 + 2 * d, :], AF.Sigmoid, r=[gp], w=[lf])
    kb.act(lf[:], lf[:], AF.Ln, r=[lf], w=[lf])
    if DBG < 10.42:
        return
    ones = kb.sb("ones", [128, 64], BF16)
    onesf = kb.sb("onesf", [128, 64])
    kb.op("dve", lambda e: e.memset(onesf[:], 1.0), r=[], w=[onesf])
    kb.copy("dve", ones[:], onesf[:], r=[onesf], w=[ones])
    lfh = kb.sb("lfh", [128, 2, NCH], BF16)
    lfl = kb.sb("lfl", [128, 2, NCH], BF16)
    kb.copy("dve", lfh[:], lf[:], r=[lf], w=[lfh])
    if DBG < 10.421:
        return
    kb.tt(lfl[:], lf[:], lfh[:], ALU.subtract, r=[lf, lfh], w=[lfl])
    if DBG < 10.422:
        return
    pgt = kb.ps("mpgate", [128, 4, 128])
    rr = kb.sb("mr", [128, 2, NCH])
    uu = kb.sb("mu", [128, 2, NCH])
    aa = [kb.sb("ma%d" % d, [64, NCH]) for d in range(2)]
    for d in range(2):
        tri = cst["triu_b"] if d == 0 else cst["tril_b"]
        kb.mm(pgt[:, d, 0:NCH], tri[:], lfh[:, d, :], True, False, r=[tri, lfh], w=[pgt])
        kb.mm(pgt[:, d, 0:NCH], tri[:], lfl[:, d, :], False, True, r=[tri, lfl], w=[pgt])
        if DBG < 10.423:
            continue
        kb.mm(pgt[0:64, 2 + d, 0:NCH], ones[:], lfh[:, d, :], True, False, r=[ones, lfh], w=[pgt])
        kb.mm(pgt[0:64, 2 + d, 0:NCH], ones[:], lfl[:, d, :], False, True, r=[ones, lfl], w=[pgt])
    if DBG < 10.43:
        return
    for d in range(2):
        kb.act(rr[:, d, :], pgt[:, d, 0:NCH], AF.Exp, r=[pgt], w=[rr])
        if DBG < 10.432:
            continue
        kb.stt(uu[:, d, :], pgt[:, d, 0:NCH], -1.0, gp[:, 2 * d, :], ALU.mult, ALU.add, r=[gp, pgt], w=[uu])
        if DBG < 10.433:
            continue
        kb.act(aa[d][:], pgt[0:64, 2 + d, 0:NCH], AF.Exp, r=[pgt], w=[aa[d]])
    if DBG < 10.44:
        return
    kb.act(uu[:], uu[:], AF.Exp, r=[uu], w=[uu])
    if DBG < 10.5:
        return
    vaug = kb.sb("vaug", [128, NCH, 66])
    kb.op("dve", lambda e: e.memset(vaug[:], 1.0), r=[], w=[vaug])
    if DBG < 10.501:
        return
    hsum = kb.sb("hsum", [128, NCH, 64])
    kb.dma("sp", hsum[:], io["mv"], r=[], w=hsum)
    if DBG < 10.502:
        return
    kb.copy("dve", vaug[:, :, 0:64], hsum[:], r=[hsum], w=[vaug])
    if DBG < 10.51:
        return
    vp = kb.sb("vp", [128, NCH, 66], BF16)
    vtmp = kb.sb("vtmp", [128, NCH, 66])
    dt_ = kb.sb("mdt", [128, 4, 8])
    htmp = kb.sb("htmp", [128, 7, 64])
    for d in range(2):
        kb.tt(vtmp[:], vaug[:], uu[:, d, :].unsqueeze(2).broadcast_to([128, NCH, 66]), ALU.mult, r=[vaug, uu], w=[vtmp])
        kb.copy("dve", vp[:], vtmp[:], r=[vtmp], w=[vp])

        def emit(pot, ns, d=d):
            g = len(ns)
            sl = nsl(ns)
            pv = pot[:, 0:g * 66].rearrange("p (g v) -> p g v", v=66)
            kb.tt(dt_[:, 0, 0:g], pv[:, :, 64], rr[:, d, sl], ALU.mult, r=[pot, rr], w=[dt_])
            kb.ts(dt_[:, 1, 0:g], dt_[:, 0, 0:g], -1.0, None, ALU.mult, r=[dt_], w=[dt_])
            kb.tt(dt_[:, 1, 0:g], dt_[:, 1, 0:g], dt_[:, 0, 0:g], ALU.max, r=[dt_], w=[dt_])
            kb.ts(dt_[:, 1, 0:g], dt_[:, 1, 0:g], 1.0, None, ALU.max, r=[dt_], w=[dt_])
            kb.op("dve", lambda e: e.reciprocal(out=dt_[:, 2, 0:g], in_=dt_[:, 1, 0:g]), r=[dt_], w=[dt_])
            kb.tt(dt_[:, 3, 0:g], dt_[:, 2, 0:g], rr[:, d, sl], ALU.mult, r=[dt_, rr], w=[dt_])
            if d == 0:
                kb.tt(hsum[:, sl, :], pv[:, :, 0:64], dt_[:, 3, 0:g].unsqueeze(2).broadcast_to([128, g, 64]), ALU.mult, r=[pot, dt_], w=[hsum])
            else:
                kb.tt(htmp[:, 0:g, :], pv[:, :, 0:64], dt_[:, 3, 0:g].unsqueeze(2).broadcast_to([128, g, 64]), ALU.mult, r=[pot, dt_], w=[htmp])
                kb.tt(hsum[:, sl, :], hsum[:, sl, :], htmp[:, 0:g, :], ALU.add, r=[hsum, htmp], w=[hsum], eng="pool")
        if DBG < 10.52:
            continue
        lin_attn(kb, cst, 64, 66, qT, kT, ktok, vp, aa[d], True, d, emit)
    if DBG < 10.7:
        return
    sk = bcast_load(kb, "msk", io["msk"], 64)
    kb.tt(xtok[:], xtok[:], sk[:].unsqueeze(1).broadcast_to([128, NCH, 64]), ALU.mult, r=[xtok, sk], w=[xtok])
    out_norm_gate(kb, cst, hsum, io["mnw"], io["mo"], AF.Sigmoid, xtok, 128, io, "mo")
    kb.pop()


def gla_B(kb, io, cst):
    kb.push()
    ident = cst["ident"]
    gvb = kb.sb("gvb", [128, NCH, 64], BF16)
    osum = kb.sb("osum", [128, NCH, 64])
    kb.dma("pool", gvb[:], io["gv"], r=[], w=gvb)
    ba = kb.sb("ba", [32, 2])
    kb.dma("sp", ba[:], io["ba"], r=[], w=ba)
    NP = 22
    rst = kb.sb("rst", [32, NP, 128])
    kb.op("pool", lambda e: e.memset(rst[:], 1.0), r=[], w=[rst])
    kb.op("pool", lambda e: e.memset(rst[:, :, 0:1], 0.0), r=[], w=[rst])
    qTt = kb.sb("gqT", [32, B_T], BF16)
    kTt = kb.sb("gkT", [32, B_T], BF16)
    ktok = kb.sb("gktok", [128, NCH, 32], BF16)
    a = kb.sb("ga", [32, NCH])
    for d in range(2):
        wa = kb.sb("wa%d" % d, [16, 32], BF16)
        kb.dma("pool", wa[:], io["wa"][d], r=[], w=wa)
        for n0 in range(0, NCH, NP):
            kb.push()
            c0 = n0 * 128
            cw_ = NP * 128
            ga = kb.sb("gain", [16, cw_], BF16)
            gq = kb.sb("gq", [32, cw_])
            gk = kb.sb("gk", [32, cw_])
            kb.dma("pool", ga[:], io["gaT"][d][:, c0:c0 + cw_], r=[], w=ga)
            kb.dma("sp", gq[:], io["gqT"][:, c0:c0 + cw_], r=[], w=gq)
            kb.dma("sp", gk[:], io["gkT"][:, c0:c0 + cw_], r=[], w=gk)
            la = kb.sb("la", [32, NP, 128])
            P = kb.sb("P", [32, NP, 128])
            Dm = kb.sb("Dm", [32, NP, 128])
            Ex = kb.sb("Ex", [32, NP, 128])
            khat = kb.sb("khat", [32, NP, 128])
            laf = la[:].rearrange("p n t -> p (n t)")
            Exf = Ex[:].rearrange("p n t -> p (n t)")
            pp = [kb.ps("gpp%d" % i, [128, 512]) for i in range(2)]
            for gi, x0 in enumerate(range(0, cw_, 512)):
                w_ = min(512, cw_ - x0)
                p = pp[gi % 2]
                kb.mm(p[0:32, 0:w_], wa[:], ga[:, x0:x0 + w_], True, True, r=[wa, ga], w=[p])
                kb.act(laf[:, x0:x0 + w_], p[0:32, 0:w_], AF.Sigmoid, r=[p, ba], w=[la], bias=ba[:, d:d + 1])
            kb.act(la[:], la[:], AF.Ln, r=[la], w=[la])
            kb.op("dve", lambda e: e.tensor_tensor_scan(out=P[:].rearrange("p n t -> p (n t)"), data0=rst[:].rearrange("p n t -> p (n t)"),
                                                         data1=laf, initial=0.0, op0=ALU.mult, op1=ALU.add), r=[rst, la], w=[P])
            kb.act(a[:, n0:n0 + NP], P[:, :, 127], AF.Exp, r=[P], w=[a], scale=1.0 / 16)
            kb.tt(Dm[:], P[:, :, 127:128].broadcast_to([32, NP, 128]), P[:], ALU.subtract, r=[P], w=[Dm])
            if d == 0:
                Bq = P
                Bke = Dm
            else:
                kb.tt(Dm[:], Dm[:], la[:], ALU.add, r=[Dm, la], w=[Dm])
                kb.tt(P[:], P[:], la[:], ALU.subtract, r=[P, la], w=[P])
                Bq = Dm
                Bke = P
            kb.act(Ex[:], Bq[:], AF.Exp, r=[Bq], w=[Ex], scale=1.0 / 16)
            kb.stt(qTt[:, c0:c0 + cw_], gq[:], float(32 ** -0.5), Exf, ALU.mult, ALU.mult, r=[gq, Ex], w=[qTt])
            kb.act(Ex[:], Bq[:], AF.Exp, r=[Bq], w=[Ex], scale=-1.0 / 16)
            kb.tt(kTt[:, c0:c0 + cw_], gk[:], Exf, ALU.mult, r=[gk, Ex], w=[kTt])
            kb.act(Ex[:], Bke[:], AF.Exp, r=[Bke], w=[Ex], scale=1.0 / 16)
            kb.tt(khat[:].rearrange("p n t -> p (n t)"), gk[:], Exf, ALU.mult, r=[gk, Ex], w=[khat])
            for gi, m0 in enumerate(range(0, NP, 16)):
                nn = min(16, NP - m0)
                p = pp[gi % 2]
                for i in range(nn):
                    kb.tr(p[:, i * 32:(i + 1) * 32], khat[:, m0 + i, :], ident[0:32, 0:32], r=[khat, ident], w=[p])
                kb.copy("dve", ktok[:, n0 + m0:n0 + m0 + nn, :].rearrange("p n d -> p (n d)"), p[:, 0:nn * 32], r=[p], w=[ktok])
            kb.pop()

        def emit(pot, ns, d=d):
            g = len(ns)
            sl = nsl(ns)
            pv = pot[:, 0:g * 64].rearrange("p (g v) -> p g v", v=64)
            if d == 0:
                kb.copy("dve", osum[:, sl, :], pv, r=[pot], w=[osum])
            else:
                kb.tt(osum[:, sl, :], pv, osum[:, sl, :], ALU.add, r=[osum, pot], w=[osum])
        lin_attn(kb, cst, 32, 64, qTt, kTt, ktok, gvb, a, False, d, emit)
    out_norm_gate(kb, cst, osum, io["gnw"], io["gr"], AF.Silu, None, 192, io, "go")
    kb.pop()


B_INPUTS = [("QT", [2, 96, SEQ], BF16), ("QcT", [2, 96, NCTX], BF16), ("KT", [2, 96, B_T], BF16), ("V", [128, NCH, 128], BF16),
            ("mxT", [64, B_T], F32), ("gqT", [32, B_T], F32), ("gkT", [32, B_T], F32), ("gaT", [2, 16, B_T], F32),
            ("mg", [128, NCH, 4], F32), ("mv", [128, NCH, 64], F32), ("mo", [128, NCH, 64], F32), ("gv", [128, NCH, 64], F32), ("gr", [128, NCH, 64], F32),
            ("cw", [64, 5], F32), ("cb", [64, 1], F32), ("wq", [64, 64], F32), ("wk", [64, 64], F32), ("gb", [4], F32),
            ("mnw", [64], F32), ("msk", [64], F32), ("wa", [2, 16, 32], F32), ("ba", [32, 2], F32), ("gnw", [64], F32)]


def build_B(need_ctx=True, parts=("attn", "mlstm", "gla")):
    nc = bass.Bass("TRN2", target_bir_lowering=False)
    io = IO()
    for nm, shp, dt in B_INPUTS:
        declare(nc, io, nm, shp, dt, "ExternalInput")
    cd = {}
    for nm in ("ident", "triu", "tril"):
        th = nc.dram_tensor("c_" + nm, CONST_SHAPES[nm], F32, kind="ExternalInput")
        cd[nm] = T(th, "c_" + nm)
    declare(nc, io, "mixT", [256, B_T], BF16, "ExternalOutput")
    with ExitStack() as st:
        kb = KB(nc, st)
        cst = load_consts(kb, cd, ["ident", "triu", "tril"])
        if "mlstm" in parts:
            mlstm_B(kb, io, cst)
        if "gla" in parts:
            gla_B(kb, io, cst)
        if "attn" in parts:
            attention_B(kb, io, need_ctx)
        while len(kb.stacks) > 1:
            kb.stacks.pop().close()
        kb.finish([io["mixT_res"]])
        print("phase B: n_inst", kb.n_inst, "n_wait", kb.n_wait, "dsems", kb.ndsem)
    return nc


def b_inmaps(inp, l, aout):
    cst, _ = consts()
    maps = []
    for b in range(NB):
        cs = [aout[4 * b + j] for j in range(4)]

        def cat_t(name, axis):
            parts = [np.take(c[name], np.arange(0, TL), axis=axis) for c in cs]
            parts.append(np.take(cs[0][name], np.arange(TL, NTOK), axis=axis))
            return np.concatenate(parts, axis=axis)
        qT = cat_t("qT", 2)
        kT = cat_t("kT", 2)
        v = cat_t("v", 0)
        fm = cat_t("fm", 1)
        tm = cat_t("tm", 0)
        for j in range(4):
            def pm(a):
                return np.ascontiguousarray(a.reshape(NCH, 128, a.shape[-1]).transpose(1, 0, 2))
            m = {"QT": np.ascontiguousarray(qT[2 * j:2 * j + 2, :, 0:SEQ]), "QcT": np.ascontiguousarray(qT[2 * j:2 * j + 2, :, SEQ:]),
                 "KT": np.ascontiguousarray(kT[2 * j:2 * j + 2]), "V": pm(v[:, 128 * j:128 * j + 128]),
                 "mxT": f32(fm[64 * j:64 * j + 64]), "gqT": f32(fm[256 + 32 * j:256 + 32 * j + 32]),
                 "gkT": f32(fm[384 + 32 * j:384 + 32 * j + 32]), "gaT": f32(fm[512:544].reshape(2, 16, B_T)),
                 "mg": pm(f32(tm[:, [j, 4 + j, 8 + j, 12 + j]])), "mv": pm(f32(tm[:, 16 + 64 * j:16 + 64 * j + 64])),
                 "mo": pm(f32(tm[:, 272 + 64 * j:272 + 64 * j + 64])), "gv": pm(f32(tm[:, 528 + 64 * j:528 + 64 * j + 64])),
                 "gr": pm(f32(tm[:, 784 + 64 * j:784 + 64 * j + 64])),
                 "cw": f32(inp["ml_conv_w"][l][:, 64 * j:64 * j + 64].T), "cb": f32(inp["ml_conv_b"][l][64 * j:64 * j + 64][:, None]),
                 "wq": f32(inp["ml_wq"][l][j]), "wk": f32(inp["ml_wk"][l][j]),
                 "gb": f32(inp["ml_gate_b"][l][[j, 4 + j, 8 + j, 12 + j]]),
                 "mnw": f32(inp["ml_norm_w"][l][64 * j:64 * j + 64]), "msk": f32(inp["ml_skip"][l][64 * j:64 * j + 64]),
                 "wa": f32(inp["gla_wa"][l][:, :, 32 * j:32 * j + 32]), "ba": f32(inp["gla_ba"][l][:, 32 * j:32 * j + 32].T),
                 "gnw": f32(inp["gla_norm_w"][l][64 * j:64 * j + 64]),
                 "c_ident": cst["ident"], "c_triu": cst["triu"], "c_tril": cst["tril"]}
            maps.append(m)
    return maps


def phase_C(kb, io, cst):
    ident = cst["ident"]
    NE = 32
    x1 = kb.sb("x1_all", [128, NT, 1024])
    h2T = kb.sb("h2T_all", [128, 8, NTOK], BF16)
    gTh = kb.sb("gTh", [32, NTOK], BF16)
    gTl = kb.sb("gTl", [32, NTOK], BF16)
    selb = kb.sb("selb", [32, NE * 128], BF16)
    kb.dma("pool", selb[:], cst["sel_d"], r=[], w=selb)
    gate2 = [kb.sb("gate2_%d" % i, [128, 1024]) for i in range(2)]
    kb.push()
    PS = [kb.ps("psC%d" % i, [128, 2, 512]) for i in range(2)]
    plg = kb.ps("plg", [128, 512])
    pgt = kb.ps("pgtC", [128, 512])
    mods = compute_mod(kb, io["cc"], io["w_mod"], io["b_mod"], [2, 3, 4, 5], PS, pre={5: (gate2[0], gate2[1])})
    n2 = bcast_load(kb, "n2", io["norm2_w"], 1024)
    G2 = []
    for i in range(2):
        g = mods[4][i]
        kb.stt(g[:], g[:], 1.0, n2[:], ALU.add, ALU.mult, r=[g, n2], w=[g])
        G2.append(g)
    gate1 = mods[2]
    S2 = mods[3]
    w_out = kb.sb("w_out", [128, 8, 1024], BF16)
    for kc in range(8):
        kb.dma("pool", w_out[:, kc, :], io["w_out"][kc * 128:(kc + 1) * 128, :], r=[], w=w_out)
    wr = kb.sb("wr", [128, 8, 36])
    kb.dma("sp", wr[:, :, 0:4], io["w_grp"].rearrange("(k p) n -> p k n", p=128), r=[], w=wr)
    kb.dma("sp", wr[:, :, 4:36], io["w_erouter"].rearrange("(k p) n -> p k n", p=128), r=[], w=wr)
    wrh = kb.sb("wrh", [128, 8, 36], BF16)
    wrl = kb.sb("wrl", [128, 8, 36], BF16)
    kb.copy("dve", wrh[:], wr[:], r=[wr], w=[wrh])
    kb.tt(wrl[:], wr[:], wrh[:], ALU.subtract, r=[wr, wrh], w=[wrl])
    rb = kb.sb("rb", [128, 36])
    kb.dma("sp", rb[:, 0:4], io["b_grp"].partition_broadcast(128), r=[], w=rb)
    kb.dma("sp", rb[:, 4:36], io["b_erouter"].partition_broadcast(128), r=[], w=rb)
    NB_ = 2
    xt = [kb.sb("xtC%d" % i, [128, 1024]) for i in range(NB_)]
    mT = [kb.sb("mTC%d" % i, [128, 8, 128], BF16) for i in range(NB_)]
    tmp = [kb.sb("tmpC", [128, 1024])] * NB_
    h2 = [kb.sb("h2C", [128, 1024])] * NB_
    h2l = [kb.sb("h2l", [128, 8, 128], BF16)] * NB_
    st = [kb.sb("stC%d" % i, [128, 16]) for i in range(NB_)]
    lg = [kb.sb("lg%d" % i, [128, 36]) for i in range(NB_)]
    rw = [kb.sb("rw%d" % i, [128, 6, 32]) for i in range(NB_)]
    m8 = [kb.sb("m8_%d" % i, [128, 8]) for i in range(NB_)]
    for ti in range(NT):
        b_ = ti % NB_
        mi = 1 if ti >= 16 else 0
        t0 = ti * 128
        X, MT, TMP, H2, H2L, ST, LG, RW, M8 = xt[b_], mT[b_], tmp[b_], h2[b_], h2l[b_], st[b_], lg[b_], rw[b_], m8[b_]
        kb.dma("sp", X[:], io["x"][t0:t0 + 128, :], r=[], w=X)
        kb.dma("sp", MT[:], io["mixT"][:, t0:t0 + 128].rearrange("(k p) t -> p k t", p=128), r=[], w=MT)
        for hf in range(2):
            for kc in range(8):
                kb.mm(PS[0][:, hf, :], MT[:, kc, :], w_out[:, kc, hf * 512:(hf + 1) * 512], kc == 0, kc == 7, r=[MT, w_out], w=[PS[0]])
        X1 = x1[:, ti, :]
        kb.tt(TMP[:], PS[0][:].rearrange("p a b -> p (a b)"), gate1[mi][:], ALU.mult, r=[PS[0], gate1[mi]], w=[TMP])
        kb.tt(X1, TMP[:], X[:], ALU.add, r=[TMP, X], w=[x1], eng="pool")
        kb.act(TMP[:], X1, AF.Square, r=[x1], w=[TMP, ST], accum_out=ST[:, 0:1])
        rstd_of(kb, ST[:, 0:1], 1, 1024, ST[:, 1:2], ST[:, 2:3], r=[ST], w=[ST])
        kb.stt(H2[:], X1, ST[:, 2:3], G2[mi][:], ALU.mult, ALU.mult, r=[x1, ST, G2[mi]], w=[H2])
        kb.tt(H2[:], H2[:], S2[mi][:], ALU.add, r=[H2, S2[mi]], w=[H2], eng="pool")
        for kc in range(8):
            kb.tr(PS[1][:, kc // 4, (kc % 4) * 128:(kc % 4 + 1) * 128], H2[:, kc * 128:(kc + 1) * 128], ident[:], r=[H2, ident], w=[PS[1]])
        hi = h2T[:, :, t0:t0 + 128]
        for a in range(2):
            kb.copy("act", h2T[:, 4 * a:4 * a + 4, t0:t0 + 128], PS[1][:, a, :].rearrange("p (b t) -> p b t", t=128), r=[PS[1]], w=[h2T])
        for a in range(2):
            kb.tt(H2L[:, 4 * a:4 * a + 4, :], PS[1][:, a, :].rearrange("p (b t) -> p b t", t=128), h2T[:, 4 * a:4 * a + 4, t0:t0 + 128],
                  ALU.subtract, r=[PS[1], h2T], w=[H2L])
        n = 0
        for kc in range(8):
            for (l_, r_, lr, rr_) in ((hi[:, kc, :], wrh[:, kc, :], h2T, wrh), (H2L[:, kc, :], wrh[:, kc, :], H2L, wrh), (hi[:, kc, :], wrl[:, kc, :], h2T, wrl)):
                kb.mm(plg[:, 0:36], l_, r_, n == 0, n == 23, r=[lr, rr_], w=[plg])
                n += 1
        kb.tt(LG[:], plg[:, 0:36], rb[:], ALU.add, r=[plg, rb], w=[LG])
        kb.op("dve", lambda e: e.tensor_reduce(out=ST[:, 4:5], in_=LG[:, 0:4], axis=AX.X, op=ALU.max), r=[LG], w=[ST])
        kb.tt(RW[:, 0, 0:4], LG[:, 0:4], ST[:, 4:5].broadcast_to([128, 4]), ALU.is_equal, r=[LG, ST], w=[RW])
        kb.ts(ST[:, 5:6], ST[:, 4:5], -1.0, None, ALU.mult, r=[ST], w=[ST])
        kb.act(RW[:, 1, 0:4], LG[:, 0:4], AF.Exp, r=[LG, ST], w=[RW, ST], bias=ST[:, 5:6], accum_out=ST[:, 6:7])
        kb.op("dve", lambda e: e.reciprocal(out=ST[:, 7:8], in_=ST[:, 6:7]), r=[ST], w=[ST])
        kb.ts(RW[:, 2, 0:4], RW[:, 0, 0:4], 1e9, -1e9, ALU.mult, ALU.add, r=[RW], w=[RW])
        EM = RW[:, 3, :]
        kb.tt(RW[:, 3, :].rearrange("p (g e) -> p g e", e=8), LG[:, 4:36].rearrange("p (g e) -> p g e", e=8),
              RW[:, 2, 0:4].unsqueeze(2).broadcast_to([128, 4, 8]), ALU.add, r=[LG, RW], w=[RW])
        kb.op("dve", lambda e: e.max(out=M8[:], in_=EM), r=[RW], w=[M8])
        kb.tt(ST[:, 8:9], M8[:, 1:2], M8[:, 0:1], ALU.subtract, r=[M8], w=[ST])
        kb.act(ST[:, 8:9], ST[:, 8:9], AF.Exp, r=[ST], w=[ST])
        kb.ts(ST[:, 8:9], ST[:, 8:9], 1.0, None, ALU.add, r=[ST], w=[ST])
        kb.op("dve", lambda e: e.reciprocal(out=ST[:, 9:10], in_=ST[:, 8:9]), r=[ST], w=[ST])
        kb.ts(ST[:, 10:11], ST[:, 9:10], -1.0, 1.0, ALU.mult, ALU.add, r=[ST], w=[ST])
        kb.tt(ST[:, 11:12], ST[:, 9:10], ST[:, 7:8], ALU.mult, r=[ST], w=[ST])
        kb.tt(ST[:, 12:13], ST[:, 10:11], ST[:, 7:8], ALU.mult, r=[ST], w=[ST])
        kb.tt(RW[:, 4, :], EM, M8[:, 0:1].broadcast_to([128, 32]), ALU.is_equal, r=[RW, M8], w=[RW])
        kb.ts(RW[:, 4, :], RW[:, 4, :], ST[:, 11:12], None, ALU.mult, r=[RW, ST], w=[RW])
        kb.tt(RW[:, 5, :], EM, M8[:, 1:2].broadcast_to([128, 32]), ALU.is_equal, r=[RW, M8], w=[RW])
        kb.ts(RW[:, 5, :], RW[:, 5, :], ST[:, 12:13], None, ALU.mult, r=[RW, ST], w=[RW])
        kb.tt(RW[:, 4, :], RW[:, 4, :], RW[:, 5, :], ALU.add, r=[RW], w=[RW])
        kb.tr(pgt[0:32, 0:128], RW[:, 4, :], ident[:], r=[RW, ident], w=[pgt])
        kb.copy("dve", gTh[:, t0:t0 + 128], pgt[0:32, 0:128], r=[pgt], w=[gTh])
        kb.tt(gTl[:, t0:t0 + 128], pgt[0:32, 0:128], gTh[:, t0:t0 + 128], ALU.subtract, r=[pgt, gTh], w=[gTl])
    kb.pop()
    kb.push()
    NSLOT = 4
    EG = 2
    wg = [kb.sb("wg%d" % i, [128, 8, 256], BF16) for i in range(NSLOT)]
    wu = [kb.sb("wu%d" % i, [128, 8, 256], BF16) for i in range(NSLOT)]
    wd = [kb.sb("wd%d" % i, [128, 2, 1024], BF16) for i in range(NSLOT)]
    pgu = [kb.ps("pgu%d" % i, [128, 2, 256]) for i in range(2)]
    pbc = kb.ps("pbcC", [128, 512])
    pacc = [kb.ps("pacc%d" % i, [128, 512]) for i in range(4)]
    sg = [kb.sb("sg%d" % i, [128, 256], BF16) for i in range(2)]
    tu = [kb.sb("tu%d" % i, [128, 256]) for i in range(2)]
    aT = [kb.sb("aT%d" % i, [128, 256], BF16) for i in range(4)]
    fl = [kb.sb("fl%d" % i, [128, 512]) for i in range(2)]
    TG = 256
    it = {"f": 0, "a": 0, "fl": 0}
    pend = []

    def load_expert(e):
        s_ = e % NSLOT
        kb.dma("pool", wg[s_][:], io["w_gate"][e].rearrange("(k p) f -> p k f", p=128), r=[], w=wg[s_])
        kb.dma("pool", wu[s_][:], io["w_up"][e].rearrange("(k p) f -> p k f", p=128), r=[], w=wu[s_])
        kb.dma("pool", wd[s_][:], io["w_down"][e].rearrange("(c p) d -> p c d", p=128), r=[], w=wd[s_])

    for e in range(min(NSLOT, NE)):
        load_expert(e)
    for g0 in range(0, NE, EG):
        for tg in range(NTOK // TG):
            c0 = tg * TG
            mi = 1 if tg >= 8 else 0
            first = True
            for e in range(g0, g0 + EG):
                s_ = e % NSLOT
                kb.mm(pbc[:, 0:TG], selb[:, e * 128:(e + 1) * 128], gTh[:, c0:c0 + TG], True, False, r=[selb, gTh], w=[pbc])
                kb.mm(pbc[:, 0:TG], selb[:, e * 128:(e + 1) * 128], gTl[:, c0:c0 + TG], False, True, r=[selb, gTl], w=[pbc])
                for fc in range(2):
                    p = pgu[it["f"] % 2]
                    sgt = sg[it["f"] % 2]
                    tut = tu[it["f"] % 2]
                    it["f"] += 1
                    at = aT[it["a"] % 4]
                    it["a"] += 1
                    for kc in range(8):
                        kb.mm(p[:, 0, :], wg[s_][:, kc, fc * 128:(fc + 1) * 128], h2T[:, kc, c0:c0 + TG], kc == 0, kc == 7, r=[wg[s_], h2T], w=[p])
                    for kc in range(8):
                        kb.mm(p[:, 1, :], wu[s_][:, kc, fc * 128:(fc + 1) * 128], h2T[:, kc, c0:c0 + TG], kc == 0, kc == 7, r=[wu[s_], h2T], w=[p])
                    kb.act(sgt[:], p[:, 0, :], AF.Silu, r=[p], w=[sgt])
                    kb.tt(tut[:], p[:, 1, :], sgt[:], ALU.mult, r=[p, sgt], w=[tut])
                    kb.tt(at[:], pbc[:, 0:TG], tut[:], ALU.mult, r=[pbc, tut], w=[at])
                    while pend:
                        pend.pop(0)()

                    def down(at=at, s_=s_, fc=fc, st_=(first and fc == 0), sp_=((e == g0 + EG - 1) and fc == 1)):
                        for sub in range(2):
                            for hf in range(2):
                                kb.mm(pacc[sub * 2 + hf][:, :], at[:, sub * 128:(sub + 1) * 128], wd[s_][:, fc, hf * 512:(hf + 1) * 512],
                                      st_, sp_, r=[at, wd[s_]], w=[pacc[sub * 2 + hf]])
                    pend.append(down)
                first = False
            while pend:
                pend.pop(0)()
            for sub in range(2):
                ti = tg * 2 + sub
                for hf in range(2):
                    f = fl[it["fl"] % 2]
                    it["fl"] += 1
                    kb.tt(f[:], pacc[sub * 2 + hf][:, :], gate2[mi][:, hf * 512:(hf + 1) * 512], ALU.mult, r=[pacc[sub * 2 + hf], gate2[mi]], w=[f])
                    kb.tt(x1[:, ti, hf * 512:(hf + 1) * 512], x1[:, ti, hf * 512:(hf + 1) * 512], f[:], ALU.add, r=[x1, f], w=[x1], eng="pool")
        for e in range(g0 + NSLOT, min(g0 + NSLOT + EG, NE)):
            if not os.environ.get("KNOLOAD"):
                load_expert(e)
    for ti in range(NT):
        kb.dma("sp", io["xo"][ti * 128:(ti + 1) * 128, :], x1[:, ti, :], r=[x1], w=io["xo_res"])
    kb.pop()


C_INPUTS = [("x", [NTOK, D], F32), ("mixT", [D, NTOK], BF16), ("cc", [128, 8, 2], F32), ("w_mod", [D, 6 * D], F32), ("b_mod", [6 * D], F32),
            ("w_out", [D, D], F32), ("norm2_w", [D], F32), ("w_grp", [D, 4], F32), ("b_grp", [4], F32),
            ("w_erouter", [D, 32], F32), ("b_erouter", [32], F32), ("w_gate", [32, D, 256], F32), ("w_up", [32, D, 256], F32),
            ("w_down", [32, 256, D], F32)]


def build_C():
    nc = bass.Bass("TRN2", target_bir_lowering=False)
    io = IO()
    for nm, shp, dt in C_INPUTS:
        declare(nc, io, nm, shp, dt, "ExternalInput")
    cd = {}
    for nm in ("ident", "sel"):
        th = nc.dram_tensor("c_" + nm, CONST_SHAPES[nm], F32, kind="ExternalInput")
        cd[nm] = T(th, "c_" + nm)
    declare(nc, io, "xo", [NTOK, D], F32, "ExternalOutput")
    with ExitStack() as st:
        kb = KB(nc, st)
        cst = load_consts(kb, cd, ["ident"])
        cst["sel_d"] = cd["sel"][:]
        phase_C(kb, io, cst)
        while len(kb.stacks) > 1:
            kb.stacks.pop().close()
        kb.finish([io["xo_res"]])
        print("phase C: n_inst", kb.n_inst, "n_wait", kb.n_wait, "dsems", kb.ndsem)
    return nc


def c_inmaps(inp, l, xl, xc, bout):
    cst, _ = consts()
    maps = []
    for b in range(NB):
        full = np.zeros((D, B_T), dtype=bout[0].dtype)
        for j in range(4):
            m = bout[4 * b + j]
            full[128 * j:128 * j + 128] = m[0:128]
            full[512 + 64 * j:512 + 64 * j + 64] = m[128:192]
            full[768 + 64 * j:768 + 64 * j + 64] = m[192:256]
        for j in range(4):
            core = 4 * b + j
            cols = np.concatenate([np.arange(j * TL, (j + 1) * TL), np.arange(SEQ, B_T)])
            mm_ = {"x": f32(core_tokens(xl, xc, core)), "mixT": np.ascontiguousarray(full[:, cols]),
                   "cc": cc_layout(inp["c"][b], inp["c_ctx"]), "c_ident": cst["ident"], "c_sel": cst["sel"]}
            for nm in ("w_mod", "b_mod", "w_out", "norm2_w", "w_grp", "b_grp", "w_erouter", "b_erouter", "w_gate", "w_up", "w_down"):
                mm_[nm] = f32(inp[nm][l])
            maps.append(mm_)
    return maps


def build_CA():
    nc = bass.Bass("TRN2", target_bir_lowering=False)
    io = IO()
    for nm, shp, dt in C_INPUTS:
        declare(nc, io, nm, shp, dt, "ExternalInput")
    ioA = IO()
    for nm, shp in A_INPUTS:
        if nm in ("x", "cc"):
            continue
        dn = nm + "_n" if nm in ("w_mod", "b_mod") else nm
        declare(nc, io, dn, shp, F32, "ExternalInput")
        ioA[nm] = io[dn]
    cd = {}
    for nm in ("ident", "sel"):
        th = nc.dram_tensor("c_" + nm, CONST_SHAPES[nm], F32, kind="ExternalInput")
        cd[nm] = T(th, "c_" + nm)
    declare(nc, io, "xo", [NTOK, D], F32, "ExternalOutput")
    for nm, shp, dt in A_OUTPUTS:
        declare(nc, io, nm, shp, dt, "ExternalOutput")
        ioA[nm] = io[nm]
        ioA[nm + "_res"] = io[nm + "_res"]
    ioA["cc"] = io["cc"]
    ioA["x"] = io["xo"]
    ioA["x_res"] = io["xo_res"]
    with ExitStack() as st:
        kb = KB(nc, st)
        cst = load_consts(kb, cd, ["ident"])
        cst["sel_d"] = cd["sel"][:]
        kb.push()
        phase_C(kb, io, cst)
        kb.pop()
        phase_A(kb, ioA, cst)
        while len(kb.stacks) > 1:
            kb.stacks.pop().close()
        kb.finish([io["xo_res"]] + [io[nm + "_res"] for nm, _, _ in A_OUTPUTS])
        print("phase C+A: n_inst", kb.n_inst, "n_wait", kb.n_wait, "dsems", kb.ndsem)
    return nc


def ca_inmaps(inp, l, xl, xc, bout):
    cst, rope = consts()
    maps = c_inmaps(inp, l, xl, xc, bout)
    for core, m in enumerate(maps):
        j = core % 4
        m["rope"] = f32(rope[j * TL:(j + 1) * TL])
        m["w_mod_n"] = f32(inp["w_mod"][l + 1])
        m["b_mod_n"] = f32(inp["b_mod"][l + 1])
        for nm in ("norm1_w", "w_in", "q_a_norm", "w_uq", "kv_a_norm", "w_ukv", "q_norm_w", "k_norm_w"):
            m[nm] = f32(inp[nm][l + 1])
    return maps


def _run(nc, maps):
    return run_bass_kernel_spmd(nc, maps, core_ids=list(range(NCORE))).results


def kernel(**inputs):
    inp = {k: np.asarray(v) for k, v in inputs.items()}
    xl = f32(inp["x"])
    xc = f32(inp["ctx"])
    ra = _run(build_A(), a_inmaps(inp, 0, xl, xc))
    for l in range(DEPTH):
        aout = [{k: np.asarray(r[k]) for k in ("qT", "kT", "v", "fm", "tm")} for r in ra]
        del ra
        rb = _run(build_B(True), b_inmaps(inp, l, aout))
        bout = [np.asarray(r["mixT"]) for r in rb]
        del rb, aout
        if l + 1 < DEPTH:
            rc = _run(build_CA(), ca_inmaps(inp, l, xl, xc, bout))
        else:
            rc = _run(build_C(), c_inmaps(inp, l, xl, xc, bout))
        xl_n = np.empty_like(xl)
        xc_n = np.empty_like(xc)
        for core in range(NCORE):
            b, j = core // 4, core % 4
            xo = np.asarray(rc[core]["xo"], dtype=np.float32)
            xl_n[b, j * TL:(j + 1) * TL] = xo[:TL]
            if j == 0:
                xc_n[b] = xo[TL:]
        xl, xc = xl_n, xc_n
        ra = rc
        del bout
    return xl
```
